# Optimizing a Trainium2 kernel written in Bass

```python
import math
import jax, jax.numpy as jnp
from jax import lax
import numpy as np

D_MODEL = 1024
BATCH = 8
SEQ = 2048
DEPTH = 2

D_MIX = D_MODEL
DA_HEADS = 4
DA_HEAD_DIM = 64
DA_V_DIM = 2 * DA_HEAD_DIM
DA_WIDTH = DA_HEADS * DA_V_DIM
DA_QK_WIDTH = DA_HEADS * 2 * DA_HEAD_DIM
Q_BLOCK = 128
ROPE_THETA = 10000.0
ML_HEADS = 4
ML_HEAD_DIM = 64
ML_WIDTH = ML_HEADS * ML_HEAD_DIM
ML_CHUNK = 64
ML_CONV = 4
S5_GROUP = 16
S5_STATE = 64
S5_WIDTH = D_MIX - DA_WIDTH - ML_WIDTH
S5_GROUPS = S5_WIDTH // S5_GROUP
N_EXPERTS = 32
TOP_K = 4
D_EXPERT = D_MODEL
SWIGLU_LIMIT = 7.0
SWIGLU_ALPHA = 1.702
DN_ALPHA = (2 * DEPTH) ** 0.25
DN_BETA = (8 * DEPTH) ** -0.25
LN_EPS = 1e-5
NEG = -1e30

OFF_DA_K = DA_QK_WIDTH
OFF_DA_V = 2 * DA_QK_WIDTH
OFF_ML_X = OFF_DA_V + DA_WIDTH
OFF_ML_V = OFF_ML_X + ML_WIDTH
OFF_ML_O = OFF_ML_V + ML_WIDTH
OFF_ML_I = OFF_ML_O + ML_WIDTH
OFF_ML_F = OFF_ML_I + ML_HEADS
OFF_S5_U = OFF_ML_F + ML_HEADS
N_IN = OFF_S5_U + S5_WIDTH
IN_SPLITS = (OFF_DA_K, OFF_DA_V, OFF_ML_X, OFF_ML_V, OFF_ML_O, OFF_ML_I, OFF_ML_F, OFF_S5_U)

kernel_name = "hybrid_diffattn_mlstm_s5_moe_block"


def layer_norm(x, g, b):
    xf = x.astype(jnp.float32)
    mu = jnp.mean(xf, axis=-1, keepdims=True)
    var = jnp.mean(jnp.square(xf - mu), axis=-1, keepdims=True)
    return (xf - mu) * lax.rsqrt(var + LN_EPS) * g + b


def rms_norm(t, g):
    tf = t.astype(jnp.float32)
    return tf * lax.rsqrt(jnp.mean(jnp.square(tf), axis=-1, keepdims=True) + LN_EPS) * g


def rope_tables(positions):
    inv = ROPE_THETA ** (-jnp.arange(0, DA_HEAD_DIM, 2, dtype=jnp.float32) / DA_HEAD_DIM)
    ang = positions.astype(jnp.float32)[..., None] * inv
    return jnp.cos(ang), jnp.sin(ang)


def apply_rope(t, cos, sin):
    t = t.astype(jnp.float32)
    half = DA_HEAD_DIM // 2
    t1, t2 = t[..., :half], t[..., half:]
    cs = cos[:, :, None, None, :]
    sn = sin[:, :, None, None, :]
    return jnp.concatenate([t1 * cs - t2 * sn, t2 * cs + t1 * sn], axis=-1)


def diff_attention(q, k, v, cos, sin, lam, lam_init, gain):
    B, S, _ = q.shape
    q = apply_rope(q.reshape(B, S, DA_HEADS, 2, DA_HEAD_DIM), cos, sin)
    k = apply_rope(k.reshape(B, S, DA_HEADS, 2, DA_HEAD_DIM), cos, sin)
    q = q.transpose(0, 2, 3, 1, 4)
    k = k.transpose(0, 2, 3, 1, 4)
    v = v.astype(jnp.float32).reshape(B, S, DA_HEADS, DA_V_DIM).transpose(0, 2, 1, 3)
    nb = S // Q_BLOCK
    qb = jnp.moveaxis(q.reshape(B, DA_HEADS, 2, nb, Q_BLOCK, DA_HEAD_DIM), 3, 0)
    kpos = jnp.arange(S)
    scale = DA_HEAD_DIM ** -0.5

    def attend(args):
        qi, bi = args
        s = jnp.einsum('bhcqd,bhckd->bhcqk', qi, k) * scale
        qpos = bi * Q_BLOCK + jnp.arange(Q_BLOCK)
        s = jnp.where(kpos[None, :] <= qpos[:, None], s, NEG)
        p = jax.nn.softmax(s, axis=-1)
        a = p[:, :, 0] - lam * p[:, :, 1]
        return jnp.einsum('bhqk,bhkv->bhqv', a, v)

    o = lax.map(attend, (qb, jnp.arange(nb)))
    o = o.transpose(1, 0, 3, 2, 4).reshape(B, S, DA_HEADS, DA_V_DIM)
    o = rms_norm(o, gain) * (1.0 - lam_init)
    return o.reshape(B, S, DA_WIDTH)


def mlstm(x_m, v, o_pre, i_pre, f_pre, conv_w, conv_b, w_q, w_k, gate_b, norm_g):
    B, S, _ = x_m.shape
    H, d, L = ML_HEADS, ML_HEAD_DIM, ML_CHUNK
    nc = S // L
    x_m = x_m.astype(jnp.float32)
    xc = lax.conv_general_dilated(
        x_m, conv_w.astype(jnp.float32)[:, None, :], window_strides=(1,),
        padding=[(ML_CONV - 1, 0)], dimension_numbers=('NWC', 'WIO', 'NWC'),
        feature_group_count=ML_WIDTH)
    xc = jax.nn.silu(xc + conv_b).reshape(B, S, H, d)
    q = jnp.einsum('bshd,hde->bhse', xc, w_q)
    k = jnp.einsum('bshd,hde->bhse', xc, w_k) * (d ** -0.5)
    v = v.astype(jnp.float32).reshape(B, S, H, d).transpose(0, 2, 1, 3)
    ig = (i_pre + gate_b[:H]).astype(jnp.float32).transpose(0, 2, 1)
    lf = jax.nn.log_sigmoid((f_pre + gate_b[H:]).astype(jnp.float32)).transpose(0, 2, 1)

    qc = q.reshape(B, H, nc, L, d)
    kc = k.reshape(B, H, nc, L, d)
    vc = v.reshape(B, H, nc, L, d)
    ic = ig.reshape(B, H, nc, L)
    bcum = jnp.cumsum(lf.reshape(B, H, nc, L), axis=-1)
    g = bcum[..., -1]
    a = g[..., None] - bcum + ic

    def step(carry, xs):
        C, n, m = carry
        k_, v_, a_, g_ = xs
        m_new = jnp.maximum(g_ + m, jnp.max(a_, axis=-1))
        dec = jnp.exp(g_ + m - m_new)
        w = jnp.exp(a_ - m_new[..., None])
        C_new = dec[..., None, None] * C + jnp.einsum('bhl,bhlv,bhlk->bhvk', w, v_, k_)
        n_new = dec[..., None] * n + jnp.einsum('bhl,bhlk->bhk', w, k_)
        return (C_new, n_new, m_new), (C, n, m)

    init = (jnp.zeros((B, H, d, d), jnp.float32), jnp.zeros((B, H, d), jnp.float32),
            jnp.full((B, H), NEG, jnp.float32))
    xs = (jnp.moveaxis(kc, 2, 0), jnp.moveaxis(vc, 2, 0), jnp.moveaxis(a, 2, 0), jnp.moveaxis(g, 2, 0))
    _, (C_prev, n_prev, m_prev) = lax.scan(step, init, xs)
    C_prev = jnp.moveaxis(C_prev, 0, 2)
    n_prev = jnp.moveaxis(n_prev, 0, 2)
    m_prev = jnp.moveaxis(m_prev, 0, 2)

    causal = jnp.tril(jnp.ones((L, L), dtype=bool))
    log_d = jnp.where(causal, bcum[..., :, None] - bcum[..., None, :] + ic[..., None, :], NEG)
    inter = bcum + m_prev[..., None]
    m = jnp.maximum(inter, jnp.max(log_d, axis=-1))
    s = jnp.einsum('bhcld,bhcsd->bhcls', qc, kc) * jnp.exp(log_d - m[..., None])
    dec = jnp.exp(inter - m)
    num = jnp.einsum('bhcls,bhcsd->bhcld', s, vc) + dec[..., None] * jnp.einsum('bhcvk,bhclk->bhclv', C_prev, qc)
    den = jnp.sum(s, axis=-1) + dec * jnp.einsum('bhck,bhclk->bhcl', n_prev, qc)
    h = num / jnp.maximum(jnp.abs(den), jnp.exp(-m))[..., None]
    h = h.reshape(B, H, S, d).transpose(0, 2, 1, 3)
    h = rms_norm(h, norm_g.reshape(H, d)).reshape(B, S, ML_WIDTH)
    return h * jax.nn.sigmoid(o_pre.astype(jnp.float32))


def complex_affine_combine(e1, e2):
    a1r, a1i, b1r, b1i = e1
    a2r, a2i, b2r, b2i = e2
    return (a1r * a2r - a1i * a2i, a1r * a2i + a1i * a2r,
            a2r * b1r - a2i * b1i + b2r, a2r * b1i + a2i * b1r + b2i)


def s5_layer(u, a_re, a_im, log_dt, b_re, b_im, c_re, c_im, d_skip, w_glu):
    B, S, _ = u.shape
    G, Hc, P = S5_GROUPS, S5_GROUP, S5_STATE
    u = u.astype(jnp.float32).reshape(B, S, G, Hc)
    a_re = a_re.astype(jnp.float32)
    a_im = a_im.astype(jnp.float32)
    dt = jnp.exp(log_dt.astype(jnp.float32))[:, None]
    mag = jnp.exp(a_re * dt)
    ab_re = mag * jnp.cos(a_im * dt)
    ab_im = mag * jnp.sin(a_im * dt)
    nr, ni = ab_re - 1.0, ab_im
    den = a_re * a_re + a_im * a_im
    fr = (nr * a_re + ni * a_im) / den
    fi = (ni * a_re - nr * a_im) / den
    bb_re = fr[..., None] * b_re - fi[..., None] * b_im
    bb_im = fr[..., None] * b_im + fi[..., None] * b_re
    bu_re = jnp.einsum('bsgh,gph->bsgp', u, bb_re)
    bu_im = jnp.einsum('bsgh,gph->bsgp', u, bb_im)
    A_re = jnp.broadcast_to(ab_re, (1, S, G, P))
    A_im = jnp.broadcast_to(ab_im, (1, S, G, P))
    _, _, x_re, x_im = lax.associative_scan(complex_affine_combine, (A_re, A_im, bu_re, bu_im), axis=1)
    y = (jnp.einsum('bsgp,ghp->bsgh', x_re, c_re) - jnp.einsum('bsgp,ghp->bsgh', x_im, c_im)
         + d_skip.reshape(G, Hc) * u)
    z = jnp.einsum('bsgh,ghj->bsgj', jax.nn.gelu(y), w_glu)
    out = z[..., :Hc] * jax.nn.sigmoid(z[..., Hc:])
    return out.reshape(B, S, S5_WIDTH)


def hybrid_mixer(h, cos, sin, layer, w_in, w_out, lam_q1, lam_k1, lam_q2, lam_k2, da_norm_g,
                 ml_conv_w, ml_conv_b, ml_w_q, ml_w_k, ml_gate_b, ml_norm_g,
                 s5_a_re, s5_a_im, s5_log_dt, s5_b_re, s5_b_im, s5_c_re, s5_c_im, s5_d, s5_w_glu):
    proj = jnp.einsum('bsd,de->bse', h, w_in)
    da_q, da_k, da_v, ml_x, ml_v, ml_o, ml_i, ml_f, s5_u = jnp.split(proj, IN_SPLITS, axis=-1)
    lam_init = 0.8 - 0.6 * math.exp(-0.3 * layer)
    lam = (jnp.exp(jnp.sum(lam_q1 * lam_k1).astype(jnp.float32))
           - jnp.exp(jnp.sum(lam_q2 * lam_k2).astype(jnp.float32)) + lam_init)
    y_da = diff_attention(da_q, da_k, da_v, cos, sin, lam, lam_init, da_norm_g)
    y_ml = mlstm(ml_x, ml_v, ml_o, ml_i, ml_f, ml_conv_w, ml_conv_b, ml_w_q, ml_w_k, ml_gate_b, ml_norm_g)
    y_s5 = s5_layer(s5_u, s5_a_re, s5_a_im, s5_log_dt, s5_b_re, s5_b_im, s5_c_re, s5_c_im, s5_d, s5_w_glu)
    y = jnp.concatenate([y_da, y_ml, y_s5], axis=-1)
    return jnp.einsum('bse,ed->bsd', y, w_out)


def moe_ffn(h, w_router, b_router, w_up, b_up, w_down, b_down):
    B, S, D = h.shape
    t = h.reshape(B * S, D)
    logits = (t @ w_router + b_router).astype(jnp.float32)
    top_v, top_i = lax.top_k(logits, TOP_K)
    probs = jax.nn.softmax(top_v, axis=-1)
    combine = jnp.einsum('tk,tke->te', probs, jax.nn.one_hot(top_i, N_EXPERTS, dtype=jnp.float32))

    def expert(acc, xs):
        w1, b1, w2, b2, gcol = xs
        z = t @ w1 + b1
        x_glu = jnp.minimum(z[:, :D_EXPERT], SWIGLU_LIMIT)
        x_lin = jnp.clip(z[:, D_EXPERT:], -SWIGLU_LIMIT, SWIGLU_LIMIT)
        act = x_glu * jax.nn.sigmoid(SWIGLU_ALPHA * x_glu) * (x_lin + 1.0)
        return acc + gcol[:, None] * (act @ w2 + b2).astype(jnp.float32), None

    acc0 = jnp.zeros((B * S, D), jnp.float32)
    y, _ = lax.scan(expert, acc0, (w_up, b_up, w_down, b_down, combine.T))
    return y.reshape(B, S, D)


def setup_inputs(seed: int = 0) -> dict:
    key = jax.random.key(seed)
    ks = iter(jax.random.split(key, 48))

    def nrm(shape, s):
        return jax.random.normal(next(ks), shape, jnp.float32) * s

    G, Hc, P = S5_GROUPS, S5_GROUP, S5_STATE
    x = nrm((BATCH, SEQ, D_MODEL), 1.0)
    c = nrm((BATCH, D_MODEL), 1.0)
    positions = jnp.broadcast_to(jnp.arange(SEQ, dtype=jnp.int32), (BATCH, SEQ))
    ada_w = nrm((DEPTH, 2, D_MODEL, 3 * D_MODEL), 0.5 * D_MODEL ** -0.5)
    ada_b = nrm((DEPTH, 2, 3 * D_MODEL), 0.02)
    w_in = nrm((DEPTH, D_MODEL, N_IN), D_MODEL ** -0.5)
    lam_q1 = nrm((DEPTH, DA_HEAD_DIM), 0.1)
    lam_k1 = nrm((DEPTH, DA_HEAD_DIM), 0.1)
    lam_q2 = nrm((DEPTH, DA_HEAD_DIM), 0.1)
    lam_k2 = nrm((DEPTH, DA_HEAD_DIM), 0.1)
    da_norm_g = 1.0 + nrm((DEPTH, DA_V_DIM), 0.02)
    ml_conv_w = nrm((DEPTH, ML_CONV, ML_WIDTH), ML_CONV ** -0.5)
    ml_conv_b = nrm((DEPTH, ML_WIDTH), 0.02)
    ml_w_q = nrm((DEPTH, ML_HEADS, ML_HEAD_DIM, ML_HEAD_DIM), ML_HEAD_DIM ** -0.5)
    ml_w_k = nrm((DEPTH, ML_HEADS, ML_HEAD_DIM, ML_HEAD_DIM), ML_HEAD_DIM ** -0.5)
    ml_gate_b = jnp.concatenate(
        [nrm((DEPTH, ML_HEADS), 0.1),
         jnp.linspace(3.0, 6.0, ML_HEADS, dtype=jnp.float32)[None, :] + nrm((DEPTH, ML_HEADS), 0.1)], axis=-1)
    ml_norm_g = 1.0 + nrm((DEPTH, ML_WIDTH), 0.02)
    s5_a_re = -0.5 + nrm((DEPTH, G, P), 0.01)
    s5_a_im = jnp.pi * jnp.arange(P, dtype=jnp.float32)[None, None, :] + nrm((DEPTH, G, P), 0.01)
    s5_log_dt = math.log(0.001) + jax.random.uniform(next(ks), (DEPTH, G), jnp.float32) * (math.log(0.1) - math.log(0.001))
    s5_b_re = nrm((DEPTH, G, P, Hc), (2 * Hc) ** -0.5)
    s5_b_im = nrm((DEPTH, G, P, Hc), (2 * Hc) ** -0.5)
    s5_c_re = nrm((DEPTH, G, Hc, P), P ** -0.5)
    s5_c_im = nrm((DEPTH, G, Hc, P), P ** -0.5)
    s5_d = nrm((DEPTH, S5_WIDTH), 0.5)
    s5_w_glu = nrm((DEPTH, G, Hc, 2 * Hc), Hc ** -0.5)
    w_out = nrm((DEPTH, D_MIX, D_MODEL), DN_BETA * D_MIX ** -0.5)
    ln_g = 1.0 + nrm((DEPTH, 2, D_MODEL), 0.02)
    ln_b = nrm((DEPTH, 2, D_MODEL), 0.02)
    w_router = nrm((DEPTH, D_MODEL, N_EXPERTS), D_MODEL ** -0.5)
    b_router = nrm((DEPTH, N_EXPERTS), 0.01)
    w_up = nrm((DEPTH, N_EXPERTS, D_MODEL, 2 * D_EXPERT), D_MODEL ** -0.5)
    b_up = nrm((DEPTH, N_EXPERTS, 2 * D_EXPERT), 0.02)
    w_down = nrm((DEPTH, N_EXPERTS, D_EXPERT, D_MODEL), DN_BETA * D_EXPERT ** -0.5)
    b_down = nrm((DEPTH, N_EXPERTS, D_MODEL), 0.02)
    return {"x": x, "c": c, "positions": positions, "ada_w": ada_w, "ada_b": ada_b, "w_in": w_in,
            "lam_q1": lam_q1, "lam_k1": lam_k1, "lam_q2": lam_q2, "lam_k2": lam_k2, "da_norm_g": da_norm_g,
            "ml_conv_w": ml_conv_w, "ml_conv_b": ml_conv_b, "ml_w_q": ml_w_q, "ml_w_k": ml_w_k,
            "ml_gate_b": ml_gate_b, "ml_norm_g": ml_norm_g,
            "s5_a_re": s5_a_re, "s5_a_im": s5_a_im, "s5_log_dt": s5_log_dt, "s5_b_re": s5_b_re,
            "s5_b_im": s5_b_im, "s5_c_re": s5_c_re, "s5_c_im": s5_c_im, "s5_d": s5_d, "s5_w_glu": s5_w_glu,
            "w_out": w_out, "ln_g": ln_g, "ln_b": ln_b, "w_router": w_router, "b_router": b_router,
            "w_up": w_up, "b_up": b_up, "w_down": w_down, "b_down": b_down}


def reference(x, c, positions, ada_w, ada_b, w_in, lam_q1, lam_k1, lam_q2, lam_k2, da_norm_g,
              ml_conv_w, ml_conv_b, ml_w_q, ml_w_k, ml_gate_b, ml_norm_g,
              s5_a_re, s5_a_im, s5_log_dt, s5_b_re, s5_b_im, s5_c_re, s5_c_im, s5_d, s5_w_glu,
              w_out, ln_g, ln_b, w_router, b_router, w_up, b_up, w_down, b_down):
    cos, sin = rope_tables(positions)
    c_act = jax.nn.silu(c)
    for l in range(DEPTH):
        mod = jnp.einsum('bd,jde->jbe', c_act, ada_w[l]) + ada_b[l][:, None, :]
        shift, scale, gate = jnp.split(mod[0], 3, axis=-1)
        h = x * (1.0 + scale[:, None, :]) + shift[:, None, :]
        y = hybrid_mixer(h, cos, sin, l, w_in[l], w_out[l], lam_q1[l], lam_k1[l], lam_q2[l], lam_k2[l],
                         da_norm_g[l], ml_conv_w[l], ml_conv_b[l], ml_w_q[l], ml_w_k[l], ml_gate_b[l],
                         ml_norm_g[l], s5_a_re[l], s5_a_im[l], s5_log_dt[l], s5_b_re[l], s5_b_im[l],
                         s5_c_re[l], s5_c_im[l], s5_d[l], s5_w_glu[l])
        x = layer_norm(DN_ALPHA * x + (1.0 + gate[:, None, :]) * y, ln_g[l, 0], ln_b[l, 0])
        shift, scale, gate = jnp.split(mod[1], 3, axis=-1)
        h = x * (1.0 + scale[:, None, :]) + shift[:, None, :]
        y = moe_ffn(h, w_router[l], b_router[l], w_up[l], b_up[l], w_down[l], b_down[l])
        x = layer_norm(DN_ALPHA * x + (1.0 + gate[:, None, :]) * y, ln_g[l, 1], ln_b[l, 1])
    return x
```

```python
import math
from contextlib import ExitStack

import numpy as np
import concourse.bass as bass
import concourse.mybir as mybir
from concourse.bass_utils import run_bass_kernel_spmd

F32 = mybir.dt.float32
BF16 = mybir.dt.bfloat16
I32 = mybir.dt.int32
AF = mybir.ActivationFunctionType
ALU = mybir.AluOpType
AX = mybir.AxisListType

D = 1024
S = 2048
NT = 16
KC = 8
DEPTH = 2
NE = 32
DN_ALPHA = (2 * DEPTH) ** 0.25
LN_EPS = 1e-5
N_IN = 2568

ENGS = ("pe", "act", "dve", "pool", "sp")


class Op:
    __slots__ = ("eng", "fn", "reads", "writes", "chan", "idx", "sig", "sigval",
                 "waits", "chan_count")

    def __init__(self, eng, fn, reads, writes, chan):
        self.eng, self.fn, self.reads, self.writes, self.chan = eng, fn, reads, writes, chan
        self.sig = False
        self.sigval = 0
        self.waits = []
        self.chan_count = 0


class Prog:
    def __init__(self, nc, same_engine_sync=True):
        self.nc = nc
        self.ops = []
        self.same_engine_sync = same_engine_sync
        self.barriers = []
        self.stack = ExitStack()

    def op(self, eng, fn, reads=(), writes=(), chan=None):
        o = Op(eng, fn, tuple(reads), tuple(writes), chan)
        o.idx = len(self.ops)
        self.ops.append(o)
        return o

    def pe(self, fn, reads=(), writes=()):
        return self.op("pe", fn, reads, writes)

    def act(self, fn, reads=(), writes=()):
        return self.op("act", fn, reads, writes)

    def dve(self, fn, reads=(), writes=()):
        return self.op("dve", fn, reads, writes)

    def pool(self, fn, reads=(), writes=()):
        return self.op("pool", fn, reads, writes)

    def dma(self, fn, reads=(), writes=(), chan="d0", eng="sp"):
        return self.op(eng, fn, reads, writes, chan)

    def barrier(self):
        self.barriers.append(len(self.ops))

    def sbuf(self, name, shape, dtype):
        return self.stack.enter_context(self.nc.sbuf_tensor(name, list(shape), dtype))

    def psum(self, name, shape, dtype):
        return self.stack.enter_context(self.nc.psum_tensor(name, list(shape), dtype))

    def resolve(self):
        ops = self.ops
        last_w, readers, last_on_eng, chan_total = {}, {}, {}, {}
        bset = sorted(set(self.barriers))
        bi = 0
        pending_barrier = {}
        deps_all = []
        for o in ops:
            while bi < len(bset) and bset[bi] <= o.idx:
                snap = (dict(last_on_eng), dict(chan_total))
                for e in ENGS:
                    pending_barrier[e] = snap
                bi += 1
            deps = set()
            cdeps = {}
            if o.eng in pending_barrier:
                leng, ctot = pending_barrier.pop(o.eng)
                for d in leng.values():
                    deps.add(d)
                for c, n in ctot.items():
                    cdeps[c] = max(cdeps.get(c, 0), n)
            for k in o.reads:
                d = last_w.get(k)
                if d is not None:
                    deps.add(d)
            for k in o.writes:
                d = last_w.get(k)
                if d is not None:
                    deps.add(d)
                for r in readers.get(k, ()):
                    deps.add(r)
            real = []
            for d in deps:
                if d is o:
                    continue
                if d.chan is not None:
                    cdeps[d.chan] = max(cdeps.get(d.chan, 0), chan_total[d.chan])
                    continue
                if d.eng == o.eng and (o.eng == "pe" or not self.same_engine_sync):
                    continue
                real.append(d)
                d.sig = True
            deps_all.append((real, cdeps))
            for k in o.writes:
                last_w[k] = o
                readers[k] = []
            for k in o.reads:
                readers.setdefault(k, []).append(o)
            if o.chan is not None:
                chan_total[o.chan] = chan_total.get(o.chan, 0) + 1
                o.chan_count = chan_total[o.chan]
            else:
                last_on_eng[o.eng] = o
        cnt = {e: 0 for e in ENGS}
        for o in ops:
            if o.chan is None and o.sig:
                cnt[o.eng] += 1
                o.sigval = cnt[o.eng]
        self.sig_counts = cnt
        self.chan_totals = chan_total
        waited = {e: {} for e in ENGS}
        for o, (real, cdeps) in zip(ops, deps_all):
            w = {}
            for d in real:
                w[d.eng] = max(w.get(d.eng, 0), d.sigval)
            for c, n in cdeps.items():
                w[("chan", c)] = max(w.get(("chan", c), 0), 16 * n)
            out = []
            for k, v in w.items():
                if waited[o.eng].get(k, 0) < v:
                    waited[o.eng][k] = v
                    out.append((k, v))
            o.waits = out

    def emit(self):
        nc = self.nc
        self.resolve()
        sems = {}
        for e in ENGS:
            sems[e] = self.stack.enter_context(nc.semaphore("s_" + e))
        for c in self.chan_totals:
            sems[("chan", c)] = self.stack.enter_context(nc.semaphore("c_" + c))
        by_eng = {e: [o for o in self.ops if o.eng == e] for e in ENGS}
        engobj = {"pe": "tensor", "act": "scalar", "dve": "vector", "pool": "gpsimd", "sp": "sync"}

        def make_section(e):
            def section(eng):
                for o in by_eng[e]:
                    for k, v in o.waits:
                        eng.wait_ge(sems[k], v)
                    ins = o.fn(eng)
                    if o.chan is not None:
                        ins.then_inc(sems[("chan", o.chan)], 16)
                    elif o.sig:
                        ins.then_inc(sems[e], 1)
            return section

        with nc.Block() as block:
            for e in ENGS:
                if by_eng[e]:
                    getattr(block, engobj[e])(make_section(e))
        self.stack.close()


class Builder:
    def __init__(self, cfg):
        self.cfg = cfg
        self.nc = bass.Bass("TRN2", target_bir_lowering=False)
        self.P = Prog(self.nc, same_engine_sync=cfg.get("ses", True))
        self.din = {}
        self.dout = {}
        self._uid = 0

    def inp(self, name, shape, dtype=F32):
        t = self.nc.dram_tensor(name, list(shape), dtype, kind="ExternalInput").ap()
        self.din[name] = t
        return t

    def outp(self, name, shape, dtype=F32):
        t = self.nc.dram_tensor(name, list(shape), dtype, kind="ExternalOutput").ap()
        self.dout[name] = t
        return t

    def uid(self, p="t"):
        self._uid += 1
        return f"{p}{self._uid}"

    def arena_init(self, nbytes):
        self.arena = self.P.sbuf("arena", [128, nbytes // 4], F32)
        self.arena_words = nbytes // 4
        self.aoff = 0

    def arena_reset(self):
        self.P.barrier()
        self.aoff = 0

    def carve(self, shape, dtype):
        esz = 2 if dtype == BF16 else 4
        n = 1
        for d_ in shape[1:]:
            n *= d_
        words = (n * esz + 3) // 4
        words = (words + 7) // 8 * 8
        assert self.aoff + words <= self.arena_words, ("arena overflow", self.aoff, words, self.arena_words)
        v = self.arena[0:shape[0], self.aoff:self.aoff + words]
        self.aoff += words
        if dtype != F32:
            v = v.bitcast(dtype)
        v = v[:, 0:n]
        if len(shape) == 3:
            v = v.rearrange("p (a b) -> p a b", b=shape[2])
        elif len(shape) == 4:
            v = v.rearrange("p (a b c) -> p a b c", b=shape[2], c=shape[3])
        return v

    def declare_common(self):
        P = self.P
        self.x_in = self.inp("x", [S, D])
        self.cT_in = self.inp("cT", [128, KC])
        self.ident_in = self.inp("ident", [128, 128])
        self.ada_w = self.inp("ada_w", [DEPTH, 2, D, 3 * D])
        self.ada_b = self.inp("ada_b", [DEPTH, 2, 3 * D])
        self.ln_g = self.inp("ln_g", [DEPTH, 2, D])
        self.ln_b = self.inp("ln_b", [DEPTH, 2, D])
        self.w_router = self.inp("w_router", [DEPTH, D, NE])
        self.b_router = self.inp("b_router", [DEPTH, NE])
        ned = self.cfg.get("n_exp_decl", NE)
        self.w_up = self.inp("w_up", [DEPTH, ned, D, 2 * D])
        self.b_upT = self.inp("b_upT", [128, DEPTH, NE, 16])
        self.w_down = self.inp("w_down", [DEPTH, ned, D, D])
        self.b_down = self.inp("b_down", [DEPTH, NE, D])
        self.out = self.outp("out", [S, D])

        self.X = P.sbuf("X", [128, NT, D], F32)
        self.HT = P.sbuf("HT", [128, KC, S], BF16)
        self.ident = P.sbuf("ident32", [128, 128], F32)
        self.identb = P.sbuf("identb", [128, 128], BF16)
        self.cT = P.sbuf("cTs", [128, KC], F32)
        self.cB = P.sbuf("cB", [128, KC, 128], BF16)
        self.eps = P.sbuf("epsc", [128, 1], F32)
        self.ps = [P.psum(f"ps{i}", [128, 512], F32) for i in range(8)]
        self.G = P.sbuf("Grow", [128, D], F32)
        self.sc1p = P.sbuf("sc1p", [128, KC], F32)
        self.shp = P.sbuf("shp", [128, KC], F32)
        self.lng = P.sbuf("lng", [128, D], F32)
        self.lnb = P.sbuf("lnb", [128, D], F32)
        self.comb = P.sbuf("comb", [128, NT, NE], F32)
        self.bup = P.sbuf("bup", [128, NE, 16], F32)
        self.lnst = P.sbuf("lnst", [128, 2, 6], F32)
        self.lnmv = P.sbuf("lnmv", [128, 2], F32)
        self.lnr = P.sbuf("lnr", [128, 1], F32)
        self.arena_init(self.cfg.get("arena_bytes", 94208))

    def emit_consts(self):
        P = self.P
        P.dma(lambda e: e.dma_start(out=self.ident[:], in_=self.ident_in[:, :]),
              writes=["ident"], chan="c0")
        P.dma(lambda e: e.dma_start(out=self.cT[:], in_=self.cT_in[:, :]),
              writes=["cT"], chan="c0")
        P.dve(lambda e: e.tensor_copy(out=self.identb[:], in_=self.ident[:]),
              reads=["ident"], writes=["identb"])
        P.dve(lambda e: e.memset(self.eps[:], LN_EPS), writes=["eps"])
        P.act(lambda e: e.activation(out=self.cT[:], in_=self.cT[:], func=AF.Silu),
              reads=["cT"], writes=["cT"])
        P.dve(lambda e: e.tensor_copy(out=self.cB[:], in_=self.cT[:].unsqueeze(2).to_broadcast([128, KC, 128])),
              reads=["cT"], writes=["cB"])

    def load_x(self, src=None):
        P = self.P
        src = self.x_in if src is None else src
        v = src.rearrange("(tt p) d -> p tt d", p=128)
        for q in range(4):
            P.dma(lambda e, q=q: e.dma_start(out=self.X[:, 4 * q:4 * q + 4, :], in_=v[:, 4 * q:4 * q + 4, :]),
                  writes=[("X", t) for t in range(4 * q, 4 * q + 4)], chan="xio")

    def store_x(self, dst=None):
        P = self.P
        dst = self.out if dst is None else dst
        v = dst.rearrange("(tt p) d -> p tt d", p=128)
        for q in range(4):
            P.dma(lambda e, q=q: e.dma_start(out=v[:, 4 * q:4 * q + 4, :], in_=self.X[:, 4 * q:4 * q + 4, :]),
                  reads=[("X", t) for t in range(4 * q, 4 * q + 4)], writes=[("out", q)], chan="xout")
        P.op("sp", lambda e: e.nop(), reads=[("out", q) for q in range(4)])

    def emit_mod(self, l, j):
        P = self.P
        ps = self.ps
        modw = [self.carve([128, KC, 512], BF16) for _ in range(2)]
        adab = [self.carve([128, 512], F32) for _ in range(2)]
        mtmp = self.carve([128, 512], F32)
        mtmp2 = self.carve([128, 512], F32)
        P.dma(lambda e: e.dma_start(out=self.lng[:], in_=self.ln_g[l, j].partition_broadcast(128)),
              writes=["lng"], chan="c0")
        P.dma(lambda e: e.dma_start(out=self.lnb[:], in_=self.ln_b[l, j].partition_broadcast(128)),
              writes=["lnb"], chan="c0")
        wv = self.ada_w[l, j].rearrange("(kc p) e -> p kc e", p=128)
        idb = self.ident[:].unsqueeze(1).to_broadcast([128, 4, 128])
        for pc in range(6):
            s = pc % 2
            P.dma(lambda e, pc=pc, s=s: e.dma_start(out=adab[s][:], in_=self.ada_b[l, j, pc * 512:(pc + 1) * 512].partition_broadcast(128)),
                  writes=[("adab", s)], chan=f"adab{s}")
            P.dma(lambda e, pc=pc, s=s: e.dma_start(out=modw[s][:], in_=wv[:, :, pc * 512:(pc + 1) * 512]),
                  writes=[("modw", s)], chan=f"modw{s}", eng="pool")
            bank = 6 + (pc % 2)
            for kc in range(KC):
                P.pe(lambda e, kc=kc, s=s, bank=bank: e.matmul(ps[bank][:], lhsT=self.cB[:, kc, :], rhs=modw[s][:, kc, :],
                                                               start=(kc == 0), stop=(kc == KC - 1)),
                     reads=["cB", ("modw", s)], writes=[("ps", bank)])
            if pc >= 4:
                P.dve(lambda e, pc=pc, bank=bank, s=s: e.scalar_tensor_tensor(out=self.G[:, (pc - 4) * 512:(pc - 3) * 512], in0=ps[bank][:], scalar=1.0,
                                                                              in1=adab[s][:], op0=ALU.add, op1=ALU.add),
                      reads=[("ps", bank), ("adab", s)], writes=["G"])
            else:
                dst, name = (self.shp, "shp") if pc < 2 else (self.sc1p, "sc1p")
                c0 = (pc % 2) * 4
                addc = 0.0 if pc < 2 else 1.0
                P.dve(lambda e, bank=bank, s=s, addc=addc: e.scalar_tensor_tensor(out=mtmp[:], in0=ps[bank][:], scalar=addc,
                                                                                   in1=adab[s][:], op0=ALU.add, op1=ALU.add),
                      reads=[("ps", bank), ("adab", s)], writes=["mtmp"])
                P.dve(lambda e: e.tensor_tensor(out=mtmp2[:].rearrange("p (k q) -> p k q", q=128),
                                                in0=mtmp[:].rearrange("p (k q) -> p k q", q=128), in1=idb, op=ALU.mult),
                      reads=["mtmp", "ident"], writes=["mtmp2"])
                P.dve(lambda e, dst=dst, c0=c0: e.tensor_reduce(out=dst[:, c0:c0 + 4], in_=mtmp2[:].rearrange("p (k q) -> p k q", q=128),
                                                                axis=AX.X, op=ALU.add),
                      reads=["mtmp2"], writes=[name])

    def emit_hT(self, l, router):
        P = self.P
        ps = self.ps
        if router:
            wr = self.wr[:, :, 0:128]
            P.dve(lambda e: e.memset(wr, 0.0), writes=[("w1l", 1)])
            P.dma(lambda e: e.dma_start(out=wr[:, :, 0:NE], in_=self.w_router[l].rearrange("(kc p) e -> p kc e", p=128)),
                  writes=[("w1l", 1)], chan="c0")
            P.dve(lambda e: e.tensor_copy(out=self.wrb[:], in_=wr), reads=[("w1l", 1)], writes=["wrb"])
            P.dve(lambda e: e.tensor_tensor(out=self.wrl[:], in0=wr, in1=self.wrb[:], op=ALU.subtract), reads=[("w1l", 1), "wrb"], writes=["wrl"])
            P.dma(lambda e: e.dma_start(out=self.brow[:], in_=self.b_router[l].partition_broadcast(128)),
                  writes=["brow"], chan="c0")
        for tg in range(4):
            for kc in range(KC):
                bank = kc % 2
                for i in range(4):
                    tt = tg * 4 + i
                    P.pe(lambda e, tt=tt, kc=kc, i=i, bank=bank: e.transpose(out=ps[bank][:, i * 128:(i + 1) * 128],
                                                                           in_=self.X[:, tt, kc * 128:(kc + 1) * 128],
                                                                           identity=self.ident[:]),
                         reads=[("X", tt), "ident"], writes=[("ps", bank)])
                if not router:
                    P.act(lambda e, kc=kc, tg=tg, bank=bank: e.activation(out=self.HT[:, kc, tg * 512:(tg + 1) * 512], in_=ps[bank][:],
                                                                          func=AF.Identity, bias=self.shp[:, kc:kc + 1],
                                                                          scale=self.sc1p[:, kc:kc + 1]),
                          reads=[("ps", bank), "sc1p", "shp"], writes=[("HT", kc, tg)])
                else:
                    hs = kc % 2
                    h32 = self.eA[hs]
                    P.dve(lambda e, kc=kc, bank=bank, h32=h32: e.tensor_scalar(out=h32[:], in0=ps[bank][:], scalar1=self.sc1p[:, kc:kc + 1], scalar2=self.shp[:, kc:kc + 1],
                                                                               op0=ALU.mult, op1=ALU.add),
                          reads=[("ps", bank), "sc1p", "shp"], writes=[("eA", hs)])
                    P.act(lambda e, kc=kc, tg=tg, h32=h32: e.activation(out=self.HT[:, kc, tg * 512:(tg + 1) * 512], in_=h32[:], func=AF.Identity),
                          reads=[("eA", hs)], writes=[("HT", kc, tg)])
                    P.pool(lambda e, kc=kc, tg=tg, h32=h32: e.tensor_tensor(out=self.hlo[:, kc, :], in0=h32[:], in1=self.HT[:, kc, tg * 512:(tg + 1) * 512], op=ALU.subtract),
                           reads=[("eA", hs), ("HT", kc, tg)], writes=[("w1g", 1)])
            if router:
                for kc in range(KC):
                    trip = ((self.wrb, self.HT[:, kc, tg * 512:(tg + 1) * 512]), (self.wrb, self.hlo[:, kc, :]), (self.wrl, self.HT[:, kc, tg * 512:(tg + 1) * 512]))
                    for ti, (wmat, rhs_) in enumerate(trip):
                        P.pe(lambda e, kc=kc, ti=ti, wmat=wmat, rhs_=rhs_: e.matmul(ps[2][:], lhsT=wmat[:, kc, :], rhs=rhs_,
                                                                                   start=(kc == 0 and ti == 0), stop=(kc == KC - 1 and ti == 2)),
                             reads=[("HT", kc, tg), ("w1g", 1), "wrb", "wrl"], writes=[("ps", 2)])
            rsub = self.cfg.get("rsub", 9)
            if router and rsub >= 2:
                P.act(lambda e: e.activation(out=self.lgT[:], in_=ps[2][0:32, :], func=AF.Identity),
                      reads=[("ps", 2)], writes=["lgT"])
                for i in range(4):
                    tt = tg * 4 + i
                    P.pe(lambda e, i=i: e.transpose(out=ps[3][:, i * 32:(i + 1) * 32], in_=self.lgT[:, i * 128:(i + 1) * 128],
                                                    identity=self.ident[0:32, 0:32]),
                         reads=["lgT", "ident"], writes=[("ps", 3)])
                lg = self.lg
                P.dve(lambda e: e.tensor_tensor(out=lg[:], in0=ps[3][:, 0:128].rearrange("p (i e) -> p i e", e=32),
                                                in1=self.brow[:].unsqueeze(1).to_broadcast([128, 4, NE]), op=ALU.add),
                      reads=[("ps", 3), "brow"], writes=["lg"])
                for i in range(4 if rsub >= 3 else 0):
                    tt = tg * 4 + i
                    t8, ng, ex, sm = self.top8, self.negm, self.ex, self.ssum
                    P.dve(lambda e, i=i: e.max(out=t8[:], in_=lg[:, i, :]), reads=["lg"], writes=["t8"])
                    P.dve(lambda e: e.tensor_scalar(out=ng[:, 0:1], in0=t8[:, 0:1], scalar1=-1.0, scalar2=None, op0=ALU.mult),
                          reads=["t8"], writes=["ng"])
                    P.act(lambda e, i=i: e.activation(out=ex[:], in_=lg[:, i, :], func=AF.Exp, bias=ng[:, 0:1], scale=1.0),
                          reads=["lg", "ng"], writes=["ex"])
                    P.dve(lambda e, i=i: e.tensor_scalar(out=self.msk[:], in0=lg[:, i, :], scalar1=t8[:, 3:4], scalar2=None, op0=ALU.is_ge),
                          reads=["lg", "t8"], writes=["msk"])
                    P.dve(lambda e: e.tensor_tensor(out=ex[:], in0=ex[:], in1=self.msk[:], op=ALU.mult),
                          reads=["ex", "msk"], writes=["ex"])
                    P.dve(lambda e: e.tensor_reduce(out=sm[:, 0:1], in_=ex[:], axis=AX.X, op=ALU.add),
                          reads=["ex"], writes=["sm"])
                    P.dve(lambda e: e.reciprocal(out=sm[:, 0:1], in_=sm[:, 0:1]), reads=["sm"], writes=["sm"])
                    P.dve(lambda e, tt=tt: e.tensor_scalar(out=self.comb[:, tt, :], in0=ex[:], scalar1=sm[:, 0:1], scalar2=None, op0=ALU.mult),
                          reads=["ex", "sm"], writes=[("comb", tt)])

    def declare_moe(self):
        self.wrb = self.carve([128, KC, 128], BF16)
        self.wrl = self.carve([128, KC, 128], BF16)
        self.brow = self.carve([128, NE], F32)
        self.lgT = self.carve([32, 512], F32)
        self.lg = self.carve([128, 4, NE], F32)
        self.top8 = self.carve([128, 8], F32)
        self.negm = self.carve([128, 8], F32)
        self.ex = self.carve([128, NE], F32)
        self.msk = self.carve([128, NE], F32)
        self.ssum = self.carve([128, 8], F32)
        self.combT = self.carve([32, NT, 128], BF16)
        NW = 2
        self.NW = NW
        self.w1g = [self.carve([128, KC, 512], BF16) for i in range(NW)]
        self.w1l = [self.carve([128, KC, 512], BF16) for i in range(NW)]
        self.w2h = [self.carve([128, 4, D], BF16) for i in range(NW)]
        self.bdn = self.carve([32, D], F32)
        self.bdnG = self.carve([32, D], BF16)
        self.NA = 2
        self.actb = [self.carve([128, 4, 512], BF16) for i in range(self.NA)]
        self.NEp = 2
        self.eA = [self.carve([128, 512], F32) for i in range(self.NEp)]
        self.eB = [self.carve([128, 512], F32) for i in range(self.NEp)]
        self.eS = [self.carve([128, 512], F32) for i in range(self.NEp)]
        self.hlo = self.w1g[1]
        self.wr = self.w1l[1].bitcast(F32)
        self.h32 = self.eA[0]

    def emit_bup(self, l):
        P = self.P
        P.dma(lambda e: e.dma_start(out=self.bup[:], in_=self.b_upT[:, l, :, :]), writes=["bup"], chan="c0")
        P.dve(lambda e: e.tensor_scalar(out=self.bup[:, :, 8:16], in0=self.bup[:, :, 8:16], scalar1=1.0, scalar2=None, op0=ALU.add),
              reads=["bup"], writes=["bup"])

    def emit_ln(self, tt, src_keys=()):
        P = self.P
        X = self.X
        k = ("X", tt)
        for h in range(2):
            P.dve(lambda e, h=h: e.bn_stats(out=self.lnst[:, h, :], in_=X[:, tt, h * 512:(h + 1) * 512]),
                  reads=[k], writes=[("lnst", h)])
        P.dve(lambda e: e.bn_aggr(out=self.lnmv[:], in_=self.lnst[:].rearrange("p a b -> p (a b)")),
              reads=[("lnst", 0), ("lnst", 1)], writes=["lnmv"])
        P.act(lambda e: e.activation(out=self.lnr[:], in_=self.lnmv[:, 1:2], func=AF.Sqrt, bias=self.eps[:, 0:1], scale=1.0),
              reads=["lnmv", "eps"], writes=["lnr"])
        P.dve(lambda e: e.reciprocal(out=self.lnr[:], in_=self.lnr[:]), reads=["lnr"], writes=["lnr"])
        P.dve(lambda e: e.tensor_scalar(out=X[:, tt, :], in0=X[:, tt, :], scalar1=self.lnmv[:, 0:1], scalar2=self.lnr[:, 0:1],
                                        op0=ALU.subtract, op1=ALU.mult),
              reads=[k, "lnmv", "lnr"], writes=[k])
        P.dve(lambda e: e.tensor_tensor(out=X[:, tt, :], in0=X[:, tt, :], in1=self.lng[:], op=ALU.mult),
              reads=[k, "lng"], writes=[k])
        P.dve(lambda e: e.tensor_tensor(out=X[:, tt, :], in0=X[:, tt, :], in1=self.lnb[:], op=ALU.add),
              reads=[k, "lnb"], writes=[k])

    def emit_moe(self, l, n_exp=NE):
        P = self.P
        ps = self.ps
        X = self.X
        P.dma(lambda e: e.dma_start(out=self.bdn[:], in_=self.b_down[l]), writes=["bdn"], chan="c0")
        P.dve(lambda e: e.tensor_tensor(out=self.bdnG[:], in0=self.bdn[:], in1=self.G[0:32, :], op=ALU.mult),
              reads=["bdn", "G"], writes=["bdnG"])
        for tt in range(NT):
            bank = 2 + (tt % 2)
            P.pe(lambda e, tt=tt, bank=bank: e.transpose(out=ps[bank][0:32, 0:128], in_=self.comb[:, tt, :], identity=self.ident[:]),
                 reads=[("comb", tt), "ident"], writes=[("ps", bank)])
            P.act(lambda e, tt=tt, bank=bank: e.activation(out=self.combT[:, tt, :], in_=ps[bank][0:32, 0:128], func=AF.Identity),
                  reads=[("ps", bank)], writes=[("combT", tt)])
        for tt in range(NT):
            P.act(lambda e, tt=tt: e.activation(out=X[:, tt, :], in_=X[:, tt, :], func=AF.Copy, scale=float(DN_ALPHA)),
                  reads=[("X", tt)], writes=[("X", tt)])

        units = [(e_, hf) for e_ in range(n_exp) for hf in range(2)]
        NW = self.NW

        def load_unit(u):
            e_, hf = units[u]
            s = u % NW
            wu = self.w_up[l, e_].rearrange("(kc p) f -> p kc f", p=128)
            wd = self.w_down[l, e_].rearrange("(j p) d -> p j d", p=128)
            P.dma(lambda e: e.dma_start(out=self.w1g[s][:], in_=wu[:, :, hf * 512:(hf + 1) * 512]),
                  writes=[("w1g", s)], chan=f"wg{s}", eng="pool")
            P.dma(lambda e: e.dma_start(out=self.w1l[s][:], in_=wu[:, :, D + hf * 512:D + (hf + 1) * 512]),
                  writes=[("w1l", s)], chan=f"wl{s}", eng="pool")
            P.dma(lambda e: e.dma_start(out=self.w2h[s][:], in_=wd[:, hf * 4:(hf + 1) * 4, :]),
                  writes=[("w2h", s)], chan=f"wd{s}", eng="pool")

        def fold_unit(u):
            s = u % NW
            for j in range(4):
                P.pool(lambda e, j=j: e.tensor_tensor(out=self.w2h[s][:, j, :], in0=self.w2h[s][:, j, :], in1=self.G[:], op=ALU.mult),
                       reads=[("w2h", s), "G"], writes=[("w2h", s)])

        epi_ctr = [0]
        zb_ctr = [0]

        def step1(u, tg, aslot):
            e_, hf = units[u]
            s = u % NW
            for j in range(4):
                zb = zb_ctr[0] % 2
                zb_ctr[0] += 1
                bg, bl = zb * 2, zb * 2 + 1
                for kc in range(KC):
                    P.pe(lambda e, kc=kc, j=j, bg=bg: e.matmul(ps[bg][:], lhsT=self.w1g[s][:, kc, j * 128:(j + 1) * 128],
                                                               rhs=self.HT[:, kc, tg * 512:(tg + 1) * 512],
                                                               start=(kc == 0), stop=(kc == KC - 1)),
                         reads=[("w1g", s), ("HT", kc, tg)], writes=[("ps", bg)])
                for kc in range(KC):
                    P.pe(lambda e, kc=kc, j=j, bl=bl: e.matmul(ps[bl][:], lhsT=self.w1l[s][:, kc, j * 128:(j + 1) * 128],
                                                               rhs=self.HT[:, kc, tg * 512:(tg + 1) * 512],
                                                               start=(kc == 0), stop=(kc == KC - 1)),
                         reads=[("w1l", s), ("HT", kc, tg)], writes=[("ps", bl)])
                es = epi_ctr[0] % self.NEp
                epi_ctr[0] += 1
                A, B, Sg = self.eA[es], self.eB[es], self.eS[es]
                fg = hf * 4 + j
                P.dve(lambda e, A=A, bg=bg, fg=fg: e.tensor_scalar(out=A[:], in0=ps[bg][:], scalar1=self.bup[:, e_, fg:fg + 1], scalar2=7.0,
                                                                   op0=ALU.add, op1=ALU.min),
                      reads=[("ps", bg), "bup"], writes=[("eA", es)])
                P.dve(lambda e, B=B, bl=bl, fg=fg: e.tensor_scalar(out=B[:], in0=ps[bl][:], scalar1=self.bup[:, e_, 8 + fg:9 + fg], scalar2=8.0,
                                                                   op0=ALU.add, op1=ALU.min),
                      reads=[("ps", bl), "bup"], writes=[("eB", es)])
                P.act(lambda e, A=A, Sg=Sg: e.activation(out=Sg[:], in_=A[:], func=AF.Sigmoid, scale=1.702),
                      reads=[("eA", es)], writes=[("eS", es)])
                P.pool(lambda e, A=A, Sg=Sg: e.tensor_tensor(out=Sg[:], in0=A[:], in1=Sg[:], op=ALU.mult),
                       reads=[("eA", es), ("eS", es)], writes=[("eS", es)])
                P.dve(lambda e, B=B, Sg=Sg, j=j: e.scalar_tensor_tensor(out=self.actb[aslot][:, j, :], in0=B[:], scalar=-6.0, in1=Sg[:],
                                                                       op0=ALU.max, op1=ALU.mult),
                      reads=[("eB", es), ("eS", es)], writes=[("actb", aslot, j)])

        ob_ctr = [0]

        def step2(u, tg, aslot):
            e_, hf = units[u]
            s = u % NW
            for i in range(4):
                tt = tg * 4 + i
                for dh in range(2):
                    ob = 4 + (ob_ctr[0] % 4)
                    ob_ctr[0] += 1
                    for j in range(4):
                        P.pe(lambda e, j=j, i=i, dh=dh, ob=ob: e.matmul(ps[ob][:], lhsT=self.actb[aslot][:, j, i * 128:(i + 1) * 128],
                                                                        rhs=self.w2h[s][:, j, dh * 512:(dh + 1) * 512],
                                                                        start=(j == 0), stop=(j == 3)),
                             reads=[("actb", aslot, j), ("w2h", s)], writes=[("ps", ob)])
                    P.dve(lambda e, tt=tt, dh=dh, ob=ob: e.scalar_tensor_tensor(out=X[:, tt, dh * 512:(dh + 1) * 512], in0=ps[ob][:],
                                                                                scalar=self.comb[:, tt, e_:e_ + 1],
                                                                                in1=X[:, tt, dh * 512:(dh + 1) * 512],
                                                                                op0=ALU.mult, op1=ALU.add),
                          reads=[("ps", ob), ("comb", tt), ("X", tt)], writes=[("X", tt)])

        work = [(u, tg) for u in range(len(units)) for tg in range(4)]
        load_unit(0)
        if len(units) > 1:
            load_unit(1)
        prev = None
        for wi, (u, tg) in enumerate(work):
            aslot = wi % self.NA
            if tg == 0:
                fold_unit(u)
            step1(u, tg, aslot)
            if prev is not None:
                pu, ptg, pas = prev
                step2(pu, ptg, pas)
                if ptg == 3 and pu + NW < len(units):
                    load_unit(pu + NW)
            prev = (u, tg, aslot)
        pu, ptg, pas = prev
        step2(pu, ptg, pas)
        for tt in range(NT):
            for dh in range(2):
                ob = 4 + (ob_ctr[0] % 4)
                ob_ctr[0] += 1
                P.pe(lambda e, tt=tt, dh=dh, ob=ob: e.matmul(ps[ob][:], lhsT=self.combT[:, tt, :], rhs=self.bdnG[:, dh * 512:(dh + 1) * 512],
                                                             start=True, stop=True),
                     reads=[("combT", tt), "bdnG"], writes=[("ps", ob)])
                P.dve(lambda e, tt=tt, dh=dh, ob=ob: e.tensor_tensor(out=X[:, tt, dh * 512:(dh + 1) * 512], in0=ps[ob][:],
                                                                     in1=X[:, tt, dh * 512:(dh + 1) * 512], op=ALU.add),
                      reads=[("ps", ob), ("X", tt)], writes=[("X", tt)])
            self.emit_ln(tt)


def build(cfg):
    B = Builder(cfg)
    B.declare_common()
    mixer_on = cfg.get("mixer", True)
    if mixer_on:
        B.declare_mixer()
        if cfg.get("s5", True):
            B.declare_s5()
    B.emit_consts()
    if mixer_on:
        B.arena_reset()
        B.emit_mixer_consts()
    B.load_x()
    for l in cfg.get("layers", range(DEPTH)):
        if mixer_on:
            B.arena_reset()
            B.emit_mod(l, 0)
            B.arena_reset()
            B.emit_hT(l, router=False)
            B.emit_mixer(l)
        if cfg.get("moe", True):
            st = cfg.get("stage", 99)
            B.arena_reset()
            if st >= 1:
                B.emit_mod(l, 1)
            B.arena_reset()
            B.declare_moe()
            B.emit_bup(l)
            if st >= 2:
                B.emit_hT(l, router=cfg.get("router", True))
            if st >= 3:
                B.emit_moe(l, n_exp=cfg.get("n_exp", NE))
    B.store_x()
    B.P.emit()
    return B


def host_inputs(inputs, b):
    f = lambda a: np.ascontiguousarray(np.asarray(a))
    m = {}
    m["x"] = f(inputs["x"][b])
    m["cT"] = f(np.asarray(inputs["c"][b]).reshape(KC, 128).T)
    m["ident"] = np.eye(128, dtype=np.float32)
    for k in ("ada_w", "ada_b", "ln_g", "ln_b", "w_router", "b_router", "w_up", "w_down", "b_down"):
        m[k] = f(inputs[k])
    m["b_upT"] = f(np.asarray(inputs["b_up"]).reshape(DEPTH, NE, 16, 128).transpose(3, 0, 1, 2))
    w_in = np.asarray(inputs["w_in"])
    m["pos"] = f(np.asarray(inputs["positions"][b]).astype(np.int32))
    p = np.arange(128)
    d = p % 64
    inv = (10000.0 ** (-(2.0 * (d % 32)) / 64.0)).astype(np.float32)
    sgn = np.where(d < 32, -1.0, 1.0).astype(np.float32)
    m["ropec"] = f(np.stack([inv, sgn], axis=1))
    perm = np.arange(512).reshape(8, 64)
    perm = np.concatenate([perm[:, 32:], perm[:, :32]], axis=1).reshape(512)
    q = w_in[:, :, 0:512]; k = w_in[:, :, 512:1024]
    def chunked(w, width):
        L_, D_, n_ = w.shape
        return f(w.reshape(L_, KC, 128, n_ // width, width).transpose(0, 3, 2, 1, 4))
    qs = q[:, :, perm]; ks = k[:, :, perm]
    qk = np.stack([np.concatenate([q.reshape(DEPTH, D, 4, 128), qs.reshape(DEPTH, D, 4, 128)], axis=3),
                   np.concatenate([k.reshape(DEPTH, D, 4, 128), ks.reshape(DEPTH, D, 4, 128)], axis=3)], axis=2)
    m["w_qk"] = chunked(qk.reshape(DEPTH, D, 2048), 256)
    m["w_v"] = chunked(w_in[:, :, 1024:1536], 256)
    m["w_mlx"] = chunked(w_in[:, :, 1536:1792], 128)
    m["w_mlvo"] = chunked(w_in[:, :, 1792:2304], 256)
    m["w_mli"] = f(w_in[:, :, 2304:2308])
    m["w_mlf"] = f(w_in[:, :, 2308:2312])
    m["tri"] = f(np.triu(np.ones((128, 128), np.float32)))
    m["lamv"] = f(np.concatenate([np.asarray(inputs[k_]) for k_ in ("lam_q1", "lam_k1", "lam_q2", "lam_k2")], axis=1))
    m["da_g"] = f(inputs["da_norm_g"])
    cw = np.asarray(inputs["ml_conv_w"])
    m["convw"] = f(cw.reshape(DEPTH, 4, 2, 128).transpose(3, 0, 2, 1))
    m["convb"] = f(np.asarray(inputs["ml_conv_b"]).reshape(DEPTH, 2, 128).transpose(2, 0, 1))
    for nm, src in (("wq_bd", "ml_w_q"), ("wk_bd", "ml_w_k")):
        w = np.asarray(inputs[src])
        bd = np.zeros((DEPTH, 2, 128, 128), np.float32)
        for hc in range(2):
            for hl in range(2):
                bd[:, hc, hl * 64:(hl + 1) * 64, hl * 64:(hl + 1) * 64] = w[:, hc * 2 + hl]
        m[nm] = bd
    gbv = np.asarray(inputs["ml_gate_b"])
    m["gate_bi"] = f(gbv[:, 0:4].reshape(DEPTH, 4, 1))
    m["gate_bf"] = f(gbv[:, 4:8].reshape(DEPTH, 4, 1))
    sel = np.zeros((4, 4, 128), np.float32)
    for h in range(4):
        sel[h, h, :] = 1.0
    m["sel4"] = sel
    m["ml_g"] = f(inputs["ml_norm_g"])
    m["w_out"] = f(inputs["w_out"])
    a_re = np.asarray(inputs["s5_a_re"]); a_im = np.asarray(inputs["s5_a_im"]); ldt = np.asarray(inputs["s5_log_dt"])
    G_, P_, H_ = 16, 64, 16
    ldt_b = np.broadcast_to(ldt[:, :, None], (DEPTH, G_, P_))
    A = np.stack([a_re, a_im, ldt_b], axis=-1)
    m["s5A"] = f(A.reshape(DEPTH, 8, 2, P_, 3).transpose(2, 3, 0, 1, 4).reshape(128, DEPTH, 8, 3))
    A2 = np.broadcast_to(A[:, :, None, :, :], (DEPTH, G_, H_, P_, 3))
    m["s5A2"] = f(A2.reshape(DEPTH, 2, 8, H_, P_, 3).transpose(2, 3, 0, 1, 4, 5).reshape(128, DEPTH, 2, P_, 3))
    bb = np.stack([np.asarray(inputs["s5_b_re"]), np.asarray(inputs["s5_b_im"])], axis=1)
    m["s5bT"] = f(bb.reshape(DEPTH, 2, 2, 8, P_, H_).transpose(3, 5, 0, 1, 2, 4).reshape(128, DEPTH, 2, 2, P_))
    cc = np.stack([np.asarray(inputs["s5_c_re"]), np.asarray(inputs["s5_c_im"])], axis=1)
    m["s5cT"] = f(cc.reshape(DEPTH, 2, 8, 2, H_, P_).transpose(3, 5, 0, 1, 2, 4).reshape(128, DEPTH, 2, 8, H_))
    row = np.arange(128)
    bmask = np.zeros((128, 8), np.float32)
    for scl in range(4):
        for s2 in range(2):
            bmask[:, scl * 2 + s2] = ((row // 16) == 2 * scl + s2)
    m["s5bm"] = bmask
    cmask = np.zeros((128, 4, 8), np.float32)
    for scl in range(4):
        for gl in range(8):
            cmask[:, scl, gl] = (gl == 2 * scl + (row // 64))
    m["s5cm"] = cmask
    m["s5d"] = f(np.asarray(inputs["s5_d"]).reshape(DEPTH, 2, 128).transpose(2, 0, 1))
    wgl = np.asarray(inputs["s5_w_glu"])
    wbd = np.zeros((DEPTH, 2, 2, 128, 128), np.float32)
    for yc in range(2):
        for gl in range(8):
            g = yc * 8 + gl
            for hf in range(2):
                wbd[:, yc, hf, gl * 16:(gl + 1) * 16, gl * 16:(gl + 1) * 16] = wgl[:, g, :, hf * 16:(hf + 1) * 16]
    m["s5wg"] = wbd
    m["w_s5u"] = chunked(w_in[:, :, 2312:2568], 128)
    m["jidx"] = f(np.broadcast_to(np.arange(512, dtype=np.float32)[None, :], (128, 512)))
    return m


_CACHE = {}


def kernel(**inputs):
    cfg = {}
    if "B" not in _CACHE:
        _CACHE["B"] = build(cfg)
    B = _CACHE["B"]
    in_maps = []
    for b in range(8):
        m = host_inputs(inputs, b)
        in_maps.append({k: m[k] for k in B.din})
    res = run_bass_kernel_spmd(B.nc, in_maps, core_ids=list(range(8)))
    out = np.stack([res.results[b]["out"] for b in range(8)], axis=0)
    return out.astype(np.float32)


def _declare_mixer(self):
    self.pos_in = self.inp("pos", [S], I32)
    self.ropec_in = self.inp("ropec", [128, 2])
    self.w_qk = self.inp("w_qk", [DEPTH, 8, 128, KC, 256])
    self.w_v = self.inp("w_v", [DEPTH, 2, 128, KC, 256])
    self.w_mlx = self.inp("w_mlx", [DEPTH, 2, 128, KC, 128])
    self.w_mlvo = self.inp("w_mlvo", [DEPTH, 2, 128, KC, 256])
    self.w_mli = self.inp("w_mli", [DEPTH, D, 4])
    self.w_mlf = self.inp("w_mlf", [DEPTH, D, 4])
    self.tri_in = self.inp("tri", [128, 128])
    self.lamv_in = self.inp("lamv", [DEPTH, 256])
    self.da_g_in = self.inp("da_g", [DEPTH, 128])
    self.convw_in = self.inp("convw", [128, DEPTH, 2, 4])
    self.convb_in = self.inp("convb", [128, DEPTH, 2])
    self.wq_bd = self.inp("wq_bd", [DEPTH, 2, 128, 128])
    self.wk_bd = self.inp("wk_bd", [DEPTH, 2, 128, 128])
    self.gbi_in = self.inp("gate_bi", [DEPTH, 4, 1])
    self.gbf_in = self.inp("gate_bf", [DEPTH, 4, 1])
    self.sel4_in = self.inp("sel4", [4, 4, 128])
    self.ml_g_in = self.inp("ml_g", [DEPTH, 256])
    self.w_out = self.inp("w_out", [DEPTH, D, D])
    P = self.P
    self.trib = P.sbuf("trib", [128, 128], BF16)
    self.ropec = P.sbuf("ropec_s", [128, 2], F32)
    self.one1 = P.sbuf("one1", [128, 1], F32)


def _emit_mixer_consts(self):
    P = self.P
    tmp = self.carve([128, 128], F32)
    P.dma(lambda e: e.dma_start(out=tmp[:], in_=self.tri_in[:, :]), writes=["tritmp"], chan="c0")
    P.dve(lambda e: e.tensor_copy(out=self.trib[:], in_=tmp[:]), reads=["tritmp"], writes=["trib"])
    P.dma(lambda e: e.dma_start(out=self.ropec[:], in_=self.ropec_in[:, :]), writes=["ropec"], chan="c0")
    P.dve(lambda e: e.memset(self.one1[:], 1.0), writes=["one1"])


def _proj_fm(self, wsrc, ncol, consume, tag):
    P, ps = self.P, self.ps
    nchunk = ncol // 128
    for c in range(nchunk):
        s = c % 2
        wt = self.wst[s]
        P.dma(lambda e, c=c, wt=wt: e.dma_start(out=wt[:, :, 0:128], in_=wsrc[c]),
              writes=[("wst", s)], chan=f"wst{s}", eng="pool")
        for tg in range(4):
            bank = (c * 4 + tg) % 2
            for kc in range(KC):
                P.pe(lambda e, kc=kc, tg=tg, bank=bank, wt=wt: e.matmul(ps[bank][:], lhsT=wt[:, kc, 0:128], rhs=self.HT[:, kc, tg * 512:(tg + 1) * 512],
                                                                      start=(kc == 0), stop=(kc == KC - 1)),
                     reads=[("wst", s), ("HT", kc, tg)], writes=[("ps", bank)])
            consume(c, tg, bank)


def _proj_tm(self, wsrc, ncol, consume):
    P, ps = self.P, self.ps
    wt = self.wst[0]
    P.dma(lambda e: e.dma_start(out=wt[:, :, 0:ncol], in_=wsrc), writes=[("wst", 0)], chan="wst0", eng="pool")
    for tt in range(NT):
        bank = 2 + (tt % 2)
        for kc in range(KC):
            P.pe(lambda e, kc=kc, tt=tt, bank=bank: e.matmul(ps[bank][:, 0:ncol], lhsT=self.HT[:, kc, tt * 128:(tt + 1) * 128], rhs=wt[:, kc, 0:ncol],
                                                           start=(kc == 0), stop=(kc == KC - 1)),
                 reads=[("wst", 0)] + [("HT", kc, tt // 4)], writes=[("ps", bank)])
        consume(tt, bank)


def _blocks_flush(self, n_keep=0):
    pend = self.__dict__.setdefault("_bpend", [])
    while len(pend) > n_keep:
        fn = pend.pop(0)
        fn()


def _blocks(self, name, qt, kT, qT, vaug, vw, out_cb, scale=1.0, dmask=None):
    P, ps = self.P, self.ps
    pt = self.pt
    BP = 2
    SB = (0, 1, 4)
    OB = (5, 6, 7)
    cnt = self.__dict__.setdefault("_bcnt", [0, 0])
    pend = self.__dict__.setdefault("_bpend", [])
    ob = OB[cnt[1] % 3]
    cnt[1] += 1
    nk = qt + 1
    for g0 in range(0, nk, 4):
        n = min(4, nk - g0)
        sb = SB[cnt[0] % 3]
        sl = cnt[0] % 3
        cnt[0] += 1
        for i in range(n):
            kt = g0 + i
            P.pe(lambda e, i=i, kt=kt, sb=sb: e.matmul(ps[sb][:, i * 128:(i + 1) * 128], lhsT=kT(kt), rhs=qT(qt), start=True, stop=True),
                 reads=[(name, "kq")], writes=[("ps", sb)])
        if dmask is None:
            P.act(lambda e, n=n, sb=sb, sl=sl: e.activation(out=pt[sl][:, 0:n * 128], in_=ps[sb][:, 0:n * 128], func=AF.Exp, scale=scale),
                  reads=[("ps", sb)], writes=[("pt", sl)])
        else:
            dsl = cnt[0] % 2
            for i in range(n):
                fr, bi = dmask(g0 + i)
                P.act(lambda e, i=i, fr=fr, bi=bi, dsl=dsl: e.activation(out=self.dt[dsl][:, i * 128:(i + 1) * 128], in_=fr, func=AF.Exp, bias=bi, scale=1.0),
                      reads=[(name, "dm")], writes=[("dt", dsl)])
            P.dve(lambda e, n=n, sb=sb, sl=sl, dsl=dsl: e.tensor_tensor(out=pt[sl][:, 0:n * 128], in0=ps[sb][:, 0:n * 128], in1=self.dt[dsl][:, 0:n * 128], op=ALU.mult),
                  reads=[("ps", sb), ("dt", dsl)], writes=[("pt", sl)])
        if g0 + n - 1 == qt:
            i = n - 1
            P.pool(lambda e, i=i, sl=sl: e.tensor_tensor(out=pt[sl][:, i * 128:(i + 1) * 128], in0=pt[sl][:, i * 128:(i + 1) * 128], in1=self.trib[:], op=ALU.mult),
                   reads=[("pt", sl), "trib"], writes=[("pt", sl)])

        def pv(n=n, g0=g0, sl=sl, ob=ob, last=(g0 + n - 1 == qt)):
            for i in range(n):
                kt = g0 + i
                P.pe(lambda e, i=i, kt=kt: e.matmul(ps[ob][:, 0:vw], lhsT=pt[sl][:, i * 128:(i + 1) * 128], rhs=vaug(kt),
                                                    start=(kt == 0), stop=(kt == qt)),
                     reads=[("pt", sl), (name, "v")], writes=[("ps", ob)])
            if last:
                out_cb(qt, ob)
        pend.append(pv)
        _blocks_flush(self, BP)


def _emit_mlstm(self, l, R1):
    P, ps = self.P, self.ps
    self.wst = [self.carve([128, KC, 256], BF16), self.carve([128, KC, 128], BF16)]
    self.pt = [self.carve([128, 512], BF16) for _ in range(3)]
    self.dt = [self.carve([128, 512], F32) for _ in range(2)]
    xm = self.carve([128, 2, S + 4], BF16)
    xc = self.carve([128, 2, S], BF16)
    QTm = self.carve([128, S], BF16)
    KTm = self.carve([128, S], BF16)
    vml = self.carve([128, NT, 4, 66], BF16)
    sgo = self.carve([128, NT, 256], BF16)
    FT = self.carve([4, S], F32)
    Frow = self.carve([128, S], F32)
    acc = Frow
    fT = Frow[0:4, :]
    iT = xm[0:4, :, :].rearrange("p a b -> p (a b)").bitcast(F32)[:, 0:S]
    biasS = self.carve([128, NT, 4], F32)
    cw = self.carve([128, 2, 4], F32)
    cb = self.carve([128, 2], F32)
    wqb = self.carve([128, 2, 128], BF16)
    wkb = self.carve([128, 2, 128], BF16)
    wtmp = self.carve([128, 2, 128], F32)
    gb = self.carve([4, 2], F32)
    sel4 = self.carve([4, 4, 128], F32)
    mlg = self.carve([128, 256], F32)
    sm16 = self.carve([128, 2, NT], F32)
    Ost = xm[:, :, :].rearrange("p a b -> p (a b)").bitcast(F32)[:, 0:NT * 65].rearrange("p (q c) -> p q c", c=65)
    htmp = self.wst[0][:, :, :].rearrange("p a b -> p (a b)").bitcast(F32)[:, 0:NT * 64].rearrange("p (q c) -> p q c", c=64)

    P.dma(lambda e: e.dma_start(out=cw[:], in_=self.convw_in[:, l]), writes=["cw"], chan="c0")
    P.dma(lambda e: e.dma_start(out=cb[:], in_=self.convb_in[:, l]), writes=["cb"], chan="c0")
    P.dma(lambda e: e.dma_start(out=gb[:, 0:1], in_=self.gbi_in[l]), writes=["gb"], chan="c0")
    P.dma(lambda e: e.dma_start(out=gb[:, 1:2], in_=self.gbf_in[l]), writes=["gb"], chan="c0")
    P.dma(lambda e: e.dma_start(out=sel4[:], in_=self.sel4_in[:, :, :]), writes=["sel4"], chan="c0")
    P.dma(lambda e: e.dma_start(out=mlg[:], in_=self.ml_g_in[l].partition_broadcast(128)), writes=["mlg"], chan="c0")
    for (src, dst, nm) in ((self.wq_bd, wqb, "wqb"), (self.wk_bd, wkb, "wkb")):
        P.dma(lambda e, src=src: e.dma_start(out=wtmp[:], in_=src[l].rearrange("c p e -> p c e")), writes=["wtmp"], chan="c0")
        P.dve(lambda e, dst=dst: e.tensor_copy(out=dst[:], in_=wtmp[:]), reads=["wtmp"], writes=[nm])
    P.dve(lambda e: e.memset(xm[:, :, 0:4], 0.0), writes=["xm"])
    P.dve(lambda e: e.memset(vml[:], 1.0), writes=[("ml", "v")])

    def cons_x(c, tg, bank):
        P.act(lambda e: e.activation(out=xm[:, c, 4 + tg * 512:4 + (tg + 1) * 512], in_=ps[bank][:], func=AF.Identity),
              reads=[("ps", bank)], writes=["xm"])
    _proj_fm(self, self.w_mlx[l], 256, cons_x, "mlx")
    for hc in range(2):
        P.dve(lambda e, hc=hc: e.tensor_scalar(out=acc[:], in0=xm[:, hc, 1:1 + S], scalar1=cw[:, hc, 0:1], scalar2=None, op0=ALU.mult),
              reads=["xm", "cw"], writes=["acc"])
        for j in range(1, 4):
            P.dve(lambda e, hc=hc, j=j: e.scalar_tensor_tensor(out=acc[:], in0=xm[:, hc, 1 + j:1 + j + S], scalar=cw[:, hc, j:j + 1], in1=acc[:],
                                                               op0=ALU.mult, op1=ALU.add),
                  reads=["xm", "cw", "acc"], writes=["acc"])
        P.act(lambda e, hc=hc: e.activation(out=xc[:, hc, :], in_=acc[:], func=AF.Silu, bias=cb[:, hc:hc + 1], scale=1.0),
              reads=["acc", "cb"], writes=["xc"])

    def cons_v2(tt, bank):
        P.act(lambda e: e.activation(out=vml[:, tt, :, 0:64], in_=ps[bank][:, 0:256].rearrange("p (h d) -> p h d", d=64), func=AF.Identity),
              reads=[("ps", bank)], writes=[("ml", "v")])

    def cons_o2(tt, bank):
        P.act(lambda e: e.activation(out=sgo[:, tt, :], in_=ps[bank][:, 0:256], func=AF.Sigmoid),
              reads=[("ps", bank)], writes=["sgo"])
    _proj_tm(self, self.w_mlvo[l, 0], 256, cons_v2)
    _proj_tm(self, self.w_mlvo[l, 1], 256, cons_o2)
    for (wsrc, dstT, nm, alias) in ((self.w_mli, iT, "iT", "xm"), (self.w_mlf, fT, "fT", "acc")):
        wt = self.wst[1]
        P.dma(lambda e, wsrc=wsrc, wt=wt: e.dma_start(out=wt[:, :, 0:4], in_=wsrc[l].rearrange("(kc p) f -> p kc f", p=128)),
              writes=[("wst", 1)], chan="wst1", eng="pool")
        for tg in range(4):
            bank = tg % 2
            for kc in range(KC):
                P.pe(lambda e, kc=kc, tg=tg, bank=bank, wt=wt: e.matmul(ps[bank][0:4, :], lhsT=wt[:, kc, 0:4], rhs=self.HT[:, kc, tg * 512:(tg + 1) * 512],
                                                                      start=(kc == 0), stop=(kc == KC - 1)),
                     reads=[("wst", 1), ("HT", kc, tg)], writes=[("ps", bank)])
            P.act(lambda e, tg=tg, bank=bank, dstT=dstT: e.activation(out=dstT[:, tg * 512:(tg + 1) * 512], in_=ps[bank][0:4, :], func=AF.Identity),
                  reads=[("ps", bank)], writes=[nm, alias])
    P.dve(lambda e: e.tensor_scalar(out=iT, in0=iT, scalar1=gb[:, 0:1], scalar2=None, op0=ALU.add), reads=["iT", "gb"], writes=["iT"])
    P.dve(lambda e: e.tensor_scalar(out=fT, in0=fT, scalar1=gb[:, 1:2], scalar2=-1.0, op0=ALU.add, op1=ALU.mult), reads=["fT", "gb"], writes=["fT"])
    P.act(lambda e: e.activation(out=fT, in_=fT, func=AF.Exp), reads=["fT"], writes=["fT"])
    P.act(lambda e: e.activation(out=fT, in_=fT, func=AF.Ln, bias=self.one1[0:4, 0:1], scale=1.0), reads=["fT", "one1"], writes=["fT"])
    P.dve(lambda e: e.tensor_scalar(out=fT, in0=fT, scalar1=-1.0, scalar2=None, op0=ALU.mult), reads=["fT"], writes=["fT"])
    ones4 = self.dt[0][0:4, :]
    P.dve(lambda e: e.memset(ones4, 1.0), writes=[("dt", 0)])
    for c4 in range(4):
        init = 0.0 if c4 == 0 else FT[:, c4 * 512 - 1:c4 * 512]
        P.dve(lambda e, c4=c4, init=init: e.tensor_tensor_scan(out=FT[:, c4 * 512:(c4 + 1) * 512], data0=ones4, data1=fT[:, c4 * 512:(c4 + 1) * 512],
                                                               initial=init, op0=ALU.mult, op1=ALU.add),
              reads=["fT", ("dt", 0), "FT"], writes=["FT"])
    P.dve(lambda e: e.tensor_tensor(out=iT, in0=iT, in1=FT[:], op=ALU.subtract), reads=["iT", "FT"], writes=["iT"])
    for tt in range(NT):
        P.pe(lambda e, tt=tt: e.transpose(out=ps[2][:, tt * 4:(tt + 1) * 4], in_=iT[:, tt * 128:(tt + 1) * 128], identity=self.ident[0:4, 0:4]),
             reads=["iT", "ident"], writes=[("ps", 2)])
    P.dve(lambda e: e.tensor_copy(out=biasS[:].rearrange("p a b -> p (a b)"), in_=ps[2][:, 0:64]), reads=[("ps", 2)], writes=["biasS"])

    for g in range(4):
        hc, hl = g // 2, g % 2
        if hl == 0:
            for tg in range(4):
                for (wb, dstT, sc, nm) in ((wqb, QTm, 1.0, "wqb"), (wkb, KTm, 0.125, "wkb")):
                    bank = (tg % 2)
                    P.pe(lambda e, hc=hc, tg=tg, wb=wb, bank=bank: e.matmul(ps[bank][:], lhsT=wb[:, hc, :], rhs=xc[:, hc, tg * 512:(tg + 1) * 512], start=True, stop=True),
                         reads=["xc", nm], writes=[("ps", bank)])
                    P.act(lambda e, tg=tg, dstT=dstT, sc=sc, bank=bank: e.activation(out=dstT[:, tg * 512:(tg + 1) * 512], in_=ps[bank][:], func=AF.Copy, scale=sc),
                          reads=[("ps", bank)], writes=[("ml", "kq")])
        for tg in range(4):
            bank = 2 + tg % 2
            P.pe(lambda e, tg=tg, bank=bank, g=g: e.matmul(ps[bank][:], lhsT=sel4[:, g, :], rhs=FT[:, tg * 512:(tg + 1) * 512], start=True, stop=True),
                 reads=["FT", "sel4"], writes=[("ps", bank)])
            P.act(lambda e, tg=tg, bank=bank: e.activation(out=Frow[:, tg * 512:(tg + 1) * 512], in_=ps[bank][:], func=AF.Identity),
                  reads=[("ps", bank), "fT", "acc"], writes=[("ml", "dm"), "acc", "fT"])

        def out_cb(qt, ob, g=g):
            P.dve(lambda e: e.tensor_copy(out=Ost[:, qt, :], in_=ps[ob][:, 0:65]), reads=[("ps", ob), "xm", "iT"], writes=["Ost", "xm"])

        for qt in range(NT):
            _blocks(self, "ml", qt,
                    kT=lambda kt, hl=hl: KTm[hl * 64:(hl + 1) * 64, kt * 128:(kt + 1) * 128],
                    qT=lambda qt_, hl=hl: QTm[hl * 64:(hl + 1) * 64, qt_ * 128:(qt_ + 1) * 128],
                    vaug=lambda kt, g=g: vml[:, kt, g, 0:65], vw=65, out_cb=out_cb,
                    dmask=lambda kt, qt=qt, g=g: (Frow[:, qt * 128:(qt + 1) * 128], biasS[:, kt, g:g + 1]))
        _blocks_flush(self)
        num = Ost[:, :, 0:64]
        b16 = lambda ap: ap.unsqueeze(2).to_broadcast([128, NT, 64])
        EK = dict(reads=["Ost", "htmp", "sm16", "mlg", "sgo", "eps", ("wst", 0)], writes=["Ost", "htmp", "sm16", ("wst", 0)])
        P.act(lambda e: e.activation(out=sm16[:, 0, :], in_=Ost[:, :, 64], func=AF.Abs), **EK)
        P.dve(lambda e: e.tensor_scalar(out=sm16[:, 0, :], in0=sm16[:, 0, :], scalar1=1.0, scalar2=None, op0=ALU.max), **EK)
        P.dve(lambda e: e.reciprocal(out=sm16[:, 0, :], in_=sm16[:, 0, :]), **EK)
        P.dve(lambda e: e.tensor_tensor(out=htmp[:], in0=num, in1=b16(sm16[:, 0, :]), op=ALU.mult), **EK)
        P.dve(lambda e: e.tensor_tensor(out=num, in0=htmp[:], in1=htmp[:], op=ALU.mult), **EK)
        P.dve(lambda e: e.tensor_reduce(out=sm16[:, 1, :], in_=num, axis=AX.X, op=ALU.add), **EK)
        P.act(lambda e: e.activation(out=sm16[:, 1, :], in_=sm16[:, 1, :], func=AF.Sqrt, bias=self.eps[:, 0:1], scale=1.0 / 64.0), **EK)
        P.dve(lambda e: e.reciprocal(out=sm16[:, 1, :], in_=sm16[:, 1, :]), **EK)
        P.dve(lambda e: e.tensor_tensor(out=htmp[:], in0=htmp[:], in1=b16(sm16[:, 1, :]), op=ALU.mult), **EK)
        P.dve(lambda e, g=g: e.tensor_tensor(out=htmp[:], in0=htmp[:], in1=mlg[:, g * 64:(g + 1) * 64].unsqueeze(1).to_broadcast([128, NT, 64]), op=ALU.mult), **EK)
        P.dve(lambda e, g=g: e.tensor_tensor(out=sgo[:, :, g * 64:(g + 1) * 64], in0=htmp[:], in1=sgo[:, :, g * 64:(g + 1) * 64], op=ALU.mult),
              reads=["htmp", "sgo"], writes=["sgo"])
    for qt in range(NT):
        for hc in range(2):
            tb = 2 + ((qt * 2 + hc) % 2)
            psb = ps[tb][:].bitcast(BF16)
            P.pe(lambda e, qt=qt, hc=hc, psb=psb: e.transpose(out=psb[:, 0:128], in_=sgo[:, qt, hc * 128:(hc + 1) * 128], identity=self.identb[:]),
                 reads=["sgo", "identb"], writes=[("ps", tb)])
            P.act(lambda e, qt=qt, hc=hc, psb=psb: e.activation(out=R1[:, hc, qt * 128:(qt + 1) * 128], in_=psb[:, 0:128], func=AF.Identity),
                  reads=[("ps", tb)], writes=[("R1", hc)])


def _emit_da(self, l, R1):
    P, ps = self.P, self.ps
    lam_init = 0.8 - 0.6 * math.exp(-0.3 * l)
    wst2 = self.carve([128, 2 * KC * 256], BF16)
    self.wst = [wst2[:, i * KC * 256:(i + 1) * KC * 256].rearrange("p (k f) -> p k f", f=256) for i in range(2)]
    odah = wst2.bitcast(F32)[:, 0:NT * 128].rearrange("p (q c) -> p q c", c=128)
    odbh = wst2[:, 0:NT * 128].rearrange("p (q c) -> p q c", c=128)
    self.pt = [self.carve([128, 512], BF16) for _ in range(3)]
    QT = self.carve([128, 4, S], BF16)
    KT = self.carve([128, 4, S], BF16)
    Va = self.carve([128, NT, 4, 130], BF16)
    t1 = self.carve([128, 512], F32)
    sq5 = self.carve([128, 5 * 512], F32)
    cosT, sinT, posf, t2, twopi = [sq5[:, i * 512:(i + 1) * 512] for i in range(5)]
    posi = t2.bitcast(I32)
    sqh = sq5[:, 0:NT * 128].rearrange("p (q c) -> p q c", c=128)
    lamt = self.carve([128, 256], F32)
    lam = self.carve([128, 8], F32)
    dag = self.carve([128, 128], F32)
    sq = self.carve([128, 128], F32)
    sd = self.carve([128, 8], F32)
    sd16 = self.carve([128, NT], F32)
    YA = self.HT
    self.da_t1 = t1

    P.dve(lambda e: e.memset(Va[:], 1.0), writes=[("da", "v")])
    P.dma(lambda e: e.dma_start(out=lamt[:], in_=self.lamv_in[l].partition_broadcast(128)), writes=["lamt"], chan="c0")
    P.dma(lambda e: e.dma_start(out=dag[:], in_=self.da_g_in[l].partition_broadcast(128)), writes=["dag"], chan="c0")
    for i in range(2):
        P.dve(lambda e, i=i: e.tensor_tensor(out=sq[:, 0:64], in0=lamt[:, i * 128:i * 128 + 64], in1=lamt[:, i * 128 + 64:i * 128 + 128], op=ALU.mult),
              reads=["lamt"], writes=["sq"])
        P.dve(lambda e, i=i: e.tensor_reduce(out=lam[:, i:i + 1], in_=sq[:, 0:64], axis=AX.X, op=ALU.add), reads=["sq"], writes=["lam"])
    P.act(lambda e: e.activation(out=lam[:, 0:2], in_=lam[:, 0:2], func=AF.Exp), reads=["lam"], writes=["lam"])
    P.dve(lambda e: e.tensor_tensor(out=lam[:, 2:3], in0=lam[:, 1:2], in1=lam[:, 0:1], op=ALU.subtract), reads=["lam"], writes=["lam"])
    P.dve(lambda e: e.tensor_scalar(out=lam[:, 2:3], in0=lam[:, 2:3], scalar1=-lam_init, scalar2=None, op0=ALU.add), reads=["lam"], writes=["lam"])
    P.dve(lambda e: e.tensor_scalar(out=dag[:], in0=dag[:], scalar1=float(1.0 - lam_init), scalar2=None, op0=ALU.mult), reads=["dag"], writes=["dag"])

    for tg in range(4):
        P.dma(lambda e, tg=tg: e.dma_start(out=posi, in_=self.pos_in[tg * 512:(tg + 1) * 512].partition_broadcast(128)),
              writes=["t2"], chan="c0")
        P.dve(lambda e: e.tensor_copy(out=posf[:], in_=posi), reads=["t2"], writes=["posf"])
        TWO_PI = 2.0 * math.pi
        for (dst, sh_, nm) in ((sinT, 0.0, "sinT"), (cosT, 0.5 * math.pi, "cosT")):
            P.dve(lambda e, sh_=sh_: e.tensor_scalar(out=t1[:], in0=posf[:], scalar1=self.ropec[:, 0:1], scalar2=sh_, op0=ALU.mult, op1=ALU.add),
                  reads=["posf", "ropec"], writes=["t1"])
            P.dve(lambda e: e.tensor_scalar(out=posi, in0=t1[:], scalar1=1.0 / TWO_PI, scalar2=None, op0=ALU.mult), reads=["t1"], writes=["t2"])
            P.dve(lambda e: e.tensor_copy(out=twopi[:], in_=posi), reads=["t2"], writes=["twopi"])
            P.dve(lambda e: e.scalar_tensor_tensor(out=t1[:], in0=twopi[:], scalar=-TWO_PI, in1=t1[:], op0=ALU.mult, op1=ALU.add),
                  reads=["twopi", "t1"], writes=["t1"])
            P.dve(lambda e: e.tensor_scalar(out=twopi[:], in0=t1[:], scalar1=math.pi, scalar2=-TWO_PI, op0=ALU.is_gt, op1=ALU.mult), reads=["t1"], writes=["twopi"])
            P.dve(lambda e: e.tensor_tensor(out=t1[:], in0=t1[:], in1=twopi[:], op=ALU.add), reads=["t1", "twopi"], writes=["t1"])
            P.dve(lambda e: e.tensor_scalar(out=twopi[:], in0=t1[:], scalar1=-math.pi, scalar2=TWO_PI, op0=ALU.is_lt, op1=ALU.mult), reads=["t1"], writes=["twopi"])
            P.dve(lambda e: e.tensor_tensor(out=t1[:], in0=t1[:], in1=twopi[:], op=ALU.add), reads=["t1", "twopi"], writes=["t1"])
            P.dve(lambda e: e.tensor_scalar(out=t1[:], in0=t1[:], scalar1=3.1415925, scalar2=-3.1415925, op0=ALU.min, op1=ALU.max), reads=["t1"], writes=["t1"])
            P.act(lambda e, dst=dst: e.activation(out=dst[:], in_=t1[:], func=AF.Sin), reads=["t1"], writes=[nm])
        P.dve(lambda e: e.tensor_scalar(out=sinT[:], in0=sinT[:], scalar1=self.ropec[:, 1:2], scalar2=None, op0=ALU.mult), reads=["sinT", "ropec"], writes=["sinT"])
        for c in range(8):
            s = c % 2
            wt = self.wst[s]
            P.dma(lambda e, wt=wt, c=c: e.dma_start(out=wt[:, :, 0:256], in_=self.w_qk[l, c]), writes=[("wst", s)], chan=f"wst{s}", eng="pool")
            b0, b1 = (0, 1) if s == 0 else (2, 3)
            ta_, tb_, ka, kb = (t1, t2, "t1", "t2") if s == 0 else (posf, twopi, "posf", "twopi")
            for kc in range(KC):
                P.pe(lambda e, kc=kc, wt=wt, tg=tg, b0=b0: e.matmul(ps[b0][:], lhsT=wt[:, kc, 0:128], rhs=self.HT[:, kc, tg * 512:(tg + 1) * 512], start=(kc == 0), stop=(kc == KC - 1)),
                     reads=[("wst", s), ("HT", kc, tg)], writes=[("ps", b0)])
            for kc in range(KC):
                P.pe(lambda e, kc=kc, wt=wt, tg=tg, b1=b1: e.matmul(ps[b1][:], lhsT=wt[:, kc, 128:256], rhs=self.HT[:, kc, tg * 512:(tg + 1) * 512], start=(kc == 0), stop=(kc == KC - 1)),
                     reads=[("wst", s), ("HT", kc, tg)], writes=[("ps", b1)])
            dstT = QT if c < 4 else KT
            P.dve(lambda e, ta_=ta_, b0=b0: e.tensor_tensor(out=ta_[:], in0=ps[b0][:], in1=cosT[:], op=ALU.mult), reads=[("ps", b0), "cosT"], writes=[ka])
            P.dve(lambda e, tb_=tb_, b1=b1: e.tensor_tensor(out=tb_[:], in0=ps[b1][:], in1=sinT[:], op=ALU.mult), reads=[("ps", b1), "sinT"], writes=[kb])
            P.pool(lambda e, dstT=dstT, c=c, tg=tg, ta_=ta_, tb_=tb_: e.tensor_tensor(out=dstT[:, c % 4, tg * 512:(tg + 1) * 512], in0=ta_[:], in1=tb_[:], op=ALU.add),
                   reads=[ka, kb], writes=[("da", "kq")])

    def cons_v(tt, bank):
        P.act(lambda e: e.activation(out=Va[:, tt, :, 0:128], in_=ps[bank][:, 0:256].rearrange("p (h d) -> p h d", d=128), func=AF.Identity),
              reads=[("ps", bank)], writes=[("da", "v")])

    def cons_v_b(tt, bank):
        P.act(lambda e: e.activation(out=Va[:, tt, 2:4, 0:128], in_=ps[bank][:, 0:256].rearrange("p (h d) -> p h d", d=128), func=AF.Identity),
              reads=[("ps", bank)], writes=[("da", "v")])

    def cons_v_a(tt, bank):
        P.act(lambda e: e.activation(out=Va[:, tt, 0:2, 0:128], in_=ps[bank][:, 0:256].rearrange("p (h d) -> p h d", d=128), func=AF.Identity),
              reads=[("ps", bank)], writes=[("da", "v")])
    _proj_tm(self, self.w_v[l, 0], 256, cons_v_a)
    _proj_tm(self, self.w_v[l, 1], 256, cons_v_b)
    self.P.barrier()

    def da_out(g, qt, ob):
        h, c = g // 2, g % 2
        if c == 0:
            P.dve(lambda e: e.reciprocal(out=sd[:, 0:1], in_=ps[ob][:, 128:129]), reads=[("ps", ob)], writes=["sd"])
            P.dve(lambda e: e.tensor_scalar(out=odah[:, qt, :], in0=ps[ob][:, 0:128], scalar1=sd[:, 0:1], scalar2=None, op0=ALU.mult),
                  reads=[("ps", ob), "sd", ("wst", 0), ("wst", 1)], writes=["odah", ("wst", 0), ("wst", 1)])
            return
        P.dve(lambda e: e.reciprocal(out=sd[:, 1:2], in_=ps[ob][:, 128:129]), reads=[("ps", ob)], writes=["sd1"])
        P.dve(lambda e: e.tensor_tensor(out=sd[:, 1:2], in0=sd[:, 1:2], in1=lam[:, 2:3], op=ALU.mult), reads=["sd1", "lam"], writes=["sd1"])
        P.dve(lambda e: e.scalar_tensor_tensor(out=odah[:, qt, :], in0=ps[ob][:, 0:128], scalar=sd[:, 1:2], in1=odah[:, qt, :], op0=ALU.mult, op1=ALU.add),
              reads=[("ps", ob), "sd1", "odah"], writes=["odah"])

    for h in range(4):
        for qt in range(NT):
            for c in range(2):
                _blocks(self, "da", qt,
                        kT=lambda kt, h=h, c=c: KT[c * 64:(c + 1) * 64, h, kt * 128:(kt + 1) * 128],
                        qT=lambda qt_, h=h, c=c: QT[c * 64:(c + 1) * 64, h, qt_ * 128:(qt_ + 1) * 128],
                        vaug=lambda kt, h=h: Va[:, kt, h, 0:129], vw=129,
                        out_cb=lambda qt_, ob, h=h, c=c: da_out(2 * h + c, qt_, ob), scale=0.125)
        _blocks_flush(self)
        bq = lambda ap: ap.unsqueeze(2).to_broadcast([128, NT, 128])
        DK = dict(reads=["odah", "sqh", "sd16", "dag", "eps", "cosT", "sinT", "posf", "t2", "twopi"], writes=["odah", "sqh", "sd16", "cosT", "sinT", "posf", "t2", "twopi"])
        P.dve(lambda e: e.tensor_tensor(out=sqh[:], in0=odah[:], in1=odah[:], op=ALU.mult), **DK)
        P.dve(lambda e: e.tensor_reduce(out=sd16[:], in_=sqh[:], axis=AX.X, op=ALU.add), **DK)
        P.act(lambda e: e.activation(out=sd16[:], in_=sd16[:], func=AF.Sqrt, bias=self.eps[:, 0:1], scale=1.0 / 128.0), **DK)
        P.dve(lambda e: e.reciprocal(out=sd16[:], in_=sd16[:]), **DK)
        P.dve(lambda e: e.tensor_tensor(out=sqh[:], in0=odah[:], in1=bq(sd16[:]), op=ALU.mult), **DK)
        P.dve(lambda e: e.tensor_tensor(out=odbh[:], in0=sqh[:], in1=dag[:].unsqueeze(1).to_broadcast([128, NT, 128]), op=ALU.mult), **DK)
        for q4 in range(4):
            tb = 2 + (q4 % 2)
            psb = ps[tb][:].bitcast(BF16)
            for i in range(4):
                qt = q4 * 4 + i
                P.pe(lambda e, qt=qt, i=i, psb=psb: e.transpose(out=psb[:, i * 128:(i + 1) * 128], in_=odbh[:, qt, :], identity=self.identb[:]),
                     reads=["odah", "identb"], writes=[("ps", tb)])
            P.act(lambda e, q4=q4, h=h, psb=psb: e.activation(out=YA[:, h, q4 * 512:(q4 + 1) * 512], in_=psb[:, 0:512], func=AF.Identity),
                  reads=[("ps", tb)], writes=[("YA", h)])
    return QT


def _emit_wout(self, l, R1, wmem):
    P, ps = self.P, self.ps
    X = self.X
    YA = self.HT
    t1 = self.da_t1
    wo = self.w_out[l].rearrange("(kc p) f -> p kc f", p=128)
    wob = [wmem[:, 0:2, :].rearrange("p a (k f) -> p (a k) f", f=512), wmem[:, 2:4, :].rearrange("p a (k f) -> p (a k) f", f=512)]
    for dh in range(2):
        P.dma(lambda e, dh=dh: e.dma_start(out=wob[dh], in_=wo[:, :, dh * 512:(dh + 1) * 512]), writes=[("wob", dh)], chan=f"wob{dh}", eng="pool")
    for tt in range(NT):
        for dh in range(2):
            bank = 4 + ((tt * 2 + dh) % 4)
            for kc in range(KC):
                src = YA[:, kc, tt * 128:(tt + 1) * 128] if kc < 4 else R1[:, kc - 4, tt * 128:(tt + 1) * 128]
                rk = ("YA", kc) if kc < 4 else ("R1", kc - 4)
                P.pe(lambda e, kc=kc, dh=dh, bank=bank, src=src: e.matmul(ps[bank][:], lhsT=src, rhs=wob[dh][:, kc, :], start=(kc == 0), stop=(kc == KC - 1)),
                     reads=[rk, ("wob", dh)], writes=[("ps", bank)])
            P.dve(lambda e, tt=tt, dh=dh, bank=bank: e.tensor_tensor(out=t1[:], in0=ps[bank][:], in1=self.G[:, dh * 512:(dh + 1) * 512], op=ALU.mult),
                  reads=[("ps", bank), "G"], writes=["t1"])
            P.dve(lambda e, tt=tt, dh=dh: e.scalar_tensor_tensor(out=X[:, tt, dh * 512:(dh + 1) * 512], in0=X[:, tt, dh * 512:(dh + 1) * 512], scalar=float(DN_ALPHA),
                                                                 in1=t1[:], op0=ALU.mult, op1=ALU.add),
                  reads=["t1", ("X", tt)], writes=[("X", tt)])
        self.emit_ln(tt)


def _emit_mixer(self, l):
    P = self.P
    self.arena_reset()
    R1 = self.carve([128, 4, S], BF16)
    P.dve(lambda e: e.memset(R1[:, 2:4, :], 0.0), writes=[("R1", 2), ("R1", 3)])
    mark = self.aoff
    _emit_mlstm(self, l, R1)
    if self.cfg.get("s5", True):
        P.barrier()
        self.aoff = mark
        self.emit_s5(l, R1)
    P.barrier()
    self.aoff = mark
    QT = _emit_da(self, l, R1)
    P.barrier()
    _emit_wout(self, l, R1, QT)


def _sin_reduce(self, P, ang, ki, kf, key, keys_extra=()):
    TWO_PI = 2.0 * math.pi
    rk = [key] + list(keys_extra)
    P.dve(lambda e: e.tensor_scalar(out=ki, in0=ang, scalar1=1.0 / TWO_PI, scalar2=None, op0=ALU.mult), reads=rk, writes=[key + "_ki"])
    P.dve(lambda e: e.tensor_copy(out=kf, in_=ki), reads=[key + "_ki"], writes=[key + "_kf"])
    P.dve(lambda e: e.scalar_tensor_tensor(out=ang, in0=kf, scalar=-TWO_PI, in1=ang, op0=ALU.mult, op1=ALU.add), reads=[key + "_kf", key], writes=[key])
    P.dve(lambda e: e.tensor_scalar(out=kf, in0=ang, scalar1=math.pi, scalar2=-TWO_PI, op0=ALU.is_gt, op1=ALU.mult), reads=[key], writes=[key + "_kf"])
    P.dve(lambda e: e.tensor_tensor(out=ang, in0=ang, in1=kf, op=ALU.add), reads=[key, key + "_kf"], writes=[key])
    P.dve(lambda e: e.tensor_scalar(out=kf, in0=ang, scalar1=-math.pi, scalar2=TWO_PI, op0=ALU.is_lt, op1=ALU.mult), reads=[key], writes=[key + "_kf"])
    P.dve(lambda e: e.tensor_tensor(out=ang, in0=ang, in1=kf, op=ALU.add), reads=[key, key + "_kf"], writes=[key])
    P.dve(lambda e: e.tensor_scalar(out=ang, in0=ang, scalar1=3.1415925, scalar2=-3.1415925, op0=ALU.min, op1=ALU.max), reads=[key], writes=[key])


def _declare_s5(self):
    self.s5A_in = self.inp("s5A", [128, DEPTH, 8, 3])
    self.s5A2_in = self.inp("s5A2", [128, DEPTH, 2, 64, 3])
    self.s5bT_in = self.inp("s5bT", [128, DEPTH, 2, 2, 64])
    self.s5cT_in = self.inp("s5cT", [128, DEPTH, 2, 8, 16])
    self.s5bm_in = self.inp("s5bm", [128, 8])
    self.s5cm_in = self.inp("s5cm", [128, 4, 8])
    self.s5d_in = self.inp("s5d", [128, DEPTH, 2])
    self.s5wg_in = self.inp("s5wg", [DEPTH, 2, 2, 128, 128])
    self.w_s5u = self.inp("w_s5u", [DEPTH, 2, 128, KC, 128])
    self.jidx_in = self.inp("jidx", [128, 512])


def _emit_s5(self, l, R1):
    P, ps = self.P, self.ps
    C3 = lambda shape: self.carve(shape, F32)
    self.wst = [self.carve([128, KC, 128], BF16) for _ in range(2)]
    uT = self.carve([128, 2, S], BF16)
    BDr = self.carve([128, 8, 128], BF16)
    BDi = self.carve([128, 8, 128], BF16)
    CBr = self.carve([128, 8, 128], BF16)
    CBi = self.carve([128, 8, 128], BF16)
    wg = self.carve([128, 4, 128], BF16)
    wgt = C3([128, 4, 128])
    A = C3([128, 8, 3])
    A2 = C3([128, 2, 64, 3])
    bT = C3([128, 2, 2, 64])
    cT = C3([128, 2, 8, 16])
    bm = C3([128, 8])
    cm = C3([128, 4, 8])
    dsk = C3([128, 2])
    jidx = C3([128, 512])
    rr = C3([128, 8])
    th = C3([128, 8])
    zst = C3([128, 8, 2])
    T = [C3([128, 128]) for _ in range(8)]
    Ti = C3([128, 128]).bitcast(I32)
    ur, ui, zr, zi, ta, tb2 = [C3([128, 512]) for _ in range(6)]
    cosB = C3([128, 4, 512])
    sinB = C3([128, 4, 512])
    c512 = C3([128, 8])
    s512 = C3([128, 8])
    ns512 = C3([128, 8])
    zin = C3([128, 8, 2])
    ztmp = C3([128, 8])
    ki = C3([128, 512]).bitcast(I32)
    rt = C3([128, 512])
    xr = self.carve([128, 512], BF16)
    xi = self.carve([128, 512], BF16)
    gyb = self.carve([128, 512], BF16)

    ld = lambda dst, src, nm: P.dma(lambda e: e.dma_start(out=dst, in_=src), writes=[nm], chan="c0")
    ld(A[:], self.s5A_in[:, l], "s5A")
    ld(A2[:], self.s5A2_in[:, l], "s5A2")
    ld(bT[:], self.s5bT_in[:, l], "s5bT")
    ld(cT[:], self.s5cT_in[:, l], "s5cT")
    ld(bm[:], self.s5bm_in[:, :], "s5bm")
    ld(cm[:], self.s5cm_in[:, :, :], "s5cm")
    ld(dsk[:], self.s5d_in[:, l], "s5d")
    ld(jidx[:], self.jidx_in[:, :], "jidx")
    for yc in range(2):
        for hf in range(2):
            ld(wgt[:, yc * 2 + hf, :], self.s5wg_in[l, yc, hf], "wgt")
    P.dve(lambda e: e.tensor_copy(out=wg[:], in_=wgt[:]), reads=["wgt"], writes=["wg"])
    P.dve(lambda e: e.memset(zst[:], 0.0), writes=["zst"])

    P.act(lambda e: e.activation(out=rr[:], in_=A[:, :, 2], func=AF.Exp), reads=["s5A"], writes=["dt8"])
    P.dve(lambda e: e.tensor_tensor(out=th[:], in0=A[:, :, 1], in1=rr[:], op=ALU.mult), reads=["s5A", "dt8"], writes=["th"])
    P.dve(lambda e: e.tensor_tensor(out=rr[:], in0=A[:, :, 0], in1=rr[:], op=ALU.mult), reads=["s5A", "dt8", "th"], writes=["dt8"])
    P.act(lambda e: e.activation(out=rr[:], in_=rr[:], func=AF.Exp), reads=["dt8"], writes=["rr"])

    CK = "c512k"
    a8 = T[0][:, 0:8]; a8b = T[1][:, 0:8]; k8 = Ti[:, 0:8]; kf8 = T[2][:, 0:8]
    P.dve(lambda e: e.tensor_scalar(out=a8, in0=th[:], scalar1=512.0, scalar2=None, op0=ALU.mult), reads=["th"], writes=[CK])
    P.dve(lambda e: e.tensor_scalar(out=a8b, in0=a8, scalar1=0.5 * math.pi, scalar2=None, op0=ALU.add), reads=[CK], writes=[CK + "b"])
    _sin_reduce(self, P, a8, k8, kf8, CK)
    P.act(lambda e: e.activation(out=s512[:], in_=a8, func=AF.Sin), reads=[CK], writes=["s512"])
    _sin_reduce(self, P, a8b, k8, kf8, CK + "b", keys_extra=(CK, CK + "_ki", CK + "_kf"))
    P.act(lambda e: e.activation(out=c512[:], in_=a8b, func=AF.Sin), reads=[CK + "b"], writes=["c512"])
    P.dve(lambda e: e.tensor_scalar(out=ns512[:], in0=s512[:], scalar1=-1.0, scalar2=None, op0=ALU.mult), reads=["s512"], writes=["ns512"])

    are = A2[:, :, :, 0].rearrange("p a b -> p (a b)")
    aim = A2[:, :, :, 1].rearrange("p a b -> p (a b)")
    ldt = A2[:, :, :, 2].rearrange("p a b -> p (a b)")
    dt_, mag, ang, sn, cs, t5, t6, t7 = [t[:] for t in T]
    ZK = "zoh"
    def zd(fn):
        P.dve(fn, reads=[ZK, "s5A2", "s5bT", "s512", "c512", "ns512", CK, CK + "b"], writes=[ZK])
    def za(fn):
        P.act(fn, reads=[ZK, "s5A2", "s512", "c512", CK, CK + "b"], writes=[ZK])
    za(lambda e: e.activation(out=dt_, in_=ldt, func=AF.Exp))
    zd(lambda e: e.tensor_tensor(out=mag, in0=are, in1=dt_, op=ALU.mult))
    za(lambda e: e.activation(out=mag, in_=mag, func=AF.Exp))
    zd(lambda e: e.tensor_tensor(out=ang, in0=aim, in1=dt_, op=ALU.mult))
    zd(lambda e: e.tensor_scalar(out=t5, in0=ang, scalar1=0.5 * math.pi, scalar2=None, op0=ALU.add))
    _sin_reduce(self, P, ang, Ti, t6, ZK)
    za(lambda e: e.activation(out=sn, in_=ang, func=AF.Sin))
    _sin_reduce(self, P, t5, Ti, t6, ZK)
    za(lambda e: e.activation(out=cs, in_=t5, func=AF.Sin))
    zd(lambda e: e.tensor_tensor(out=cs, in0=cs, in1=mag, op=ALU.mult))
    zd(lambda e: e.tensor_scalar(out=cs, in0=cs, scalar1=-1.0, scalar2=None, op0=ALU.add))
    zd(lambda e: e.tensor_tensor(out=sn, in0=sn, in1=mag, op=ALU.mult))
    zd(lambda e: e.tensor_tensor(out=t5, in0=are, in1=are, op=ALU.mult))
    zd(lambda e: e.tensor_tensor(out=t6, in0=aim, in1=aim, op=ALU.mult))
    zd(lambda e: e.tensor_tensor(out=t7, in0=t5, in1=t6, op=ALU.add))
    zd(lambda e: e.reciprocal(out=t7, in_=t7))
    zd(lambda e: e.tensor_tensor(out=t5, in0=cs, in1=are, op=ALU.mult))
    zd(lambda e: e.tensor_tensor(out=dt_, in0=sn, in1=aim, op=ALU.mult))
    zd(lambda e: e.tensor_tensor(out=t5, in0=t5, in1=dt_, op=ALU.add))
    zd(lambda e: e.tensor_tensor(out=t5, in0=t5, in1=t7, op=ALU.mult))
    zd(lambda e: e.tensor_tensor(out=t6, in0=sn, in1=are, op=ALU.mult))
    zd(lambda e: e.tensor_tensor(out=dt_, in0=cs, in1=aim, op=ALU.mult))
    zd(lambda e: e.tensor_tensor(out=t6, in0=t6, in1=dt_, op=ALU.subtract))
    zd(lambda e: e.tensor_tensor(out=t6, in0=t6, in1=t7, op=ALU.mult))
    bre = bT[:, 0, :, :].rearrange("p a b -> p (a b)")
    bim = bT[:, 1, :, :].rearrange("p a b -> p (a b)")
    zd(lambda e: e.tensor_tensor(out=mag, in0=t5, in1=bre, op=ALU.mult))
    zd(lambda e: e.tensor_tensor(out=dt_, in0=t6, in1=bim, op=ALU.mult))
    zd(lambda e: e.tensor_tensor(out=mag, in0=mag, in1=dt_, op=ALU.subtract))
    zd(lambda e: e.tensor_tensor(out=ang, in0=t5, in1=bim, op=ALU.mult))
    zd(lambda e: e.tensor_tensor(out=dt_, in0=t6, in1=bre, op=ALU.mult))
    zd(lambda e: e.tensor_tensor(out=ang, in0=ang, in1=dt_, op=ALU.add))
    for (src, dst, nm) in ((mag, BDr, "BDr"), (ang, BDi, "BDi")):
        for sc in range(8):
            for s2 in range(2):
                P.dve(lambda e, src=src, dst=dst, sc=sc, s2=s2: e.tensor_scalar(out=dst[:, sc, s2 * 64:(s2 + 1) * 64], in0=src[:, (sc // 4) * 64:(sc // 4 + 1) * 64],
                                                                               scalar1=bm[:, (sc % 4) * 2 + s2:(sc % 4) * 2 + s2 + 1], scalar2=None, op0=ALU.mult),
                      reads=[ZK, "s5bm"], writes=[nm])
    for (ri, dst, nm, sgn) in ((0, CBr, "CBr", 1.0), (1, CBi, "CBi", -1.0)):
        for sc in range(8):
            P.dve(lambda e, ri=ri, dst=dst, sc=sc, sgn=sgn: e.scalar_tensor_tensor(
                out=dst[:, sc, :].rearrange("p (g h) -> p g h", h=16),
                in0=cT[:, ri, sc, :].unsqueeze(1).to_broadcast([128, 8, 16]), scalar=sgn,
                in1=cm[:, sc % 4, :].unsqueeze(2).to_broadcast([128, 8, 16]), op0=ALU.mult, op1=ALU.mult),
                  reads=["s5cT", "s5cm"], writes=[nm])

    def cons_u(c, tg, bank):
        P.act(lambda e: e.activation(out=uT[:, c, tg * 512:(tg + 1) * 512], in_=ps[bank][:], func=AF.Identity),
              reads=[("ps", bank)], writes=["uT"])
    _proj_fm(self, self.w_s5u[l], 256, cons_u, "s5u")

    GC = 2.0 * math.sqrt(2.0 / math.pi)
    SK = "s5w"
    for yc in range(2):
        for scl in range(4):
            sc = yc * 4 + scl
            for (dst, shf) in ((sinB, 0.0), (cosB, 0.5 * math.pi)):
                P.dve(lambda e, sc=sc, shf=shf: e.tensor_scalar(out=ta[:], in0=jidx[:], scalar1=th[:, sc:sc + 1], scalar2=shf, op0=ALU.mult, op1=ALU.add),
                      reads=["jidx", "th", SK], writes=[SK])
                _sin_reduce(self, P, ta[:], ki, tb2[:], SK)
                P.act(lambda e, dst=dst, scl=scl: e.activation(out=dst[:, scl, :], in_=ta[:], func=AF.Sin), reads=[SK], writes=[SK])
        for tg in range(4):
            yb = 6 + (tg % 2)
            for scl in range(4):
                sc = yc * 4 + scl
                cosT = cosB[:, scl, :]
                sinT = sinB[:, scl, :]
                P.pe(lambda e, sc=sc, yc=yc, tg=tg: e.matmul(ps[0][:], lhsT=BDr[:, sc, :], rhs=uT[:, yc, tg * 512:(tg + 1) * 512], start=True, stop=True),
                     reads=["BDr", "uT"], writes=[("ps", 0)])
                P.pe(lambda e, sc=sc, yc=yc, tg=tg: e.matmul(ps[1][:], lhsT=BDi[:, sc, :], rhs=uT[:, yc, tg * 512:(tg + 1) * 512], start=True, stop=True),
                     reads=["BDi", "uT"], writes=[("ps", 1)])
                W = dict(reads=[SK, ("ps", 0), ("ps", 1), "rr", "zst", "c512", "s512", "ns512"], writes=[SK])
                if tg == 0:
                    P.dve(lambda e, sc=sc: e.memset(zin[:, sc, :], 0.0), **W)
                else:
                    P.dve(lambda e, sc=sc: e.tensor_tensor(out=ztmp[:, 0:1], in0=zst[:, sc, 0:1], in1=c512[:, sc:sc + 1], op=ALU.mult), **W)
                    P.dve(lambda e, sc=sc: e.scalar_tensor_tensor(out=zin[:, sc, 0:1], in0=zst[:, sc, 1:2], scalar=ns512[:, sc:sc + 1], in1=ztmp[:, 0:1],
                                                                  op0=ALU.mult, op1=ALU.add), **W)
                    P.dve(lambda e, sc=sc: e.tensor_tensor(out=ztmp[:, 1:2], in0=zst[:, sc, 1:2], in1=c512[:, sc:sc + 1], op=ALU.mult), **W)
                    P.dve(lambda e, sc=sc: e.scalar_tensor_tensor(out=zin[:, sc, 1:2], in0=zst[:, sc, 0:1], scalar=s512[:, sc:sc + 1], in1=ztmp[:, 1:2],
                                                                  op0=ALU.mult, op1=ALU.add), **W)
                P.dve(lambda e, cosT=cosT: e.tensor_tensor(out=ur[:], in0=ps[0][:], in1=cosT, op=ALU.mult), **W)
                P.dve(lambda e, sinT=sinT: e.tensor_tensor(out=ta[:], in0=ps[1][:], in1=sinT, op=ALU.mult), **W)
                P.dve(lambda e: e.tensor_tensor(out=ur[:], in0=ur[:], in1=ta[:], op=ALU.add), **W)
                P.dve(lambda e, cosT=cosT: e.tensor_tensor(out=ui[:], in0=ps[1][:], in1=cosT, op=ALU.mult), **W)
                P.dve(lambda e, sinT=sinT: e.tensor_tensor(out=tb2[:], in0=ps[0][:], in1=sinT, op=ALU.mult), **W)
                P.dve(lambda e: e.tensor_tensor(out=ui[:], in0=ui[:], in1=tb2[:], op=ALU.subtract), **W)
                P.dve(lambda e, sc=sc: e.tensor_copy(out=rt[:], in_=rr[:, sc:sc + 1].to_broadcast([128, 512])), **W)
                P.dve(lambda e, sc=sc: e.tensor_tensor_scan(out=zr[:], data0=rt[:], data1=ur[:], initial=zin[:, sc, 0:1], op0=ALU.mult, op1=ALU.add), **W)
                P.dve(lambda e, sc=sc: e.tensor_tensor_scan(out=zi[:], data0=rt[:], data1=ui[:], initial=zin[:, sc, 1:2], op0=ALU.mult, op1=ALU.add), **W)
                P.dve(lambda e, sc=sc: e.tensor_copy(out=zst[:, sc, 0:1], in_=zr[:, 511:512]), reads=[SK], writes=[SK, "zst"])
                P.dve(lambda e, sc=sc: e.tensor_copy(out=zst[:, sc, 1:2], in_=zi[:, 511:512]), reads=[SK], writes=[SK, "zst"])
                P.dve(lambda e, cosT=cosT: e.tensor_tensor(out=ta[:], in0=zr[:], in1=cosT, op=ALU.mult), **W)
                P.dve(lambda e, sinT=sinT: e.tensor_tensor(out=tb2[:], in0=zi[:], in1=sinT, op=ALU.mult), **W)
                P.dve(lambda e: e.tensor_tensor(out=xr[:], in0=ta[:], in1=tb2[:], op=ALU.subtract), reads=[SK, "s_xr"], writes=[SK, "s_xr"])
                P.dve(lambda e, sinT=sinT: e.tensor_tensor(out=ur[:], in0=zr[:], in1=sinT, op=ALU.mult), **W)
                P.dve(lambda e, cosT=cosT: e.tensor_tensor(out=ui[:], in0=zi[:], in1=cosT, op=ALU.mult), **W)
                P.dve(lambda e: e.tensor_tensor(out=xi[:], in0=ur[:], in1=ui[:], op=ALU.add), reads=[SK, "s_xi"], writes=[SK, "s_xi"])
                P.pe(lambda e, sc=sc, scl=scl, yb=yb: e.matmul(ps[yb][:], lhsT=CBr[:, sc, :], rhs=xr[:], start=(scl == 0), stop=False),
                     reads=["CBr", "s_xr"], writes=[("ps", yb)])
                P.pe(lambda e, sc=sc, scl=scl, yb=yb: e.matmul(ps[yb][:], lhsT=CBi[:, sc, :], rhs=xi[:], start=False, stop=(scl == 3)),
                     reads=["CBi", "s_xi"], writes=[("ps", yb)])
            YK = dict(reads=[SK, "uT", "s5d", ("ps", yb)], writes=[SK])
            P.dve(lambda e, yc=yc, tg=tg, yb=yb: e.scalar_tensor_tensor(out=zr[:], in0=uT[:, yc, tg * 512:(tg + 1) * 512], scalar=dsk[:, yc:yc + 1], in1=ps[yb][:],
                                                                       op0=ALU.mult, op1=ALU.add), **YK)
            P.dve(lambda e: e.tensor_tensor(out=zi[:], in0=zr[:], in1=zr[:], op=ALU.mult), **YK)
            P.dve(lambda e: e.tensor_scalar(out=zi[:], in0=zi[:], scalar1=0.044715, scalar2=1.0, op0=ALU.mult, op1=ALU.add), **YK)
            P.dve(lambda e: e.tensor_tensor(out=zi[:], in0=zi[:], in1=zr[:], op=ALU.mult), **YK)
            P.act(lambda e: e.activation(out=zi[:], in_=zi[:], func=AF.Sigmoid, scale=GC), **YK)
            P.dve(lambda e: e.tensor_tensor(out=gyb[:], in0=zi[:], in1=zr[:], op=ALU.mult), reads=[SK, "s_gy"], writes=[SK, "s_gy"])
            P.pe(lambda e, yc=yc: e.matmul(ps[2][:], lhsT=wg[:, yc * 2, :], rhs=gyb[:], start=True, stop=True), reads=["wg", "s_gy"], writes=[("ps", 2)])
            P.pe(lambda e, yc=yc: e.matmul(ps[3][:], lhsT=wg[:, yc * 2 + 1, :], rhs=gyb[:], start=True, stop=True), reads=["wg", "s_gy"], writes=[("ps", 3)])
            P.act(lambda e: e.activation(out=ur[:], in_=ps[3][:], func=AF.Sigmoid), reads=[("ps", 3), SK], writes=[SK])
            P.dve(lambda e, yc=yc, tg=tg: e.tensor_tensor(out=R1[:, 2 + yc, tg * 512:(tg + 1) * 512], in0=ps[2][:], in1=ur[:], op=ALU.mult),
                  reads=[("ps", 2), SK], writes=[("R1", 2 + yc), SK])


Builder.declare_s5 = _declare_s5
Builder.emit_s5 = _emit_s5
Builder.declare_mixer = _declare_mixer
Builder.emit_mixer_consts = _emit_mixer_consts
Builder.emit_mixer = _emit_mixer
```

```python
import math
from contextlib import ExitStack

import numpy as np
import concourse.bass as bass
import concourse.mybir as mybir
from concourse.bass_utils import run_bass_kernel_spmd

F32 = mybir.dt.float32
BF16 = mybir.dt.bfloat16
I32 = mybir.dt.int32
AF = mybir.ActivationFunctionType
ALU = mybir.AluOpType
AX = mybir.AxisListType

D = 1024
S = 2048
NT = 16
KC = 8
DEPTH = 2
NE = 32
DN_ALPHA = (2 * DEPTH) ** 0.25
LN_EPS = 1e-5
N_IN = 2568

ENGS = ("pe", "act", "dve", "pool", "sp")


class Op:
    __slots__ = ("eng", "fn", "reads", "writes", "chan", "idx", "sig", "sigval",
                 "waits", "chan_count")

    def __init__(self, eng, fn, reads, writes, chan):
        self.eng, self.fn, self.reads, self.writes, self.chan = eng, fn, reads, writes, chan
        self.sig = False
        self.sigval = 0
        self.waits = []
        self.chan_count = 0


class Prog:
    def __init__(self, nc, same_engine_sync=True):
        self.nc = nc
        self.ops = []
        self.same_engine_sync = same_engine_sync
        self.barriers = []
        self.stack = ExitStack()

    def op(self, eng, fn, reads=(), writes=(), chan=None):
        o = Op(eng, fn, tuple(reads), tuple(writes), chan)
        o.idx = len(self.ops)
        self.ops.append(o)
        return o

    def pe(self, fn, reads=(), writes=()):
        return self.op("pe", fn, reads, writes)

    def act(self, fn, reads=(), writes=()):
        return self.op("act", fn, reads, writes)

    def dve(self, fn, reads=(), writes=()):
        return self.op("dve", fn, reads, writes)

    def pool(self, fn, reads=(), writes=()):
        return self.op("pool", fn, reads, writes)

    def dma(self, fn, reads=(), writes=(), chan="d0", eng="sp"):
        return self.op(eng, fn, reads, writes, chan)

    def barrier(self):
        self.barriers.append(len(self.ops))

    def sbuf(self, name, shape, dtype):
        return self.stack.enter_context(self.nc.sbuf_tensor(name, list(shape), dtype))

    def psum(self, name, shape, dtype):
        return self.stack.enter_context(self.nc.psum_tensor(name, list(shape), dtype))

    def resolve(self):
        ops = self.ops
        last_w, readers, last_on_eng, chan_total = {}, {}, {}, {}
        bset = sorted(set(self.barriers))
        bi = 0
        pending_barrier = {}
        deps_all = []
        for o in ops:
            while bi < len(bset) and bset[bi] <= o.idx:
                snap = (dict(last_on_eng), dict(chan_total))
                for e in ENGS:
                    pending_barrier[e] = snap
                bi += 1
            deps = set()
            cdeps = {}
            if o.eng in pending_barrier:
                leng, ctot = pending_barrier.pop(o.eng)
                for d in leng.values():
                    deps.add(d)
                for c, n in ctot.items():
                    cdeps[c] = max(cdeps.get(c, 0), n)
            for k in o.reads:
                d = last_w.get(k)
                if d is not None:
                    deps.add(d)
            for k in o.writes:
                d = last_w.get(k)
                if d is not None:
                    deps.add(d)
                for r in readers.get(k, ()):
                    deps.add(r)
            real = []
            for d in deps:
                if d is o:
                    continue
                if d.chan is not None:
                    cdeps[d.chan] = max(cdeps.get(d.chan, 0), chan_total[d.chan])
                    continue
                if d.eng == o.eng and (o.eng == "pe" or not self.same_engine_sync):
                    continue
                real.append(d)
                d.sig = True
            deps_all.append((real, cdeps))
            for k in o.writes:
                last_w[k] = o
                readers[k] = []
            for k in o.reads:
                readers.setdefault(k, []).append(o)
            if o.chan is not None:
                chan_total[o.chan] = chan_total.get(o.chan, 0) + 1
                o.chan_count = chan_total[o.chan]
            else:
                last_on_eng[o.eng] = o
        cnt = {e: 0 for e in ENGS}
        for o in ops:
            if o.chan is None and o.sig:
                cnt[o.eng] += 1
                o.sigval = cnt[o.eng]
        self.sig_counts = cnt
        self.chan_totals = chan_total
        waited = {e: {} for e in ENGS}
        for o, (real, cdeps) in zip(ops, deps_all):
            w = {}
            for d in real:
                w[d.eng] = max(w.get(d.eng, 0), d.sigval)
            for c, n in cdeps.items():
                w[("chan", c)] = max(w.get(("chan", c), 0), 16 * n)
            out = []
            for k, v in w.items():
                if waited[o.eng].get(k, 0) < v:
                    waited[o.eng][k] = v
                    out.append((k, v))
            o.waits = out

    def emit(self):
        nc = self.nc
        self.resolve()
        sems = {}
        for e in ENGS:
            sems[e] = self.stack.enter_context(nc.semaphore("s_" + e))
        for c in self.chan_totals:
            sems[("chan", c)] = self.stack.enter_context(nc.semaphore("c_" + c))
        by_eng = {e: [o for o in self.ops if o.eng == e] for e in ENGS}
        engobj = {"pe": "tensor", "act": "scalar", "dve": "vector", "pool": "gpsimd", "sp": "sync"}

        def make_section(e):
            def section(eng):
                for o in by_eng[e]:
                    for k, v in o.waits:
                        eng.wait_ge(sems[k], v)
                    ins = o.fn(eng)
                    if o.chan is not None:
                        ins.then_inc(sems[("chan", o.chan)], 16)
                    elif o.sig:
                        ins.then_inc(sems[e], 1)
            return section

        with nc.Block() as block:
            for e in ENGS:
                if by_eng[e]:
                    getattr(block, engobj[e])(make_section(e))
        self.stack.close()


class Builder:
    def __init__(self, cfg):
        self.cfg = cfg
        self.nc = bass.Bass("TRN2", target_bir_lowering=False)
        self.P = Prog(self.nc, same_engine_sync=cfg.get("ses", True))
        self.din = {}
        self.dout = {}
        self._uid = 0

    def inp(self, name, shape, dtype=F32):
        t = self.nc.dram_tensor(name, list(shape), dtype, kind="ExternalInput").ap()
        self.din[name] = t
        return t

    def outp(self, name, shape, dtype=F32):
        t = self.nc.dram_tensor(name, list(shape), dtype, kind="ExternalOutput").ap()
        self.dout[name] = t
        return t

    def uid(self, p="t"):
        self._uid += 1
        return f"{p}{self._uid}"

    def arena_init(self, nbytes):
        self.arena = self.P.sbuf("arena", [128, nbytes // 4], F32)
        self.arena_words = nbytes // 4
        self.aoff = 0

    def arena_reset(self):
        self.P.barrier()
        self.aoff = 0

    def carve(self, shape, dtype):
        esz = 2 if dtype == BF16 else 4
        n = 1
        for d_ in shape[1:]:
            n *= d_
        words = (n * esz + 3) // 4
        words = (words + 7) // 8 * 8
        assert self.aoff + words <= self.arena_words, ("arena overflow", self.aoff, words, self.arena_words)
        v = self.arena[0:shape[0], self.aoff:self.aoff + words]
        self.aoff += words
        if dtype != F32:
            v = v.bitcast(dtype)
        v = v[:, 0:n]
        if len(shape) == 3:
            v = v.rearrange("p (a b) -> p a b", b=shape[2])
        elif len(shape) == 4:
            v = v.rearrange("p (a b c) -> p a b c", b=shape[2], c=shape[3])
        return v

    def declare_common(self):
        P = self.P
        self.x_in = self.inp("x", [S, D])
        self.cT_in = self.inp("cT", [128, KC])
        self.ident_in = self.inp("ident", [128, 128])
        self.ada_w = self.inp("ada_w", [DEPTH, 2, D, 3 * D])
        self.ada_b = self.inp("ada_b", [DEPTH, 2, 3 * D])
        self.ln_g = self.inp("ln_g", [DEPTH, 2, D])
        self.ln_b = self.inp("ln_b", [DEPTH, 2, D])
        self.w_router = self.inp("w_router", [DEPTH, D, NE])
        self.b_router = self.inp("b_router", [DEPTH, NE])
        ned = self.cfg.get("n_exp_decl", NE)
        self.w_up = self.inp("w_up", [DEPTH, ned, D, 2 * D])
        self.b_upT = self.inp("b_upT", [128, DEPTH, NE, 16])
        self.w_down = self.inp("w_down", [DEPTH, ned, D, D])
        self.b_down = self.inp("b_down", [DEPTH, NE, D])
        self.out = self.outp("out", [S, D])

        self.X = P.sbuf("X", [128, NT, D], F32)
        self.HT = P.sbuf("HT", [128, KC, S], BF16)
        self.ident = P.sbuf("ident32", [128, 128], F32)
        self.identb = P.sbuf("identb", [128, 128], BF16)
        self.cT = P.sbuf("cTs", [128, KC], F32)
        self.cB = P.sbuf("cB", [128, KC, 128], BF16)
        self.eps = P.sbuf("epsc", [128, 1], F32)
        self.ps = [P.psum(f"ps{i}", [128, 512], F32) for i in range(8)]
        self.G = P.sbuf("Grow", [128, D], F32)
        self.sc1p = P.sbuf("sc1p", [128, KC], F32)
        self.shp = P.sbuf("shp", [128, KC], F32)
        self.lng = P.sbuf("lng", [128, D], F32)
        self.lnb = P.sbuf("lnb", [128, D], F32)
        self.comb = P.sbuf("comb", [128, NT, NE], F32)
        self.bup = P.sbuf("bup", [128, NE, 16], F32)
        self.lnst = P.sbuf("lnst", [128, 2, 6], F32)
        self.lnmv = P.sbuf("lnmv", [128, 2], F32)
        self.lnr = P.sbuf("lnr", [128, 1], F32)
        self.arena_init(self.cfg.get("arena_bytes", 94208))

    def emit_consts(self):
        P = self.P
        P.dma(lambda e: e.dma_start(out=self.ident[:], in_=self.ident_in[:, :]),
              writes=["ident"], chan="c0")
        P.dma(lambda e: e.dma_start(out=self.cT[:], in_=self.cT_in[:, :]),
              writes=["cT"], chan="c0")
        P.dve(lambda e: e.tensor_copy(out=self.identb[:], in_=self.ident[:]),
              reads=["ident"], writes=["identb"])
        P.dve(lambda e: e.memset(self.eps[:], LN_EPS), writes=["eps"])
        P.act(lambda e: e.activation(out=self.cT[:], in_=self.cT[:], func=AF.Silu),
              reads=["cT"], writes=["cT"])
        P.dve(lambda e: e.tensor_copy(out=self.cB[:], in_=self.cT[:].unsqueeze(2).to_broadcast([128, KC, 128])),
              reads=["cT"], writes=["cB"])

    def load_x(self, src=None):
        P = self.P
        src = self.x_in if src is None else src
        v = src.rearrange("(tt p) d -> p tt d", p=128)
        for q in range(4):
            P.dma(lambda e, q=q: e.dma_start(out=self.X[:, 4 * q:4 * q + 4, :], in_=v[:, 4 * q:4 * q + 4, :]),
                  writes=[("X", t) for t in range(4 * q, 4 * q + 4)], chan="xio")

    def store_x(self, dst=None):
        P = self.P
        dst = self.out if dst is None else dst
        v = dst.rearrange("(tt p) d -> p tt d", p=128)
        for q in range(4):
            P.dma(lambda e, q=q: e.dma_start(out=v[:, 4 * q:4 * q + 4, :], in_=self.X[:, 4 * q:4 * q + 4, :]),
                  reads=[("X", t) for t in range(4 * q, 4 * q + 4)], writes=[("out", q)], chan="xout")
        P.op("sp", lambda e: e.nop(), reads=[("out", q) for q in range(4)])

    def emit_mod(self, l, j):
        P = self.P
        ps = self.ps
        modw = [self.carve([128, KC, 512], BF16) for _ in range(2)]
        adab = [self.carve([128, 512], F32) for _ in range(2)]
        mtmp = self.carve([128, 512], F32)
        mtmp2 = self.carve([128, 512], F32)
        P.dma(lambda e: e.dma_start(out=self.lng[:], in_=self.ln_g[l, j].partition_broadcast(128)),
              writes=["lng"], chan="c0")
        P.dma(lambda e: e.dma_start(out=self.lnb[:], in_=self.ln_b[l, j].partition_broadcast(128)),
              writes=["lnb"], chan="c0")
        wv = self.ada_w[l, j].rearrange("(kc p) e -> p kc e", p=128)
        idb = self.ident[:].unsqueeze(1).to_broadcast([128, 4, 128])
        for pc in range(6):
            s = pc % 2
            P.dma(lambda e, pc=pc, s=s: e.dma_start(out=adab[s][:], in_=self.ada_b[l, j, pc * 512:(pc + 1) * 512].partition_broadcast(128)),
                  writes=[("adab", s)], chan=f"adab{s}")
            P.dma(lambda e, pc=pc, s=s: e.dma_start(out=modw[s][:], in_=wv[:, :, pc * 512:(pc + 1) * 512]),
                  writes=[("modw", s)], chan=f"modw{s}", eng="pool")
            bank = 6 + (pc % 2)
            for kc in range(KC):
                P.pe(lambda e, kc=kc, s=s, bank=bank: e.matmul(ps[bank][:], lhsT=self.cB[:, kc, :], rhs=modw[s][:, kc, :],
                                                               start=(kc == 0), stop=(kc == KC - 1)),
                     reads=["cB", ("modw", s)], writes=[("ps", bank)])
            if pc >= 4:
                P.dve(lambda e, pc=pc, bank=bank, s=s: e.scalar_tensor_tensor(out=self.G[:, (pc - 4) * 512:(pc - 3) * 512], in0=ps[bank][:], scalar=1.0,
                                                                              in1=adab[s][:], op0=ALU.add, op1=ALU.add),
                      reads=[("ps", bank), ("adab", s)], writes=["G"])
            else:
                dst, name = (self.shp, "shp") if pc < 2 else (self.sc1p, "sc1p")
                c0 = (pc % 2) * 4
                addc = 0.0 if pc < 2 else 1.0
                P.dve(lambda e, bank=bank, s=s, addc=addc: e.scalar_tensor_tensor(out=mtmp[:], in0=ps[bank][:], scalar=addc,
                                                                                   in1=adab[s][:], op0=ALU.add, op1=ALU.add),
                      reads=[("ps", bank), ("adab", s)], writes=["mtmp"])
                P.dve(lambda e: e.tensor_tensor(out=mtmp2[:].rearrange("p (k q) -> p k q", q=128),
                                                in0=mtmp[:].rearrange("p (k q) -> p k q", q=128), in1=idb, op=ALU.mult),
                      reads=["mtmp", "ident"], writes=["mtmp2"])
                P.dve(lambda e, dst=dst, c0=c0: e.tensor_reduce(out=dst[:, c0:c0 + 4], in_=mtmp2[:].rearrange("p (k q) -> p k q", q=128),
                                                                axis=AX.X, op=ALU.add),
                      reads=["mtmp2"], writes=[name])

    def emit_hT(self, l, router):
        P = self.P
        ps = self.ps
        if router:
            wr = self.wr[:, :, 0:128]
            P.dve(lambda e: e.memset(wr, 0.0), writes=[("w1l", 1)])
            P.dma(lambda e: e.dma_start(out=wr[:, :, 0:NE], in_=self.w_router[l].rearrange("(kc p) e -> p kc e", p=128)),
                  writes=[("w1l", 1)], chan="c0")
            P.dve(lambda e: e.tensor_copy(out=self.wrb[:], in_=wr), reads=[("w1l", 1)], writes=["wrb"])
            P.dve(lambda e: e.tensor_tensor(out=self.wrl[:], in0=wr, in1=self.wrb[:], op=ALU.subtract), reads=[("w1l", 1), "wrb"], writes=["wrl"])
            P.dma(lambda e: e.dma_start(out=self.brow[:], in_=self.b_router[l].partition_broadcast(128)),
                  writes=["brow"], chan="c0")
        for tg in range(4):
            for kc in range(KC):
                bank = kc % 2
                for i in range(4):
                    tt = tg * 4 + i
                    P.pe(lambda e, tt=tt, kc=kc, i=i, bank=bank: e.transpose(out=ps[bank][:, i * 128:(i + 1) * 128],
                                                                           in_=self.X[:, tt, kc * 128:(kc + 1) * 128],
                                                                           identity=self.ident[:]),
                         reads=[("X", tt), "ident"], writes=[("ps", bank)])
                if not router:
                    P.act(lambda e, kc=kc, tg=tg, bank=bank: e.activation(out=self.HT[:, kc, tg * 512:(tg + 1) * 512], in_=ps[bank][:],
                                                                          func=AF.Identity, bias=self.shp[:, kc:kc + 1],
                                                                          scale=self.sc1p[:, kc:kc + 1]),
                          reads=[("ps", bank), "sc1p", "shp"], writes=[("HT", kc, tg)])
                else:
                    hs = kc % 2
                    h32 = self.eA[hs]
                    P.dve(lambda e, kc=kc, bank=bank, h32=h32: e.tensor_scalar(out=h32[:], in0=ps[bank][:], scalar1=self.sc1p[:, kc:kc + 1], scalar2=self.shp[:, kc:kc + 1],
                                                                               op0=ALU.mult, op1=ALU.add),
                          reads=[("ps", bank), "sc1p", "shp"], writes=[("eA", hs)])
                    P.act(lambda e, kc=kc, tg=tg, h32=h32: e.activation(out=self.HT[:, kc, tg * 512:(tg + 1) * 512], in_=h32[:], func=AF.Identity),
                          reads=[("eA", hs)], writes=[("HT", kc, tg)])
                    P.pool(lambda e, kc=kc, tg=tg, h32=h32: e.tensor_tensor(out=self.hlo[:, kc, :], in0=h32[:], in1=self.HT[:, kc, tg * 512:(tg + 1) * 512], op=ALU.subtract),
                           reads=[("eA", hs), ("HT", kc, tg)], writes=[("w1g", 1)])
            if router:
                for kc in range(KC):
                    trip = ((self.wrb, self.HT[:, kc, tg * 512:(tg + 1) * 512]), (self.wrb, self.hlo[:, kc, :]), (self.wrl, self.HT[:, kc, tg * 512:(tg + 1) * 512]))
                    for ti, (wmat, rhs_) in enumerate(trip):
                        P.pe(lambda e, kc=kc, ti=ti, wmat=wmat, rhs_=rhs_: e.matmul(ps[2][:], lhsT=wmat[:, kc, :], rhs=rhs_,
                                                                                   start=(kc == 0 and ti == 0), stop=(kc == KC - 1 and ti == 2)),
                             reads=[("HT", kc, tg), ("w1g", 1), "wrb", "wrl"], writes=[("ps", 2)])
            rsub = self.cfg.get("rsub", 9)
            if router and rsub >= 2:
                P.act(lambda e: e.activation(out=self.lgT[:], in_=ps[2][0:32, :], func=AF.Identity),
                      reads=[("ps", 2)], writes=["lgT"])
                for i in range(4):
                    tt = tg * 4 + i
                    P.pe(lambda e, i=i: e.transpose(out=ps[3][:, i * 32:(i + 1) * 32], in_=self.lgT[:, i * 128:(i + 1) * 128],
                                                    identity=self.ident[0:32, 0:32]),
                         reads=["lgT", "ident"], writes=[("ps", 3)])
                lg = self.lg
                P.dve(lambda e: e.tensor_tensor(out=lg[:], in0=ps[3][:, 0:128].rearrange("p (i e) -> p i e", e=32),
                                                in1=self.brow[:].unsqueeze(1).to_broadcast([128, 4, NE]), op=ALU.add),
                      reads=[("ps", 3), "brow"], writes=["lg"])
                for i in range(4 if rsub >= 3 else 0):
                    tt = tg * 4 + i
                    t8, ng, ex, sm = self.top8, self.negm, self.ex, self.ssum
                    P.dve(lambda e, i=i: e.max(out=t8[:], in_=lg[:, i, :]), reads=["lg"], writes=["t8"])
                    P.dve(lambda e: e.tensor_scalar(out=ng[:, 0:1], in0=t8[:, 0:1], scalar1=-1.0, scalar2=None, op0=ALU.mult),
                          reads=["t8"], writes=["ng"])
                    P.act(lambda e, i=i: e.activation(out=ex[:], in_=lg[:, i, :], func=AF.Exp, bias=ng[:, 0:1], scale=1.0),
                          reads=["lg", "ng"], writes=["ex"])
                    P.dve(lambda e, i=i: e.tensor_scalar(out=self.msk[:], in0=lg[:, i, :], scalar1=t8[:, 3:4], scalar2=None, op0=ALU.is_ge),
                          reads=["lg", "t8"], writes=["msk"])
                    P.dve(lambda e: e.tensor_tensor(out=ex[:], in0=ex[:], in1=self.msk[:], op=ALU.mult),
                          reads=["ex", "msk"], writes=["ex"])
                    P.dve(lambda e: e.tensor_reduce(out=sm[:, 0:1], in_=ex[:], axis=AX.X, op=ALU.add),
                          reads=["ex"], writes=["sm"])
                    P.dve(lambda e: e.reciprocal(out=sm[:, 0:1], in_=sm[:, 0:1]), reads=["sm"], writes=["sm"])
                    P.dve(lambda e, tt=tt: e.tensor_scalar(out=self.comb[:, tt, :], in0=ex[:], scalar1=sm[:, 0:1], scalar2=None, op0=ALU.mult),
                          reads=["ex", "sm"], writes=[("comb", tt)])

    def declare_moe(self):
        self.wrb = self.carve([128, KC, 128], BF16)
        self.wrl = self.carve([128, KC, 128], BF16)
        self.brow = self.carve([128, NE], F32)
        self.lgT = self.carve([32, 512], F32)
        self.lg = self.carve([128, 4, NE], F32)
        self.top8 = self.carve([128, 8], F32)
        self.negm = self.carve([128, 8], F32)
        self.ex = self.carve([128, NE], F32)
        self.msk = self.carve([128, NE], F32)
        self.ssum = self.carve([128, 8], F32)
        self.combT = self.carve([32, NT, 128], BF16)
        NW = 2
        self.NW = NW
        self.w1g = [self.carve([128, KC, 512], BF16) for i in range(NW)]
        self.w1l = [self.carve([128, KC, 512], BF16) for i in range(NW)]
        self.w2h = [self.carve([128, 4, D], BF16) for i in range(NW)]
        self.bdn = self.carve([32, D], F32)
        self.bdnG = self.carve([32, D], BF16)
        self.NA = 2
        self.actb = [self.carve([128, 4, 512], BF16) for i in range(self.NA)]
        self.NEp = 2
        self.eA = [self.carve([128, 512], F32) for i in range(self.NEp)]
        self.eB = [self.carve([128, 512], F32) for i in range(self.NEp)]
        self.eS = [self.carve([128, 512], F32) for i in range(self.NEp)]
        self.hlo = self.w1g[1]
        self.wr = self.w1l[1].bitcast(F32)
        self.h32 = self.eA[0]

    def emit_bup(self, l):
        P = self.P
        P.dma(lambda e: e.dma_start(out=self.bup[:], in_=self.b_upT[:, l, :, :]), writes=["bup"], chan="c0")
        P.dve(lambda e: e.tensor_scalar(out=self.bup[:, :, 8:16], in0=self.bup[:, :, 8:16], scalar1=1.0, scalar2=None, op0=ALU.add),
              reads=["bup"], writes=["bup"])

    def emit_ln(self, tt, src_keys=()):
        P = self.P
        X = self.X
        k = ("X", tt)
        for h in range(2):
            P.dve(lambda e, h=h: e.bn_stats(out=self.lnst[:, h, :], in_=X[:, tt, h * 512:(h + 1) * 512]),
                  reads=[k], writes=[("lnst", h)])
        P.dve(lambda e: e.bn_aggr(out=self.lnmv[:], in_=self.lnst[:].rearrange("p a b -> p (a b)")),
              reads=[("lnst", 0), ("lnst", 1)], writes=["lnmv"])
        P.act(lambda e: e.activation(out=self.lnr[:], in_=self.lnmv[:, 1:2], func=AF.Sqrt, bias=self.eps[:, 0:1], scale=1.0),
              reads=["lnmv", "eps"], writes=["lnr"])
        P.dve(lambda e: e.reciprocal(out=self.lnr[:], in_=self.lnr[:]), reads=["lnr"], writes=["lnr"])
        P.dve(lambda e: e.tensor_scalar(out=X[:, tt, :], in0=X[:, tt, :], scalar1=self.lnmv[:, 0:1], scalar2=self.lnr[:, 0:1],
                                        op0=ALU.subtract, op1=ALU.mult),
              reads=[k, "lnmv", "lnr"], writes=[k])
        P.dve(lambda e: e.tensor_tensor(out=X[:, tt, :], in0=X[:, tt, :], in1=self.lng[:], op=ALU.mult),
              reads=[k, "lng"], writes=[k])
        P.dve(lambda e: e.tensor_tensor(out=X[:, tt, :], in0=X[:, tt, :], in1=self.lnb[:], op=ALU.add),
              reads=[k, "lnb"], writes=[k])

    def emit_moe(self, l, n_exp=NE):
        P = self.P
        ps = self.ps
        X = self.X
        P.dma(lambda e: e.dma_start(out=self.bdn[:], in_=self.b_down[l]), writes=["bdn"], chan="c0")
        P.dve(lambda e: e.tensor_tensor(out=self.bdnG[:], in0=self.bdn[:], in1=self.G[0:32, :], op=ALU.mult),
              reads=["bdn", "G"], writes=["bdnG"])
        for tt in range(NT):
            bank = 2 + (tt % 2)
            P.pe(lambda e, tt=tt, bank=bank: e.transpose(out=ps[bank][0:32, 0:128], in_=self.comb[:, tt, :], identity=self.ident[:]),
                 reads=[("comb", tt), "ident"], writes=[("ps", bank)])
            P.act(lambda e, tt=tt, bank=bank: e.activation(out=self.combT[:, tt, :], in_=ps[bank][0:32, 0:128], func=AF.Identity),
                  reads=[("ps", bank)], writes=[("combT", tt)])
        for tt in range(NT):
            P.act(lambda e, tt=tt: e.activation(out=X[:, tt, :], in_=X[:, tt, :], func=AF.Copy, scale=float(DN_ALPHA)),
                  reads=[("X", tt)], writes=[("X", tt)])

        for tt in range(NT):
            for dh in range(2):
                ob = 4 + ((tt * 2 + dh) % 4)
                P.pe(lambda e, tt=tt, dh=dh, ob=ob: e.matmul(ps[ob][:], lhsT=self.combT[:, tt, :], rhs=self.bdnG[:, dh * 512:(dh + 1) * 512],
                                                             start=True, stop=True),
                     reads=[("combT", tt), "bdnG"], writes=[("ps", ob)])
                P.dve(lambda e, tt=tt, dh=dh, ob=ob: e.tensor_tensor(out=X[:, tt, dh * 512:(dh + 1) * 512], in0=ps[ob][:],
                                                                     in1=X[:, tt, dh * 512:(dh + 1) * 512], op=ALU.add),
                      reads=[("ps", ob), ("X", tt)], writes=[("X", tt)])

        units = [(e_, hf) for e_ in range(n_exp) for hf in range(2)]
        NW = self.NW

        def load_unit(u):
            e_, hf = units[u]
            s = u % NW
            wu = self.w_up[l, e_].rearrange("(kc p) f -> p kc f", p=128)
            wd = self.w_down[l, e_].rearrange("(j p) d -> p j d", p=128)
            P.dma(lambda e: e.dma_start(out=self.w1g[s][:], in_=wu[:, :, hf * 512:(hf + 1) * 512]),
                  writes=[("w1g", s)], chan=f"wg{s}", eng="pool")
            P.dma(lambda e: e.dma_start(out=self.w1l[s][:], in_=wu[:, :, D + hf * 512:D + (hf + 1) * 512]),
                  writes=[("w1l", s)], chan=f"wl{s}", eng="pool")
            P.dma(lambda e: e.dma_start(out=self.w2h[s][:], in_=wd[:, hf * 4:(hf + 1) * 4, :]),
                  writes=[("w2h", s)], chan=f"wd{s}", eng="pool")

        def fold_unit(u):
            s = u % NW
            for j in range(4):
                P.pool(lambda e, j=j: e.tensor_tensor(out=self.w2h[s][:, j, :], in0=self.w2h[s][:, j, :], in1=self.G[:], op=ALU.mult),
                       reads=[("w2h", s), "G"], writes=[("w2h", s)])

        epi_ctr = [0]
        zb_ctr = [0]

        def step1(u, tg, aslot):
            e_, hf = units[u]
            s = u % NW
            for j in range(4):
                zb = zb_ctr[0] % 2
                zb_ctr[0] += 1
                bg, bl = zb * 2, zb * 2 + 1
                for kc in range(KC):
                    P.pe(lambda e, kc=kc, j=j, bg=bg: e.matmul(ps[bg][:], lhsT=self.w1g[s][:, kc, j * 128:(j + 1) * 128],
                                                               rhs=self.HT[:, kc, tg * 512:(tg + 1) * 512],
                                                               start=(kc == 0), stop=(kc == KC - 1)),
                         reads=[("w1g", s), ("HT", kc, tg)], writes=[("ps", bg)])
                for kc in range(KC):
                    P.pe(lambda e, kc=kc, j=j, bl=bl: e.matmul(ps[bl][:], lhsT=self.w1l[s][:, kc, j * 128:(j + 1) * 128],
                                                               rhs=self.HT[:, kc, tg * 512:(tg + 1) * 512],
                                                               start=(kc == 0), stop=(kc == KC - 1)),
                         reads=[("w1l", s), ("HT", kc, tg)], writes=[("ps", bl)])
                es = epi_ctr[0] % self.NEp
                epi_ctr[0] += 1
                A, B, Sg = self.eA[es], self.eB[es], self.eS[es]
                fg = hf * 4 + j
                P.dve(lambda e, A=A, bg=bg, fg=fg: e.tensor_scalar(out=A[:], in0=ps[bg][:], scalar1=self.bup[:, e_, fg:fg + 1], scalar2=7.0,
                                                                   op0=ALU.add, op1=ALU.min),
                      reads=[("ps", bg), "bup"], writes=[("eA", es)])
                P.dve(lambda e, B=B, bl=bl, fg=fg: e.tensor_scalar(out=B[:], in0=ps[bl][:], scalar1=self.bup[:, e_, 8 + fg:9 + fg], scalar2=8.0,
                                                                   op0=ALU.add, op1=ALU.min),
                      reads=[("ps", bl), "bup"], writes=[("eB", es)])
                P.act(lambda e, A=A, Sg=Sg: e.activation(out=Sg[:], in_=A[:], func=AF.Sigmoid, scale=1.702),
                      reads=[("eA", es)], writes=[("eS", es)])
                P.pool(lambda e, A=A, Sg=Sg: e.tensor_tensor(out=Sg[:], in0=A[:], in1=Sg[:], op=ALU.mult),
                       reads=[("eA", es), ("eS", es)], writes=[("eS", es)])
                P.dve(lambda e, B=B, Sg=Sg, j=j: e.scalar_tensor_tensor(out=self.actb[aslot][:, j, :], in0=B[:], scalar=-6.0, in1=Sg[:],
                                                                       op0=ALU.max, op1=ALU.mult),
                      reads=[("eB", es), ("eS", es)], writes=[("actb", aslot, j)])

        ob_ctr = [0]

        def step2(u, tg, aslot):
            e_, hf = units[u]
            s = u % NW
            for i in range(4):
                tt = tg * 4 + i
                for dh in range(2):
                    ob = 4 + (ob_ctr[0] % 4)
                    ob_ctr[0] += 1
                    for j in range(4):
                        P.pe(lambda e, j=j, i=i, dh=dh, ob=ob: e.matmul(ps[ob][:], lhsT=self.actb[aslot][:, j, i * 128:(i + 1) * 128],
                                                                        rhs=self.w2h[s][:, j, dh * 512:(dh + 1) * 512],
                                                                        start=(j == 0), stop=(j == 3)),
                             reads=[("actb", aslot, j), ("w2h", s)], writes=[("ps", ob)])
                    P.dve(lambda e, tt=tt, dh=dh, ob=ob: e.scalar_tensor_tensor(out=X[:, tt, dh * 512:(dh + 1) * 512], in0=ps[ob][:],
                                                                                scalar=self.comb[:, tt, e_:e_ + 1],
                                                                                in1=X[:, tt, dh * 512:(dh + 1) * 512],
                                                                                op0=ALU.mult, op1=ALU.add),
                          reads=[("ps", ob), ("comb", tt), ("X", tt)], writes=[("X", tt)])
                if u == len(units) - 1:
                    self.emit_ln(tt)

        work = [(u, tg) for u in range(len(units)) for tg in range(4)]
        load_unit(0)
        if len(units) > 1:
            load_unit(1)
        prev = None
        for wi, (u, tg) in enumerate(work):
            aslot = wi % self.NA
            if tg == 0:
                fold_unit(u)
            step1(u, tg, aslot)
            if prev is not None:
                pu, ptg, pas = prev
                step2(pu, ptg, pas)
                if ptg == 3 and pu + NW < len(units):
                    load_unit(pu + NW)
            prev = (u, tg, aslot)
        pu, ptg, pas = prev
        step2(pu, ptg, pas)
def build(cfg):
    B = Builder(cfg)
    B.declare_common()
    mixer_on = cfg.get("mixer", True)
    if mixer_on:
        B.declare_mixer()
        if cfg.get("s5", True):
            B.declare_s5()
    B.emit_consts()
    if mixer_on:
        B.arena_reset()
        B.emit_mixer_consts()
    B.load_x()
    for l in cfg.get("layers", range(DEPTH)):
        if mixer_on:
            B.arena_reset()
            B.emit_mod(l, 0)
            B.arena_reset()
            B.emit_hT(l, router=False)
            B.emit_mixer(l)
        if cfg.get("moe", True):
            st = cfg.get("stage", 99)
            B.arena_reset()
            if st >= 1:
                B.emit_mod(l, 1)
            B.arena_reset()
            B.declare_moe()
            B.emit_bup(l)
            if st >= 2:
                B.emit_hT(l, router=cfg.get("router", True))
            if st >= 3:
                B.emit_moe(l, n_exp=cfg.get("n_exp", NE))
    B.store_x()
    B.P.emit()
    return B


def host_inputs(inputs, b):
    f = lambda a: np.ascontiguousarray(np.asarray(a))
    m = {}
    m["x"] = f(inputs["x"][b])
    m["cT"] = f(np.asarray(inputs["c"][b]).reshape(KC, 128).T)
    m["ident"] = np.eye(128, dtype=np.float32)
    for k in ("ada_w", "ada_b", "ln_g", "ln_b", "w_router", "b_router", "w_up", "w_down", "b_down"):
        m[k] = f(inputs[k])
    m["b_upT"] = f(np.asarray(inputs["b_up"]).reshape(DEPTH, NE, 16, 128).transpose(3, 0, 1, 2))
    w_in = np.asarray(inputs["w_in"])
    m["pos"] = f(np.asarray(inputs["positions"][b]).astype(np.int32))
    p = np.arange(128)
    d = p % 64
    inv = (10000.0 ** (-(2.0 * (d % 32)) / 64.0)).astype(np.float32)
    sgn = np.where(d < 32, -1.0, 1.0).astype(np.float32)
    m["ropec"] = f(np.stack([inv, sgn], axis=1))
    perm = np.arange(512).reshape(8, 64)
    perm = np.concatenate([perm[:, 32:], perm[:, :32]], axis=1).reshape(512)
    q = w_in[:, :, 0:512]; k = w_in[:, :, 512:1024]
    def chunked(w, width):
        L_, D_, n_ = w.shape
        return f(w.reshape(L_, KC, 128, n_ // width, width).transpose(0, 3, 2, 1, 4))
    qs = q[:, :, perm]; ks = k[:, :, perm]
    qk = np.stack([np.concatenate([q.reshape(DEPTH, D, 4, 128), qs.reshape(DEPTH, D, 4, 128)], axis=3),
                   np.concatenate([k.reshape(DEPTH, D, 4, 128), ks.reshape(DEPTH, D, 4, 128)], axis=3)], axis=2)
    m["w_qk"] = chunked(qk.reshape(DEPTH, D, 2048), 256)
    m["w_v"] = chunked(w_in[:, :, 1024:1536], 256)
    m["w_mlx"] = chunked(w_in[:, :, 1536:1792], 128)
    m["w_mlvo"] = chunked(w_in[:, :, 1792:2304], 256)
    m["w_mli"] = f(w_in[:, :, 2304:2308])
    m["w_mlf"] = f(w_in[:, :, 2308:2312])
    m["tri"] = f(np.triu(np.ones((128, 128), np.float32)))
    m["lamv"] = f(np.concatenate([np.asarray(inputs[k_]) for k_ in ("lam_q1", "lam_k1", "lam_q2", "lam_k2")], axis=1))
    m["da_g"] = f(inputs["da_norm_g"])
    cw = np.asarray(inputs["ml_conv_w"])
    m["convw"] = f(cw.reshape(DEPTH, 4, 2, 128).transpose(3, 0, 2, 1))
    m["convb"] = f(np.asarray(inputs["ml_conv_b"]).reshape(DEPTH, 2, 128).transpose(2, 0, 1))
    for nm, src in (("wq_bd", "ml_w_q"), ("wk_bd", "ml_w_k")):
        w = np.asarray(inputs[src])
        bd = np.zeros((DEPTH, 2, 128, 128), np.float32)
        for hc in range(2):
            for hl in range(2):
                bd[:, hc, hl * 64:(hl + 1) * 64, hl * 64:(hl + 1) * 64] = w[:, hc * 2 + hl]
        m[nm] = bd
    gbv = np.asarray(inputs["ml_gate_b"])
    m["gate_bi"] = f(gbv[:, 0:4].reshape(DEPTH, 4, 1))
    m["gate_bf"] = f(gbv[:, 4:8].reshape(DEPTH, 4, 1))
    sel = np.zeros((4, 4, 128), np.float32)
    for h in range(4):
        sel[h, h, :] = 1.0
    m["sel4"] = sel
    m["ml_g"] = f(inputs["ml_norm_g"])
    m["w_out"] = f(inputs["w_out"])
    a_re = np.asarray(inputs["s5_a_re"]); a_im = np.asarray(inputs["s5_a_im"]); ldt = np.asarray(inputs["s5_log_dt"])
    G_, P_, H_ = 16, 64, 16
    ldt_b = np.broadcast_to(ldt[:, :, None], (DEPTH, G_, P_))
    A = np.stack([a_re, a_im, ldt_b], axis=-1)
    m["s5A"] = f(A.reshape(DEPTH, 8, 2, P_, 3).transpose(2, 3, 0, 1, 4).reshape(128, DEPTH, 8, 3))
    A2 = np.broadcast_to(A[:, :, None, :, :], (DEPTH, G_, H_, P_, 3))
    m["s5A2"] = f(A2.reshape(DEPTH, 2, 8, H_, P_, 3).transpose(2, 3, 0, 1, 4, 5).reshape(128, DEPTH, 2, P_, 3))
    bb = np.stack([np.asarray(inputs["s5_b_re"]), np.asarray(inputs["s5_b_im"])], axis=1)
    m["s5bT"] = f(bb.reshape(DEPTH, 2, 2, 8, P_, H_).transpose(3, 5, 0, 1, 2, 4).reshape(128, DEPTH, 2, 2, P_))
    cc = np.stack([np.asarray(inputs["s5_c_re"]), np.asarray(inputs["s5_c_im"])], axis=1)
    m["s5cT"] = f(cc.reshape(DEPTH, 2, 8, 2, H_, P_).transpose(3, 5, 0, 1, 2, 4).reshape(128, DEPTH, 2, 8, H_))
    row = np.arange(128)
    bmask = np.zeros((128, 8), np.float32)
    for scl in range(4):
        for s2 in range(2):
            bmask[:, scl * 2 + s2] = ((row // 16) == 2 * scl + s2)
    m["s5bm"] = bmask
    cmask = np.zeros((128, 4, 8), np.float32)
    for scl in range(4):
        for gl in range(8):
            cmask[:, scl, gl] = (gl == 2 * scl + (row // 64))
    m["s5cm"] = cmask
    m["s5d"] = f(np.asarray(inputs["s5_d"]).reshape(DEPTH, 2, 128).transpose(2, 0, 1))
    wgl = np.asarray(inputs["s5_w_glu"])
    wbd = np.zeros((DEPTH, 2, 2, 128, 128), np.float32)
    for yc in range(2):
        for gl in range(8):
            g = yc * 8 + gl
            for hf in range(2):
                wbd[:, yc, hf, gl * 16:(gl + 1) * 16, gl * 16:(gl + 1) * 16] = wgl[:, g, :, hf * 16:(hf + 1) * 16]
    m["s5wg"] = wbd
    m["w_s5u"] = chunked(w_in[:, :, 2312:2568], 128)
    m["jidx"] = f(np.broadcast_to(np.arange(512, dtype=np.float32)[None, :], (128, 512)))
    return m


_CACHE = {}


def kernel(**inputs):
    cfg = {}
    if "B" not in _CACHE:
        _CACHE["B"] = build(cfg)
    B = _CACHE["B"]
    in_maps = []
    for b in range(8):
        m = host_inputs(inputs, b)
        in_maps.append({k: m[k] for k in B.din})
    res = run_bass_kernel_spmd(B.nc, in_maps, core_ids=list(range(8)))
    out = np.stack([res.results[b]["out"] for b in range(8)], axis=0)
    return out.astype(np.float32)


def _declare_mixer(self):
    self.pos_in = self.inp("pos", [S], I32)
    self.ropec_in = self.inp("ropec", [128, 2])
    self.w_qk = self.inp("w_qk", [DEPTH, 8, 128, KC, 256])
    self.w_v = self.inp("w_v", [DEPTH, 2, 128, KC, 256])
    self.w_mlx = self.inp("w_mlx", [DEPTH, 2, 128, KC, 128])
    self.w_mlvo = self.inp("w_mlvo", [DEPTH, 2, 128, KC, 256])
    self.w_mli = self.inp("w_mli", [DEPTH, D, 4])
    self.w_mlf = self.inp("w_mlf", [DEPTH, D, 4])
    self.tri_in = self.inp("tri", [128, 128])
    self.lamv_in = self.inp("lamv", [DEPTH, 256])
    self.da_g_in = self.inp("da_g", [DEPTH, 128])
    self.convw_in = self.inp("convw", [128, DEPTH, 2, 4])
    self.convb_in = self.inp("convb", [128, DEPTH, 2])
    self.wq_bd = self.inp("wq_bd", [DEPTH, 2, 128, 128])
    self.wk_bd = self.inp("wk_bd", [DEPTH, 2, 128, 128])
    self.gbi_in = self.inp("gate_bi", [DEPTH, 4, 1])
    self.gbf_in = self.inp("gate_bf", [DEPTH, 4, 1])
    self.sel4_in = self.inp("sel4", [4, 4, 128])
    self.ml_g_in = self.inp("ml_g", [DEPTH, 256])
    self.w_out = self.inp("w_out", [DEPTH, D, D])
    P = self.P
    self.trib = P.sbuf("trib", [128, 128], BF16)
    self.ropec = P.sbuf("ropec_s", [128, 2], F32)
    self.one1 = P.sbuf("one1", [128, 1], F32)


def _emit_mixer_consts(self):
    P = self.P
    tmp = self.carve([128, 128], F32)
    P.dma(lambda e: e.dma_start(out=tmp[:], in_=self.tri_in[:, :]), writes=["tritmp"], chan="c0")
    P.dve(lambda e: e.tensor_copy(out=self.trib[:], in_=tmp[:]), reads=["tritmp"], writes=["trib"])
    P.dma(lambda e: e.dma_start(out=self.ropec[:], in_=self.ropec_in[:, :]), writes=["ropec"], chan="c0")
    P.dve(lambda e: e.memset(self.one1[:], 1.0), writes=["one1"])


def _proj_fm(self, wsrc, ncol, consume, tag):
    P, ps = self.P, self.ps
    nchunk = ncol // 128
    for c in range(nchunk):
        s = c % 2
        wt = self.wst[s]
        P.dma(lambda e, c=c, wt=wt: e.dma_start(out=wt[:, :, 0:128], in_=wsrc[c]),
              writes=[("wst", s)], chan=f"wst{s}", eng="pool")
        for tg in range(4):
            bank = (c * 4 + tg) % 2
            for kc in range(KC):
                P.pe(lambda e, kc=kc, tg=tg, bank=bank, wt=wt: e.matmul(ps[bank][:], lhsT=wt[:, kc, 0:128], rhs=self.HT[:, kc, tg * 512:(tg + 1) * 512],
                                                                      start=(kc == 0), stop=(kc == KC - 1)),
                     reads=[("wst", s), ("HT", kc, tg)], writes=[("ps", bank)])
            consume(c, tg, bank)


def _proj_tm(self, wsrc, ncol, consume):
    P, ps = self.P, self.ps
    wt = self.wst[0]
    P.dma(lambda e: e.dma_start(out=wt[:, :, 0:ncol], in_=wsrc), writes=[("wst", 0)], chan="wst0", eng="pool")
    for tt in range(NT):
        bank = 2 + (tt % 2)
        for kc in range(KC):
            P.pe(lambda e, kc=kc, tt=tt, bank=bank: e.matmul(ps[bank][:, 0:ncol], lhsT=self.HT[:, kc, tt * 128:(tt + 1) * 128], rhs=wt[:, kc, 0:ncol],
                                                           start=(kc == 0), stop=(kc == KC - 1)),
                 reads=[("wst", 0)] + [("HT", kc, tt // 4)], writes=[("ps", bank)])
        consume(tt, bank)


def _blocks_flush(self, n_keep=0):
    pend = self.__dict__.setdefault("_bpend", [])
    while len(pend) > n_keep:
        fn = pend.pop(0)
        fn()


def _blocks(self, name, qt, kT, qT, vaug, vw, out_cb, scale=1.0, dmask=None):
    P, ps = self.P, self.ps
    pt = self.pt
    BP = 2
    SB = (0, 1, 4)
    OB = (5, 6, 7)
    cnt = self.__dict__.setdefault("_bcnt", [0, 0])
    pend = self.__dict__.setdefault("_bpend", [])
    ob = OB[cnt[1] % 3]
    cnt[1] += 1
    nk = qt + 1
    for g0 in range(0, nk, 4):
        n = min(4, nk - g0)
        sb = SB[cnt[0] % 3]
        sl = cnt[0] % 3
        cnt[0] += 1
        for i in range(n):
            kt = g0 + i
            P.pe(lambda e, i=i, kt=kt, sb=sb: e.matmul(ps[sb][:, i * 128:(i + 1) * 128], lhsT=kT(kt), rhs=qT(qt), start=True, stop=True),
                 reads=[(name, "kq")], writes=[("ps", sb)])
        if dmask is None:
            P.act(lambda e, n=n, sb=sb, sl=sl: e.activation(out=pt[sl][:, 0:n * 128], in_=ps[sb][:, 0:n * 128], func=AF.Exp, scale=scale),
                  reads=[("ps", sb)], writes=[("pt", sl)])
        else:
            dsl = cnt[0] % 2
            for i in range(n):
                fr, bi = dmask(g0 + i)
                P.act(lambda e, i=i, fr=fr, bi=bi, dsl=dsl: e.activation(out=self.dt[dsl][:, i * 128:(i + 1) * 128], in_=fr, func=AF.Exp, bias=bi, scale=1.0),
                      reads=[(name, "dm")], writes=[("dt", dsl)])
            P.dve(lambda e, n=n, sb=sb, sl=sl, dsl=dsl: e.tensor_tensor(out=pt[sl][:, 0:n * 128], in0=ps[sb][:, 0:n * 128], in1=self.dt[dsl][:, 0:n * 128], op=ALU.mult),
                  reads=[("ps", sb), ("dt", dsl)], writes=[("pt", sl)])
        if g0 + n - 1 == qt:
            i = n - 1
            P.pool(lambda e, i=i, sl=sl: e.tensor_tensor(out=pt[sl][:, i * 128:(i + 1) * 128], in0=pt[sl][:, i * 128:(i + 1) * 128], in1=self.trib[:], op=ALU.mult),
                   reads=[("pt", sl), "trib"], writes=[("pt", sl)])

        def pv(n=n, g0=g0, sl=sl, ob=ob, last=(g0 + n - 1 == qt)):
            for i in range(n):
                kt = g0 + i
                P.pe(lambda e, i=i, kt=kt: e.matmul(ps[ob][:, 0:vw], lhsT=pt[sl][:, i * 128:(i + 1) * 128], rhs=vaug(kt),
                                                    start=(kt == 0), stop=(kt == qt)),
                     reads=[("pt", sl), (name, "v")], writes=[("ps", ob)])
            if last:
                out_cb(qt, ob)
        pend.append(pv)
        _blocks_flush(self, BP)


def _emit_mlstm(self, l, R1):
    P, ps = self.P, self.ps
    self.wst = [self.carve([128, KC, 256], BF16), self.carve([128, KC, 128], BF16)]
    self.pt = [self.carve([128, 512], BF16) for _ in range(3)]
    self.dt = [self.carve([128, 512], F32) for _ in range(2)]
    xm = self.carve([128, 2, S + 4], BF16)
    xc = self.carve([128, 2, S], BF16)
    QTm = self.carve([128, S], BF16)
    KTm = self.carve([128, S], BF16)
    vml = self.carve([128, NT, 4, 66], BF16)
    sgo = self.carve([128, NT, 256], BF16)
    FT = self.carve([4, S], F32)
    Frow = self.carve([128, S], F32)
    acc = Frow
    fT = Frow[0:4, :]
    iT = xm[0:4, :, :].rearrange("p a b -> p (a b)").bitcast(F32)[:, 0:S]
    biasS = self.carve([128, NT, 4], F32)
    cw = self.carve([128, 2, 4], F32)
    cb = self.carve([128, 2], F32)
    wqb = self.carve([128, 2, 128], BF16)
    wkb = self.carve([128, 2, 128], BF16)
    wtmp = self.carve([128, 2, 128], F32)
    gb = self.carve([4, 2], F32)
    sel4 = self.carve([4, 4, 128], F32)
    mlg = self.carve([128, 256], F32)
    sm16 = self.carve([128, 2, NT], F32)
    Ost = xm[:, :, :].rearrange("p a b -> p (a b)").bitcast(F32)[:, 0:NT * 65].rearrange("p (q c) -> p q c", c=65)
    htmp = self.wst[0][:, :, :].rearrange("p a b -> p (a b)").bitcast(F32)[:, 0:NT * 64].rearrange("p (q c) -> p q c", c=64)

    P.dma(lambda e: e.dma_start(out=cw[:], in_=self.convw_in[:, l]), writes=["cw"], chan="c0")
    P.dma(lambda e: e.dma_start(out=cb[:], in_=self.convb_in[:, l]), writes=["cb"], chan="c0")
    P.dma(lambda e: e.dma_start(out=gb[:, 0:1], in_=self.gbi_in[l]), writes=["gb"], chan="c0")
    P.dma(lambda e: e.dma_start(out=gb[:, 1:2], in_=self.gbf_in[l]), writes=["gb"], chan="c0")
    P.dma(lambda e: e.dma_start(out=sel4[:], in_=self.sel4_in[:, :, :]), writes=["sel4"], chan="c0")
    P.dma(lambda e: e.dma_start(out=mlg[:], in_=self.ml_g_in[l].partition_broadcast(128)), writes=["mlg"], chan="c0")
    for (src, dst, nm) in ((self.wq_bd, wqb, "wqb"), (self.wk_bd, wkb, "wkb")):
        P.dma(lambda e, src=src: e.dma_start(out=wtmp[:], in_=src[l].rearrange("c p e -> p c e")), writes=["wtmp"], chan="c0")
        P.dve(lambda e, dst=dst: e.tensor_copy(out=dst[:], in_=wtmp[:]), reads=["wtmp"], writes=[nm])
    P.dve(lambda e: e.memset(xm[:, :, 0:4], 0.0), writes=["xm"])
    P.dve(lambda e: e.memset(vml[:], 1.0), writes=[("ml", "v")])

    def cons_x(c, tg, bank):
        P.act(lambda e: e.activation(out=xm[:, c, 4 + tg * 512:4 + (tg + 1) * 512], in_=ps[bank][:], func=AF.Identity),
              reads=[("ps", bank)], writes=["xm"])
    _proj_fm(self, self.w_mlx[l], 256, cons_x, "mlx")
    for hc in range(2):
        P.dve(lambda e, hc=hc: e.tensor_scalar(out=acc[:], in0=xm[:, hc, 1:1 + S], scalar1=cw[:, hc, 0:1], scalar2=None, op0=ALU.mult),
              reads=["xm", "cw"], writes=["acc"])
        for j in range(1, 4):
            P.dve(lambda e, hc=hc, j=j: e.scalar_tensor_tensor(out=acc[:], in0=xm[:, hc, 1 + j:1 + j + S], scalar=cw[:, hc, j:j + 1], in1=acc[:],
                                                               op0=ALU.mult, op1=ALU.add),
                  reads=["xm", "cw", "acc"], writes=["acc"])
        P.act(lambda e, hc=hc: e.activation(out=xc[:, hc, :], in_=acc[:], func=AF.Silu, bias=cb[:, hc:hc + 1], scale=1.0),
              reads=["acc", "cb"], writes=["xc"])

    def cons_v2(tt, bank):
        P.act(lambda e: e.activation(out=vml[:, tt, :, 0:64], in_=ps[bank][:, 0:256].rearrange("p (h d) -> p h d", d=64), func=AF.Identity),
              reads=[("ps", bank)], writes=[("ml", "v")])

    def cons_o2(tt, bank):
        P.act(lambda e: e.activation(out=sgo[:, tt, :], in_=ps[bank][:, 0:256], func=AF.Sigmoid),
              reads=[("ps", bank)], writes=["sgo"])
    _proj_tm(self, self.w_mlvo[l, 0], 256, cons_v2)
    _proj_tm(self, self.w_mlvo[l, 1], 256, cons_o2)
    for (wsrc, dstT, nm, alias) in ((self.w_mli, iT, "iT", "xm"), (self.w_mlf, fT, "fT", "acc")):
        wt = self.wst[1]
        P.dma(lambda e, wsrc=wsrc, wt=wt: e.dma_start(out=wt[:, :, 0:4], in_=wsrc[l].rearrange("(kc p) f -> p kc f", p=128)),
              writes=[("wst", 1)], chan="wst1", eng="pool")
        for tg in range(4):
            bank = tg % 2
            for kc in range(KC):
                P.pe(lambda e, kc=kc, tg=tg, bank=bank, wt=wt: e.matmul(ps[bank][0:4, :], lhsT=wt[:, kc, 0:4], rhs=self.HT[:, kc, tg * 512:(tg + 1) * 512],
                                                                      start=(kc == 0), stop=(kc == KC - 1)),
                     reads=[("wst", 1), ("HT", kc, tg)], writes=[("ps", bank)])
            P.act(lambda e, tg=tg, bank=bank, dstT=dstT: e.activation(out=dstT[:, tg * 512:(tg + 1) * 512], in_=ps[bank][0:4, :], func=AF.Identity),
                  reads=[("ps", bank)], writes=[nm, alias])
    P.dve(lambda e: e.tensor_scalar(out=iT, in0=iT, scalar1=gb[:, 0:1], scalar2=None, op0=ALU.add), reads=["iT", "gb"], writes=["iT"])
    P.dve(lambda e: e.tensor_scalar(out=fT, in0=fT, scalar1=gb[:, 1:2], scalar2=-1.0, op0=ALU.add, op1=ALU.mult), reads=["fT", "gb"], writes=["fT"])
    P.act(lambda e: e.activation(out=fT, in_=fT, func=AF.Exp), reads=["fT"], writes=["fT"])
    P.act(lambda e: e.activation(out=fT, in_=fT, func=AF.Ln, bias=self.one1[0:4, 0:1], scale=1.0), reads=["fT", "one1"], writes=["fT"])
    P.dve(lambda e: e.tensor_scalar(out=fT, in0=fT, scalar1=-1.0, scalar2=None, op0=ALU.mult), reads=["fT"], writes=["fT"])
    ones4 = self.dt[0][0:4, :]
    P.dve(lambda e: e.memset(ones4, 1.0), writes=[("dt", 0)])
    for c4 in range(4):
        init = 0.0 if c4 == 0 else FT[:, c4 * 512 - 1:c4 * 512]
        P.dve(lambda e, c4=c4, init=init: e.tensor_tensor_scan(out=FT[:, c4 * 512:(c4 + 1) * 512], data0=ones4, data1=fT[:, c4 * 512:(c4 + 1) * 512],
                                                               initial=init, op0=ALU.mult, op1=ALU.add),
              reads=["fT", ("dt", 0), "FT"], writes=["FT"])
    P.dve(lambda e: e.tensor_tensor(out=iT, in0=iT, in1=FT[:], op=ALU.subtract), reads=["iT", "FT"], writes=["iT"])
    for tt in range(NT):
        P.pe(lambda e, tt=tt: e.transpose(out=ps[2][:, tt * 4:(tt + 1) * 4], in_=iT[:, tt * 128:(tt + 1) * 128], identity=self.ident[0:4, 0:4]),
             reads=["iT", "ident"], writes=[("ps", 2)])
    P.dve(lambda e: e.tensor_copy(out=biasS[:].rearrange("p a b -> p (a b)"), in_=ps[2][:, 0:64]), reads=[("ps", 2)], writes=["biasS"])

    for g in range(4):
        hc, hl = g // 2, g % 2
        if hl == 0:
            for tg in range(4):
                for (wb, dstT, sc, nm) in ((wqb, QTm, 1.0, "wqb"), (wkb, KTm, 0.125, "wkb")):
                    bank = (tg % 2)
                    P.pe(lambda e, hc=hc, tg=tg, wb=wb, bank=bank: e.matmul(ps[bank][:], lhsT=wb[:, hc, :], rhs=xc[:, hc, tg * 512:(tg + 1) * 512], start=True, stop=True),
                         reads=["xc", nm], writes=[("ps", bank)])
                    P.act(lambda e, tg=tg, dstT=dstT, sc=sc, bank=bank: e.activation(out=dstT[:, tg * 512:(tg + 1) * 512], in_=ps[bank][:], func=AF.Copy, scale=sc),
                          reads=[("ps", bank)], writes=[("ml", "kq")])
        for tg in range(4):
            bank = 2 + tg % 2
            P.pe(lambda e, tg=tg, bank=bank, g=g: e.matmul(ps[bank][:], lhsT=sel4[:, g, :], rhs=FT[:, tg * 512:(tg + 1) * 512], start=True, stop=True),
                 reads=["FT", "sel4"], writes=[("ps", bank)])
            P.act(lambda e, tg=tg, bank=bank: e.activation(out=Frow[:, tg * 512:(tg + 1) * 512], in_=ps[bank][:], func=AF.Identity),
                  reads=[("ps", bank), "fT", "acc"], writes=[("ml", "dm"), "acc", "fT"])

        def out_cb(qt, ob, g=g):
            P.dve(lambda e: e.tensor_copy(out=Ost[:, qt, :], in_=ps[ob][:, 0:65]), reads=[("ps", ob), "xm", "iT"], writes=["Ost", "xm"])

        for qt in range(NT):
            _blocks(self, "ml", qt,
                    kT=lambda kt, hl=hl: KTm[hl * 64:(hl + 1) * 64, kt * 128:(kt + 1) * 128],
                    qT=lambda qt_, hl=hl: QTm[hl * 64:(hl + 1) * 64, qt_ * 128:(qt_ + 1) * 128],
                    vaug=lambda kt, g=g: vml[:, kt, g, 0:65], vw=65, out_cb=out_cb,
                    dmask=lambda kt, qt=qt, g=g: (Frow[:, qt * 128:(qt + 1) * 128], biasS[:, kt, g:g + 1]))
        _blocks_flush(self)
        num = Ost[:, :, 0:64]
        b16 = lambda ap: ap.unsqueeze(2).to_broadcast([128, NT, 64])
        EK = dict(reads=["Ost", "htmp", "sm16", "mlg", "sgo", "eps", ("wst", 0)], writes=["Ost", "htmp", "sm16", ("wst", 0)])
        P.act(lambda e: e.activation(out=sm16[:, 0, :], in_=Ost[:, :, 64], func=AF.Abs), **EK)
        P.dve(lambda e: e.tensor_scalar(out=sm16[:, 0, :], in0=sm16[:, 0, :], scalar1=1.0, scalar2=None, op0=ALU.max), **EK)
        P.dve(lambda e: e.reciprocal(out=sm16[:, 0, :], in_=sm16[:, 0, :]), **EK)
        P.dve(lambda e: e.tensor_tensor(out=htmp[:], in0=num, in1=b16(sm16[:, 0, :]), op=ALU.mult), **EK)
        P.dve(lambda e: e.tensor_tensor(out=num, in0=htmp[:], in1=htmp[:], op=ALU.mult), **EK)
        P.dve(lambda e: e.tensor_reduce(out=sm16[:, 1, :], in_=num, axis=AX.X, op=ALU.add), **EK)
        P.act(lambda e: e.activation(out=sm16[:, 1, :], in_=sm16[:, 1, :], func=AF.Sqrt, bias=self.eps[:, 0:1], scale=1.0 / 64.0), **EK)
        P.dve(lambda e: e.reciprocal(out=sm16[:, 1, :], in_=sm16[:, 1, :]), **EK)
        P.dve(lambda e: e.tensor_tensor(out=htmp[:], in0=htmp[:], in1=b16(sm16[:, 1, :]), op=ALU.mult), **EK)
        P.dve(lambda e, g=g: e.tensor_tensor(out=htmp[:], in0=htmp[:], in1=mlg[:, g * 64:(g + 1) * 64].unsqueeze(1).to_broadcast([128, NT, 64]), op=ALU.mult), **EK)
        P.dve(lambda e, g=g: e.tensor_tensor(out=sgo[:, :, g * 64:(g + 1) * 64], in0=htmp[:], in1=sgo[:, :, g * 64:(g + 1) * 64], op=ALU.mult),
              reads=["htmp", "sgo"], writes=["sgo"])
    for qt in range(NT):
        for hc in range(2):
            tb = 2 + ((qt * 2 + hc) % 2)
            psb = ps[tb][:].bitcast(BF16)
            P.pe(lambda e, qt=qt, hc=hc, psb=psb: e.transpose(out=psb[:, 0:128], in_=sgo[:, qt, hc * 128:(hc + 1) * 128], identity=self.identb[:]),
                 reads=["sgo", "identb"], writes=[("ps", tb)])
            P.act(lambda e, qt=qt, hc=hc, psb=psb: e.activation(out=R1[:, hc, qt * 128:(qt + 1) * 128], in_=psb[:, 0:128], func=AF.Identity),
                  reads=[("ps", tb)], writes=[("R1", hc)])


def _emit_da(self, l, R1):
    P, ps = self.P, self.ps
    lam_init = 0.8 - 0.6 * math.exp(-0.3 * l)
    wst2 = self.carve([128, 2 * KC * 256], BF16)
    self.wst = [wst2[:, i * KC * 256:(i + 1) * KC * 256].rearrange("p (k f) -> p k f", f=256) for i in range(2)]
    odah = wst2.bitcast(F32)[:, 0:NT * 128].rearrange("p (q c) -> p q c", c=128)
    odbh = wst2[:, 0:NT * 128].rearrange("p (q c) -> p q c", c=128)
    self.pt = [self.carve([128, 512], BF16) for _ in range(3)]
    QT = self.carve([128, 4, S], BF16)
    KT = self.carve([128, 4, S], BF16)
    Va = self.carve([128, NT, 4, 130], BF16)
    t1 = self.carve([128, 512], F32)
    sq5 = self.carve([128, 5 * 512], F32)
    cosT, sinT, posf, t2, twopi = [sq5[:, i * 512:(i + 1) * 512] for i in range(5)]
    posi = t2.bitcast(I32)
    sqh = sq5[:, 0:NT * 128].rearrange("p (q c) -> p q c", c=128)
    lamt = self.carve([128, 256], F32)
    lam = self.carve([128, 8], F32)
    dag = self.carve([128, 128], F32)
    sq = self.carve([128, 128], F32)
    sd = self.carve([128, 8], F32)
    sd16 = self.carve([128, NT], F32)
    YA = self.HT
    self.da_t1 = t1

    P.dve(lambda e: e.memset(Va[:], 1.0), writes=[("da", "v")])
    P.dma(lambda e: e.dma_start(out=lamt[:], in_=self.lamv_in[l].partition_broadcast(128)), writes=["lamt"], chan="c0")
    P.dma(lambda e: e.dma_start(out=dag[:], in_=self.da_g_in[l].partition_broadcast(128)), writes=["dag"], chan="c0")
    for i in range(2):
        P.dve(lambda e, i=i: e.tensor_tensor(out=sq[:, 0:64], in0=lamt[:, i * 128:i * 128 + 64], in1=lamt[:, i * 128 + 64:i * 128 + 128], op=ALU.mult),
              reads=["lamt"], writes=["sq"])
        P.dve(lambda e, i=i: e.tensor_reduce(out=lam[:, i:i + 1], in_=sq[:, 0:64], axis=AX.X, op=ALU.add), reads=["sq"], writes=["lam"])
    P.act(lambda e: e.activation(out=lam[:, 0:2], in_=lam[:, 0:2], func=AF.Exp), reads=["lam"], writes=["lam"])
    P.dve(lambda e: e.tensor_tensor(out=lam[:, 2:3], in0=lam[:, 1:2], in1=lam[:, 0:1], op=ALU.subtract), reads=["lam"], writes=["lam"])
    P.dve(lambda e: e.tensor_scalar(out=lam[:, 2:3], in0=lam[:, 2:3], scalar1=-lam_init, scalar2=None, op0=ALU.add), reads=["lam"], writes=["lam"])
    P.dve(lambda e: e.tensor_scalar(out=dag[:], in0=dag[:], scalar1=float(1.0 - lam_init), scalar2=None, op0=ALU.mult), reads=["dag"], writes=["dag"])

    for tg in range(4):
        P.dma(lambda e, tg=tg: e.dma_start(out=posi, in_=self.pos_in[tg * 512:(tg + 1) * 512].partition_broadcast(128)),
              writes=["t2"], chan="c0")
        P.dve(lambda e: e.tensor_copy(out=posf[:], in_=posi), reads=["t2"], writes=["posf"])
        TWO_PI = 2.0 * math.pi
        for (dst, sh_, nm) in ((sinT, 0.0, "sinT"), (cosT, 0.5 * math.pi, "cosT")):
            P.dve(lambda e, sh_=sh_: e.tensor_scalar(out=t1[:], in0=posf[:], scalar1=self.ropec[:, 0:1], scalar2=sh_, op0=ALU.mult, op1=ALU.add),
                  reads=["posf", "ropec"], writes=["t1"])
            P.dve(lambda e: e.tensor_scalar(out=posi, in0=t1[:], scalar1=1.0 / TWO_PI, scalar2=None, op0=ALU.mult), reads=["t1"], writes=["t2"])
            P.dve(lambda e: e.tensor_copy(out=twopi[:], in_=posi), reads=["t2"], writes=["twopi"])
            P.dve(lambda e: e.scalar_tensor_tensor(out=t1[:], in0=twopi[:], scalar=-TWO_PI, in1=t1[:], op0=ALU.mult, op1=ALU.add),
                  reads=["twopi", "t1"], writes=["t1"])
            P.dve(lambda e: e.tensor_scalar(out=twopi[:], in0=t1[:], scalar1=math.pi, scalar2=-TWO_PI, op0=ALU.is_gt, op1=ALU.mult), reads=["t1"], writes=["twopi"])
            P.dve(lambda e: e.tensor_tensor(out=t1[:], in0=t1[:], in1=twopi[:], op=ALU.add), reads=["t1", "twopi"], writes=["t1"])
            P.dve(lambda e: e.tensor_scalar(out=twopi[:], in0=t1[:], scalar1=-math.pi, scalar2=TWO_PI, op0=ALU.is_lt, op1=ALU.mult), reads=["t1"], writes=["twopi"])
            P.dve(lambda e: e.tensor_tensor(out=t1[:], in0=t1[:], in1=twopi[:], op=ALU.add), reads=["t1", "twopi"], writes=["t1"])
            P.dve(lambda e: e.tensor_scalar(out=t1[:], in0=t1[:], scalar1=3.1415925, scalar2=-3.1415925, op0=ALU.min, op1=ALU.max), reads=["t1"], writes=["t1"])
            P.act(lambda e, dst=dst: e.activation(out=dst[:], in_=t1[:], func=AF.Sin), reads=["t1"], writes=[nm])
        P.dve(lambda e: e.tensor_scalar(out=sinT[:], in0=sinT[:], scalar1=self.ropec[:, 1:2], scalar2=None, op0=ALU.mult), reads=["sinT", "ropec"], writes=["sinT"])
        for c in range(8):
            s = c % 2
            wt = self.wst[s]
            P.dma(lambda e, wt=wt, c=c: e.dma_start(out=wt[:, :, 0:256], in_=self.w_qk[l, c]), writes=[("wst", s)], chan=f"wst{s}", eng="pool")
            b0, b1 = (0, 1) if s == 0 else (2, 3)
            ta_, tb_, ka, kb = (t1, t2, "t1", "t2") if s == 0 else (posf, twopi, "posf", "twopi")
            for kc in range(KC):
                P.pe(lambda e, kc=kc, wt=wt, tg=tg, b0=b0: e.matmul(ps[b0][:], lhsT=wt[:, kc, 0:128], rhs=self.HT[:, kc, tg * 512:(tg + 1) * 512], start=(kc == 0), stop=(kc == KC - 1)),
                     reads=[("wst", s), ("HT", kc, tg)], writes=[("ps", b0)])
            for kc in range(KC):
                P.pe(lambda e, kc=kc, wt=wt, tg=tg, b1=b1: e.matmul(ps[b1][:], lhsT=wt[:, kc, 128:256], rhs=self.HT[:, kc, tg * 512:(tg + 1) * 512], start=(kc == 0), stop=(kc == KC - 1)),
                     reads=[("wst", s), ("HT", kc, tg)], writes=[("ps", b1)])
            dstT = QT if c < 4 else KT
            P.dve(lambda e, ta_=ta_, b0=b0: e.tensor_tensor(out=ta_[:], in0=ps[b0][:], in1=cosT[:], op=ALU.mult), reads=[("ps", b0), "cosT"], writes=[ka])
            P.dve(lambda e, tb_=tb_, b1=b1: e.tensor_tensor(out=tb_[:], in0=ps[b1][:], in1=sinT[:], op=ALU.mult), reads=[("ps", b1), "sinT"], writes=[kb])
            P.pool(lambda e, dstT=dstT, c=c, tg=tg, ta_=ta_, tb_=tb_: e.tensor_tensor(out=dstT[:, c % 4, tg * 512:(tg + 1) * 512], in0=ta_[:], in1=tb_[:], op=ALU.add),
                   reads=[ka, kb], writes=[("da", "kq")])

    def cons_v(tt, bank):
        P.act(lambda e: e.activation(out=Va[:, tt, :, 0:128], in_=ps[bank][:, 0:256].rearrange("p (h d) -> p h d", d=128), func=AF.Identity),
              reads=[("ps", bank)], writes=[("da", "v")])

    def cons_v_b(tt, bank):
        P.act(lambda e: e.activation(out=Va[:, tt, 2:4, 0:128], in_=ps[bank][:, 0:256].rearrange("p (h d) -> p h d", d=128), func=AF.Identity),
              reads=[("ps", bank)], writes=[("da", "v")])

    def cons_v_a(tt, bank):
        P.act(lambda e: e.activation(out=Va[:, tt, 0:2, 0:128], in_=ps[bank][:, 0:256].rearrange("p (h d) -> p h d", d=128), func=AF.Identity),
              reads=[("ps", bank)], writes=[("da", "v")])
    _proj_tm(self, self.w_v[l, 0], 256, cons_v_a)
    _proj_tm(self, self.w_v[l, 1], 256, cons_v_b)
    self.P.barrier()

    def da_out(g, qt, ob):
        h, c = g // 2, g % 2
        if c == 0:
            P.dve(lambda e: e.reciprocal(out=sd[:, 0:1], in_=ps[ob][:, 128:129]), reads=[("ps", ob)], writes=["sd"])
            P.dve(lambda e: e.tensor_scalar(out=odah[:, qt, :], in0=ps[ob][:, 0:128], scalar1=sd[:, 0:1], scalar2=None, op0=ALU.mult),
                  reads=[("ps", ob), "sd", ("wst", 0), ("wst", 1)], writes=["odah", ("wst", 0), ("wst", 1)])
            return
        P.dve(lambda e: e.reciprocal(out=sd[:, 1:2], in_=ps[ob][:, 128:129]), reads=[("ps", ob)], writes=["sd1"])
        P.dve(lambda e: e.tensor_tensor(out=sd[:, 1:2], in0=sd[:, 1:2], in1=lam[:, 2:3], op=ALU.mult), reads=["sd1", "lam"], writes=["sd1"])
        P.dve(lambda e: e.scalar_tensor_tensor(out=odah[:, qt, :], in0=ps[ob][:, 0:128], scalar=sd[:, 1:2], in1=odah[:, qt, :], op0=ALU.mult, op1=ALU.add),
              reads=[("ps", ob), "sd1", "odah"], writes=["odah"])

    for h in range(4):
        for qt in range(NT):
            for c in range(2):
                _blocks(self, "da", qt,
                        kT=lambda kt, h=h, c=c: KT[c * 64:(c + 1) * 64, h, kt * 128:(kt + 1) * 128],
                        qT=lambda qt_, h=h, c=c: QT[c * 64:(c + 1) * 64, h, qt_ * 128:(qt_ + 1) * 128],
                        vaug=lambda kt, h=h: Va[:, kt, h, 0:129], vw=129,
                        out_cb=lambda qt_, ob, h=h, c=c: da_out(2 * h + c, qt_, ob), scale=0.125)
        _blocks_flush(self)
        bq = lambda ap: ap.unsqueeze(2).to_broadcast([128, NT, 128])
        DK = dict(reads=["odah", "sqh", "sd16", "dag", "eps", "cosT", "sinT", "posf", "t2", "twopi"], writes=["odah", "sqh", "sd16", "cosT", "sinT", "posf", "t2", "twopi"])
        P.dve(lambda e: e.tensor_tensor(out=sqh[:], in0=odah[:], in1=odah[:], op=ALU.mult), **DK)
        P.dve(lambda e: e.tensor_reduce(out=sd16[:], in_=sqh[:], axis=AX.X, op=ALU.add), **DK)
        P.act(lambda e: e.activation(out=sd16[:], in_=sd16[:], func=AF.Sqrt, bias=self.eps[:, 0:1], scale=1.0 / 128.0), **DK)
        P.dve(lambda e: e.reciprocal(out=sd16[:], in_=sd16[:]), **DK)
        P.dve(lambda e: e.tensor_tensor(out=sqh[:], in0=odah[:], in1=bq(sd16[:]), op=ALU.mult), **DK)
        P.dve(lambda e: e.tensor_tensor(out=odbh[:], in0=sqh[:], in1=dag[:].unsqueeze(1).to_broadcast([128, NT, 128]), op=ALU.mult), **DK)
        for q4 in range(4):
            tb = 2 + (q4 % 2)
            psb = ps[tb][:].bitcast(BF16)
            for i in range(4):
                qt = q4 * 4 + i
                P.pe(lambda e, qt=qt, i=i, psb=psb: e.transpose(out=psb[:, i * 128:(i + 1) * 128], in_=odbh[:, qt, :], identity=self.identb[:]),
                     reads=["odah", "identb"], writes=[("ps", tb)])
            P.act(lambda e, q4=q4, h=h, psb=psb: e.activation(out=YA[:, h, q4 * 512:(q4 + 1) * 512], in_=psb[:, 0:512], func=AF.Identity),
                  reads=[("ps", tb)], writes=[("YA", h)])
    return QT


def _emit_wout(self, l, R1, wmem):
    P, ps = self.P, self.ps
    X = self.X
    YA = self.HT
    t1 = self.da_t1
    wo = self.w_out[l].rearrange("(kc p) f -> p kc f", p=128)
    wob = [wmem[:, 0:2, :].rearrange("p a (k f) -> p (a k) f", f=512), wmem[:, 2:4, :].rearrange("p a (k f) -> p (a k) f", f=512)]
    for dh in range(2):
        P.dma(lambda e, dh=dh: e.dma_start(out=wob[dh], in_=wo[:, :, dh * 512:(dh + 1) * 512]), writes=[("wob", dh)], chan=f"wob{dh}", eng="pool")
    for tt in range(NT):
        for dh in range(2):
            bank = 4 + ((tt * 2 + dh) % 4)
            for kc in range(KC):
                src = YA[:, kc, tt * 128:(tt + 1) * 128] if kc < 4 else R1[:, kc - 4, tt * 128:(tt + 1) * 128]
                rk = ("YA", kc) if kc < 4 else ("R1", kc - 4)
                P.pe(lambda e, kc=kc, dh=dh, bank=bank, src=src: e.matmul(ps[bank][:], lhsT=src, rhs=wob[dh][:, kc, :], start=(kc == 0), stop=(kc == KC - 1)),
                     reads=[rk, ("wob", dh)], writes=[("ps", bank)])
            P.dve(lambda e, tt=tt, dh=dh, bank=bank: e.tensor_tensor(out=t1[:], in0=ps[bank][:], in1=self.G[:, dh * 512:(dh + 1) * 512], op=ALU.mult),
                  reads=[("ps", bank), "G"], writes=["t1"])
            P.dve(lambda e, tt=tt, dh=dh: e.scalar_tensor_tensor(out=X[:, tt, dh * 512:(dh + 1) * 512], in0=X[:, tt, dh * 512:(dh + 1) * 512], scalar=float(DN_ALPHA),
                                                                 in1=t1[:], op0=ALU.mult, op1=ALU.add),
                  reads=["t1", ("X", tt)], writes=[("X", tt)])
        self.emit_ln(tt)


def _emit_mixer(self, l):
    P = self.P
    self.arena_reset()
    R1 = self.carve([128, 4, S], BF16)
    P.dve(lambda e: e.memset(R1[:, 2:4, :], 0.0), writes=[("R1", 2), ("R1", 3)])
    mark = self.aoff
    _emit_mlstm(self, l, R1)
    if self.cfg.get("s5", True):
        P.barrier()
        self.aoff = mark
        self.emit_s5(l, R1)
    P.barrier()
    self.aoff = mark
    QT = _emit_da(self, l, R1)
    P.barrier()
    _emit_wout(self, l, R1, QT)


def _sin_reduce(self, P, ang, ki, kf, key, keys_extra=()):
    TWO_PI = 2.0 * math.pi
    rk = [key] + list(keys_extra)
    P.dve(lambda e: e.tensor_scalar(out=ki, in0=ang, scalar1=1.0 / TWO_PI, scalar2=None, op0=ALU.mult), reads=rk, writes=[key + "_ki"])
    P.dve(lambda e: e.tensor_copy(out=kf, in_=ki), reads=[key + "_ki"], writes=[key + "_kf"])
    P.dve(lambda e: e.scalar_tensor_tensor(out=ang, in0=kf, scalar=-TWO_PI, in1=ang, op0=ALU.mult, op1=ALU.add), reads=[key + "_kf", key], writes=[key])
    P.dve(lambda e: e.tensor_scalar(out=kf, in0=ang, scalar1=math.pi, scalar2=-TWO_PI, op0=ALU.is_gt, op1=ALU.mult), reads=[key], writes=[key + "_kf"])
    P.dve(lambda e: e.tensor_tensor(out=ang, in0=ang, in1=kf, op=ALU.add), reads=[key, key + "_kf"], writes=[key])
    P.dve(lambda e: e.tensor_scalar(out=kf, in0=ang, scalar1=-math.pi, scalar2=TWO_PI, op0=ALU.is_lt, op1=ALU.mult), reads=[key], writes=[key + "_kf"])
    P.dve(lambda e: e.tensor_tensor(out=ang, in0=ang, in1=kf, op=ALU.add), reads=[key, key + "_kf"], writes=[key])
    P.dve(lambda e: e.tensor_scalar(out=ang, in0=ang, scalar1=3.1415925, scalar2=-3.1415925, op0=ALU.min, op1=ALU.max), reads=[key], writes=[key])


def _declare_s5(self):
    self.s5A_in = self.inp("s5A", [128, DEPTH, 8, 3])
    self.s5A2_in = self.inp("s5A2", [128, DEPTH, 2, 64, 3])
    self.s5bT_in = self.inp("s5bT", [128, DEPTH, 2, 2, 64])
    self.s5cT_in = self.inp("s5cT", [128, DEPTH, 2, 8, 16])
    self.s5bm_in = self.inp("s5bm", [128, 8])
    self.s5cm_in = self.inp("s5cm", [128, 4, 8])
    self.s5d_in = self.inp("s5d", [128, DEPTH, 2])
    self.s5wg_in = self.inp("s5wg", [DEPTH, 2, 2, 128, 128])
    self.w_s5u = self.inp("w_s5u", [DEPTH, 2, 128, KC, 128])
    self.jidx_in = self.inp("jidx", [128, 512])


def _emit_s5(self, l, R1):
    P, ps = self.P, self.ps
    C3 = lambda shape: self.carve(shape, F32)
    self.wst = [self.carve([128, KC, 128], BF16) for _ in range(2)]
    uT = self.carve([128, 2, S], BF16)
    BDr = self.carve([128, 8, 128], BF16)
    BDi = self.carve([128, 8, 128], BF16)
    CBr = self.carve([128, 8, 128], BF16)
    CBi = self.carve([128, 8, 128], BF16)
    wg = self.carve([128, 4, 128], BF16)
    wgt = C3([128, 4, 128])
    A = C3([128, 8, 3])
    A2 = C3([128, 2, 64, 3])
    bT = C3([128, 2, 2, 64])
    cT = C3([128, 2, 8, 16])
    bm = C3([128, 8])
    cm = C3([128, 4, 8])
    dsk = C3([128, 2])
    jidx = C3([128, 512])
    rr = C3([128, 8])
    th = C3([128, 8])
    zst = C3([128, 8, 2])
    T = [C3([128, 128]) for _ in range(8)]
    Ti = C3([128, 128]).bitcast(I32)
    ur, ui, zr, zi, ta, tb2 = [C3([128, 512]) for _ in range(6)]
    cosB = C3([128, 4, 512])
    sinB = C3([128, 4, 512])
    c512 = C3([128, 8])
    s512 = C3([128, 8])
    ns512 = C3([128, 8])
    zin = C3([128, 8, 2])
    ztmp = C3([128, 8])
    ki = C3([128, 512]).bitcast(I32)
    rt = C3([128, 512])
    xr = self.carve([128, 512], BF16)
    xi = self.carve([128, 512], BF16)
    gyb = self.carve([128, 512], BF16)

    ld = lambda dst, src, nm: P.dma(lambda e: e.dma_start(out=dst, in_=src), writes=[nm], chan="c0")
    ld(A[:], self.s5A_in[:, l], "s5A")
    ld(A2[:], self.s5A2_in[:, l], "s5A2")
    ld(bT[:], self.s5bT_in[:, l], "s5bT")
    ld(cT[:], self.s5cT_in[:, l], "s5cT")
    ld(bm[:], self.s5bm_in[:, :], "s5bm")
    ld(cm[:], self.s5cm_in[:, :, :], "s5cm")
    ld(dsk[:], self.s5d_in[:, l], "s5d")
    ld(jidx[:], self.jidx_in[:, :], "jidx")
    for yc in range(2):
        for hf in range(2):
            ld(wgt[:, yc * 2 + hf, :], self.s5wg_in[l, yc, hf], "wgt")
    P.dve(lambda e: e.tensor_copy(out=wg[:], in_=wgt[:]), reads=["wgt"], writes=["wg"])
    P.dve(lambda e: e.memset(zst[:], 0.0), writes=["zst"])

    P.act(lambda e: e.activation(out=rr[:], in_=A[:, :, 2], func=AF.Exp), reads=["s5A"], writes=["dt8"])
    P.dve(lambda e: e.tensor_tensor(out=th[:], in0=A[:, :, 1], in1=rr[:], op=ALU.mult), reads=["s5A", "dt8"], writes=["th"])
    P.dve(lambda e: e.tensor_tensor(out=rr[:], in0=A[:, :, 0], in1=rr[:], op=ALU.mult), reads=["s5A", "dt8", "th"], writes=["dt8"])
    P.act(lambda e: e.activation(out=rr[:], in_=rr[:], func=AF.Exp), reads=["dt8"], writes=["rr"])

    CK = "c512k"
    a8 = T[0][:, 0:8]; a8b = T[1][:, 0:8]; k8 = Ti[:, 0:8]; kf8 = T[2][:, 0:8]
    P.dve(lambda e: e.tensor_scalar(out=a8, in0=th[:], scalar1=512.0, scalar2=None, op0=ALU.mult), reads=["th"], writes=[CK])
    P.dve(lambda e: e.tensor_scalar(out=a8b, in0=a8, scalar1=0.5 * math.pi, scalar2=None, op0=ALU.add), reads=[CK], writes=[CK + "b"])
    _sin_reduce(self, P, a8, k8, kf8, CK)
    P.act(lambda e: e.activation(out=s512[:], in_=a8, func=AF.Sin), reads=[CK], writes=["s512"])
    _sin_reduce(self, P, a8b, k8, kf8, CK + "b", keys_extra=(CK, CK + "_ki", CK + "_kf"))
    P.act(lambda e: e.activation(out=c512[:], in_=a8b, func=AF.Sin), reads=[CK + "b"], writes=["c512"])
    P.dve(lambda e: e.tensor_scalar(out=ns512[:], in0=s512[:], scalar1=-1.0, scalar2=None, op0=ALU.mult), reads=["s512"], writes=["ns512"])

    are = A2[:, :, :, 0].rearrange("p a b -> p (a b)")
    aim = A2[:, :, :, 1].rearrange("p a b -> p (a b)")
    ldt = A2[:, :, :, 2].rearrange("p a b -> p (a b)")
    dt_, mag, ang, sn, cs, t5, t6, t7 = [t[:] for t in T]
    ZK = "zoh"
    def zd(fn):
        P.dve(fn, reads=[ZK, "s5A2", "s5bT", "s512", "c512", "ns512", CK, CK + "b"], writes=[ZK])
    def za(fn):
        P.act(fn, reads=[ZK, "s5A2", "s512", "c512", CK, CK + "b"], writes=[ZK])
    za(lambda e: e.activation(out=dt_, in_=ldt, func=AF.Exp))
    zd(lambda e: e.tensor_tensor(out=mag, in0=are, in1=dt_, op=ALU.mult))
    za(lambda e: e.activation(out=mag, in_=mag, func=AF.Exp))
    zd(lambda e: e.tensor_tensor(out=ang, in0=aim, in1=dt_, op=ALU.mult))
    zd(lambda e: e.tensor_scalar(out=t5, in0=ang, scalar1=0.5 * math.pi, scalar2=None, op0=ALU.add))
    _sin_reduce(self, P, ang, Ti, t6, ZK)
    za(lambda e: e.activation(out=sn, in_=ang, func=AF.Sin))
    _sin_reduce(self, P, t5, Ti, t6, ZK)
    za(lambda e: e.activation(out=cs, in_=t5, func=AF.Sin))
    zd(lambda e: e.tensor_tensor(out=cs, in0=cs, in1=mag, op=ALU.mult))
    zd(lambda e: e.tensor_scalar(out=cs, in0=cs, scalar1=-1.0, scalar2=None, op0=ALU.add))
    zd(lambda e: e.tensor_tensor(out=sn, in0=sn, in1=mag, op=ALU.mult))
    zd(lambda e: e.tensor_tensor(out=t5, in0=are, in1=are, op=ALU.mult))
    zd(lambda e: e.tensor_tensor(out=t6, in0=aim, in1=aim, op=ALU.mult))
    zd(lambda e: e.tensor_tensor(out=t7, in0=t5, in1=t6, op=ALU.add))
    zd(lambda e: e.reciprocal(out=t7, in_=t7))
    zd(lambda e: e.tensor_tensor(out=t5, in0=cs, in1=are, op=ALU.mult))
    zd(lambda e: e.tensor_tensor(out=dt_, in0=sn, in1=aim, op=ALU.mult))
    zd(lambda e: e.tensor_tensor(out=t5, in0=t5, in1=dt_, op=ALU.add))
    zd(lambda e: e.tensor_tensor(out=t5, in0=t5, in1=t7, op=ALU.mult))
    zd(lambda e: e.tensor_tensor(out=t6, in0=sn, in1=are, op=ALU.mult))
    zd(lambda e: e.tensor_tensor(out=dt_, in0=cs, in1=aim, op=ALU.mult))
    zd(lambda e: e.tensor_tensor(out=t6, in0=t6, in1=dt_, op=ALU.subtract))
    zd(lambda e: e.tensor_tensor(out=t6, in0=t6, in1=t7, op=ALU.mult))
    bre = bT[:, 0, :, :].rearrange("p a b -> p (a b)")
    bim = bT[:, 1, :, :].rearrange("p a b -> p (a b)")
    zd(lambda e: e.tensor_tensor(out=mag, in0=t5, in1=bre, op=ALU.mult))
    zd(lambda e: e.tensor_tensor(out=dt_, in0=t6, in1=bim, op=ALU.mult))
    zd(lambda e: e.tensor_tensor(out=mag, in0=mag, in1=dt_, op=ALU.subtract))
    zd(lambda e: e.tensor_tensor(out=ang, in0=t5, in1=bim, op=ALU.mult))
    zd(lambda e: e.tensor_tensor(out=dt_, in0=t6, in1=bre, op=ALU.mult))
    zd(lambda e: e.tensor_tensor(out=ang, in0=ang, in1=dt_, op=ALU.add))
    for (src, dst, nm) in ((mag, BDr, "BDr"), (ang, BDi, "BDi")):
        for sc in range(8):
            for s2 in range(2):
                P.dve(lambda e, src=src, dst=dst, sc=sc, s2=s2: e.tensor_scalar(out=dst[:, sc, s2 * 64:(s2 + 1) * 64], in0=src[:, (sc // 4) * 64:(sc // 4 + 1) * 64],
                                                                               scalar1=bm[:, (sc % 4) * 2 + s2:(sc % 4) * 2 + s2 + 1], scalar2=None, op0=ALU.mult),
                      reads=[ZK, "s5bm"], writes=[nm])
    for (ri, dst, nm, sgn) in ((0, CBr, "CBr", 1.0), (1, CBi, "CBi", -1.0)):
        for sc in range(8):
            P.dve(lambda e, ri=ri, dst=dst, sc=sc, sgn=sgn: e.scalar_tensor_tensor(
                out=dst[:, sc, :].rearrange("p (g h) -> p g h", h=16),
                in0=cT[:, ri, sc, :].unsqueeze(1).to_broadcast([128, 8, 16]), scalar=sgn,
                in1=cm[:, sc % 4, :].unsqueeze(2).to_broadcast([128, 8, 16]), op0=ALU.mult, op1=ALU.mult),
                  reads=["s5cT", "s5cm"], writes=[nm])

    def cons_u(c, tg, bank):
        P.act(lambda e: e.activation(out=uT[:, c, tg * 512:(tg + 1) * 512], in_=ps[bank][:], func=AF.Identity),
              reads=[("ps", bank)], writes=["uT"])
    _proj_fm(self, self.w_s5u[l], 256, cons_u, "s5u")

    GC = 2.0 * math.sqrt(2.0 / math.pi)
    SK = "s5w"
    for yc in range(2):
        for scl in range(4):
            sc = yc * 4 + scl
            for (dst, shf) in ((sinB, 0.0), (cosB, 0.5 * math.pi)):
                P.dve(lambda e, sc=sc, shf=shf: e.tensor_scalar(out=ta[:], in0=jidx[:], scalar1=th[:, sc:sc + 1], scalar2=shf, op0=ALU.mult, op1=ALU.add),
                      reads=["jidx", "th", SK], writes=[SK])
                _sin_reduce(self, P, ta[:], ki, tb2[:], SK)
                P.act(lambda e, dst=dst, scl=scl: e.activation(out=dst[:, scl, :], in_=ta[:], func=AF.Sin), reads=[SK], writes=[SK])
        for tg in range(4):
            yb = 6 + (tg % 2)
            for scl in range(4):
                sc = yc * 4 + scl
                cosT = cosB[:, scl, :]
                sinT = sinB[:, scl, :]
                P.pe(lambda e, sc=sc, yc=yc, tg=tg: e.matmul(ps[0][:], lhsT=BDr[:, sc, :], rhs=uT[:, yc, tg * 512:(tg + 1) * 512], start=True, stop=True),
                     reads=["BDr", "uT"], writes=[("ps", 0)])
                P.pe(lambda e, sc=sc, yc=yc, tg=tg: e.matmul(ps[1][:], lhsT=BDi[:, sc, :], rhs=uT[:, yc, tg * 512:(tg + 1) * 512], start=True, stop=True),
                     reads=["BDi", "uT"], writes=[("ps", 1)])
                W = dict(reads=[SK, ("ps", 0), ("ps", 1), "rr", "zst", "c512", "s512", "ns512"], writes=[SK])
                if tg == 0:
                    P.dve(lambda e, sc=sc: e.memset(zin[:, sc, :], 0.0), **W)
                else:
                    P.dve(lambda e, sc=sc: e.tensor_tensor(out=ztmp[:, 0:1], in0=zst[:, sc, 0:1], in1=c512[:, sc:sc + 1], op=ALU.mult), **W)
                    P.dve(lambda e, sc=sc: e.scalar_tensor_tensor(out=zin[:, sc, 0:1], in0=zst[:, sc, 1:2], scalar=ns512[:, sc:sc + 1], in1=ztmp[:, 0:1],
                                                                  op0=ALU.mult, op1=ALU.add), **W)
                    P.dve(lambda e, sc=sc: e.tensor_tensor(out=ztmp[:, 1:2], in0=zst[:, sc, 1:2], in1=c512[:, sc:sc + 1], op=ALU.mult), **W)
                    P.dve(lambda e, sc=sc: e.scalar_tensor_tensor(out=zin[:, sc, 1:2], in0=zst[:, sc, 0:1], scalar=s512[:, sc:sc + 1], in1=ztmp[:, 1:2],
                                                                  op0=ALU.mult, op1=ALU.add), **W)
                P.dve(lambda e, cosT=cosT: e.tensor_tensor(out=ur[:], in0=ps[0][:], in1=cosT, op=ALU.mult), **W)
                P.dve(lambda e, sinT=sinT: e.tensor_tensor(out=ta[:], in0=ps[1][:], in1=sinT, op=ALU.mult), **W)
                P.dve(lambda e: e.tensor_tensor(out=ur[:], in0=ur[:], in1=ta[:], op=ALU.add), **W)
                P.dve(lambda e, cosT=cosT: e.tensor_tensor(out=ui[:], in0=ps[1][:], in1=cosT, op=ALU.mult), **W)
                P.dve(lambda e, sinT=sinT: e.tensor_tensor(out=tb2[:], in0=ps[0][:], in1=sinT, op=ALU.mult), **W)
                P.dve(lambda e: e.tensor_tensor(out=ui[:], in0=ui[:], in1=tb2[:], op=ALU.subtract), **W)
                P.dve(lambda e, sc=sc: e.tensor_copy(out=rt[:], in_=rr[:, sc:sc + 1].to_broadcast([128, 512])), **W)
                P.dve(lambda e, sc=sc: e.tensor_tensor_scan(out=zr[:], data0=rt[:], data1=ur[:], initial=zin[:, sc, 0:1], op0=ALU.mult, op1=ALU.add), **W)
                P.dve(lambda e, sc=sc: e.tensor_tensor_scan(out=zi[:], data0=rt[:], data1=ui[:], initial=zin[:, sc, 1:2], op0=ALU.mult, op1=ALU.add), **W)
                P.dve(lambda e, sc=sc: e.tensor_copy(out=zst[:, sc, 0:1], in_=zr[:, 511:512]), reads=[SK], writes=[SK, "zst"])
                P.dve(lambda e, sc=sc: e.tensor_copy(out=zst[:, sc, 1:2], in_=zi[:, 511:512]), reads=[SK], writes=[SK, "zst"])
                P.dve(lambda e, cosT=cosT: e.tensor_tensor(out=ta[:], in0=zr[:], in1=cosT, op=ALU.mult), **W)
                P.dve(lambda e, sinT=sinT: e.tensor_tensor(out=tb2[:], in0=zi[:], in1=sinT, op=ALU.mult), **W)
                P.dve(lambda e: e.tensor_tensor(out=xr[:], in0=ta[:], in1=tb2[:], op=ALU.subtract), reads=[SK, "s_xr"], writes=[SK, "s_xr"])
                P.dve(lambda e, sinT=sinT: e.tensor_tensor(out=ur[:], in0=zr[:], in1=sinT, op=ALU.mult), **W)
                P.dve(lambda e, cosT=cosT: e.tensor_tensor(out=ui[:], in0=zi[:], in1=cosT, op=ALU.mult), **W)
                P.dve(lambda e: e.tensor_tensor(out=xi[:], in0=ur[:], in1=ui[:], op=ALU.add), reads=[SK, "s_xi"], writes=[SK, "s_xi"])
                P.pe(lambda e, sc=sc, scl=scl, yb=yb: e.matmul(ps[yb][:], lhsT=CBr[:, sc, :], rhs=xr[:], start=(scl == 0), stop=False),
                     reads=["CBr", "s_xr"], writes=[("ps", yb)])
                P.pe(lambda e, sc=sc, scl=scl, yb=yb: e.matmul(ps[yb][:], lhsT=CBi[:, sc, :], rhs=xi[:], start=False, stop=(scl == 3)),
                     reads=["CBi", "s_xi"], writes=[("ps", yb)])
            YK = dict(reads=[SK, "uT", "s5d", ("ps", yb)], writes=[SK])
            P.dve(lambda e, yc=yc, tg=tg, yb=yb: e.scalar_tensor_tensor(out=zr[:], in0=uT[:, yc, tg * 512:(tg + 1) * 512], scalar=dsk[:, yc:yc + 1], in1=ps[yb][:],
                                                                       op0=ALU.mult, op1=ALU.add), **YK)
            P.dve(lambda e: e.tensor_tensor(out=zi[:], in0=zr[:], in1=zr[:], op=ALU.mult), **YK)
            P.dve(lambda e: e.tensor_scalar(out=zi[:], in0=zi[:], scalar1=0.044715, scalar2=1.0, op0=ALU.mult, op1=ALU.add), **YK)
            P.dve(lambda e: e.tensor_tensor(out=zi[:], in0=zi[:], in1=zr[:], op=ALU.mult), **YK)
            P.act(lambda e: e.activation(out=zi[:], in_=zi[:], func=AF.Sigmoid, scale=GC), **YK)
            P.dve(lambda e: e.tensor_tensor(out=gyb[:], in0=zi[:], in1=zr[:], op=ALU.mult), reads=[SK, "s_gy"], writes=[SK, "s_gy"])
            P.pe(lambda e, yc=yc: e.matmul(ps[2][:], lhsT=wg[:, yc * 2, :], rhs=gyb[:], start=True, stop=True), reads=["wg", "s_gy"], writes=[("ps", 2)])
            P.pe(lambda e, yc=yc: e.matmul(ps[3][:], lhsT=wg[:, yc * 2 + 1, :], rhs=gyb[:], start=True, stop=True), reads=["wg", "s_gy"], writes=[("ps", 3)])
            P.act(lambda e: e.activation(out=ur[:], in_=ps[3][:], func=AF.Sigmoid), reads=[("ps", 3), SK], writes=[SK])
            P.dve(lambda e, yc=yc, tg=tg: e.tensor_tensor(out=R1[:, 2 + yc, tg * 512:(tg + 1) * 512], in0=ps[2][:], in1=ur[:], op=ALU.mult),
                  reads=[("ps", 2), SK], writes=[("R1", 2 + yc), SK])


Builder.declare_s5 = _declare_s5
Builder.emit_s5 = _emit_s5
Builder.declare_mixer = _declare_mixer
Builder.emit_mixer_consts = _emit_mixer_consts
Builder.emit_mixer = _emit_mixer
```

```python
import math
from contextlib import ExitStack

import numpy as np
import concourse.bass as bass
import concourse.mybir as mybir
from concourse.bass_utils import run_bass_kernel_spmd

F32 = mybir.dt.float32
BF16 = mybir.dt.bfloat16
I32 = mybir.dt.int32
AF = mybir.ActivationFunctionType
ALU = mybir.AluOpType
AX = mybir.AxisListType

D = 1024
S = 2048
NT = 16
KC = 8
DEPTH = 2
NE = 32
DN_ALPHA = (2 * DEPTH) ** 0.25
LN_EPS = 1e-5
N_IN = 2568

ENGS = ("pe", "act", "dve", "pool", "sp")


class Op:
    __slots__ = ("eng", "fn", "reads", "writes", "chan", "idx", "sig", "sigval",
                 "waits", "chan_count")

    def __init__(self, eng, fn, reads, writes, chan):
        self.eng, self.fn, self.reads, self.writes, self.chan = eng, fn, reads, writes, chan
        self.sig = False
        self.sigval = 0
        self.waits = []
        self.chan_count = 0


class Prog:
    def __init__(self, nc, same_engine_sync=True):
        self.nc = nc
        self.ops = []
        self.same_engine_sync = same_engine_sync
        self.barriers = []
        self.stack = ExitStack()

    def op(self, eng, fn, reads=(), writes=(), chan=None):
        o = Op(eng, fn, tuple(reads), tuple(writes), chan)
        o.idx = len(self.ops)
        self.ops.append(o)
        return o

    def pe(self, fn, reads=(), writes=()):
        return self.op("pe", fn, reads, writes)

    def act(self, fn, reads=(), writes=()):
        return self.op("act", fn, reads, writes)

    def dve(self, fn, reads=(), writes=()):
        return self.op("dve", fn, reads, writes)

    def pool(self, fn, reads=(), writes=()):
        return self.op("pool", fn, reads, writes)

    def dma(self, fn, reads=(), writes=(), chan="d0", eng="sp"):
        return self.op(eng, fn, reads, writes, chan)

    def barrier(self):
        self.barriers.append(len(self.ops))

    def sbuf(self, name, shape, dtype):
        return self.stack.enter_context(self.nc.sbuf_tensor(name, list(shape), dtype))

    def psum(self, name, shape, dtype):
        return self.stack.enter_context(self.nc.psum_tensor(name, list(shape), dtype))

    def resolve(self):
        ops = self.ops
        last_w, readers, last_on_eng, chan_total = {}, {}, {}, {}
        bset = sorted(set(self.barriers))
        bi = 0
        pending_barrier = {}
        deps_all = []
        for o in ops:
            while bi < len(bset) and bset[bi] <= o.idx:
                snap = (dict(last_on_eng), dict(chan_total))
                for e in ENGS:
                    pending_barrier[e] = snap
                bi += 1
            deps = set()
            cdeps = {}
            if o.eng in pending_barrier:
                leng, ctot = pending_barrier.pop(o.eng)
                for d in leng.values():
                    deps.add(d)
                for c, n in ctot.items():
                    cdeps[c] = max(cdeps.get(c, 0), n)
            for k in o.reads:
                d = last_w.get(k)
                if d is not None:
                    deps.add(d)
            for k in o.writes:
                d = last_w.get(k)
                if d is not None:
                    deps.add(d)
                for r in readers.get(k, ()):
                    deps.add(r)
            real = []
            for d in deps:
                if d is o:
                    continue
                if d.chan is not None:
                    cdeps[d.chan] = max(cdeps.get(d.chan, 0), chan_total[d.chan])
                    continue
                if d.eng == o.eng and (o.eng == "pe" or not self.same_engine_sync):
                    continue
                real.append(d)
                d.sig = True
            deps_all.append((real, cdeps))
            for k in o.writes:
                last_w[k] = o
                readers[k] = []
            for k in o.reads:
                readers.setdefault(k, []).append(o)
            if o.chan is not None:
                chan_total[o.chan] = chan_total.get(o.chan, 0) + 1
                o.chan_count = chan_total[o.chan]
            else:
                last_on_eng[o.eng] = o
        cnt = {e: 0 for e in ENGS}
        for o in ops:
            if o.chan is None and o.sig:
                cnt[o.eng] += 1
                o.sigval = cnt[o.eng]
        self.sig_counts = cnt
        self.chan_totals = chan_total
        waited = {e: {} for e in ENGS}
        for o, (real, cdeps) in zip(ops, deps_all):
            w = {}
            for d in real:
                w[d.eng] = max(w.get(d.eng, 0), d.sigval)
            for c, n in cdeps.items():
                w[("chan", c)] = max(w.get(("chan", c), 0), 16 * n)
            out = []
            for k, v in w.items():
                if waited[o.eng].get(k, 0) < v:
                    waited[o.eng][k] = v
                    out.append((k, v))
            o.waits = out

    def emit(self):
        nc = self.nc
        self.resolve()
        sems = {}
        for e in ENGS:
            sems[e] = self.stack.enter_context(nc.semaphore("s_" + e))
        for c in self.chan_totals:
            sems[("chan", c)] = self.stack.enter_context(nc.semaphore("c_" + c))
        by_eng = {e: [o for o in self.ops if o.eng == e] for e in ENGS}
        engobj = {"pe": "tensor", "act": "scalar", "dve": "vector", "pool": "gpsimd", "sp": "sync"}

        def make_section(e):
            def section(eng):
                for o in by_eng[e]:
                    for k, v in o.waits:
                        eng.wait_ge(sems[k], v)
                    ins = o.fn(eng)
                    if o.chan is not None:
                        ins.then_inc(sems[("chan", o.chan)], 16)
                    elif o.sig:
                        ins.then_inc(sems[e], 1)
            return section

        with nc.Block() as block:
            for e in ENGS:
                if by_eng[e]:
                    getattr(block, engobj[e])(make_section(e))
        self.stack.close()


class Builder:
    def __init__(self, cfg):
        self.cfg = cfg
        self.nc = bass.Bass("TRN2", target_bir_lowering=False)
        self.P = Prog(self.nc, same_engine_sync=cfg.get("ses", True))
        self.din = {}
        self.dout = {}
        self._uid = 0

    def inp(self, name, shape, dtype=F32):
        t = self.nc.dram_tensor(name, list(shape), dtype, kind="ExternalInput").ap()
        self.din[name] = t
        return t

    def outp(self, name, shape, dtype=F32):
        t = self.nc.dram_tensor(name, list(shape), dtype, kind="ExternalOutput").ap()
        self.dout[name] = t
        return t

    def uid(self, p="t"):
        self._uid += 1
        return f"{p}{self._uid}"

    def arena_init(self, nbytes):
        self.arena = self.P.sbuf("arena", [128, nbytes // 4], F32)
        self.arena_words = nbytes // 4
        self.aoff = 0

    def arena_reset(self):
        self.P.barrier()
        self.aoff = 0

    def carve(self, shape, dtype):
        esz = 2 if dtype == BF16 else 4
        n = 1
        for d_ in shape[1:]:
            n *= d_
        words = (n * esz + 3) // 4
        words = (words + 7) // 8 * 8
        assert self.aoff + words <= self.arena_words, ("arena overflow", self.aoff, words, self.arena_words)
        v = self.arena[0:shape[0], self.aoff:self.aoff + words]
        self.aoff += words
        if dtype != F32:
            v = v.bitcast(dtype)
        v = v[:, 0:n]
        if len(shape) == 3:
            v = v.rearrange("p (a b) -> p a b", b=shape[2])
        elif len(shape) == 4:
            v = v.rearrange("p (a b c) -> p a b c", b=shape[2], c=shape[3])
        return v

    def declare_common(self):
        P = self.P
        self.x_in = self.inp("x", [S, D])
        self.cT_in = self.inp("cT", [128, KC])
        self.ident_in = self.inp("ident", [128, 128])
        self.ada_w = self.inp("ada_w", [DEPTH, 2, D, 3 * D])
        self.ada_b = self.inp("ada_b", [DEPTH, 2, 3 * D])
        self.ln_g = self.inp("ln_g", [DEPTH, 2, D])
        self.ln_b = self.inp("ln_b", [DEPTH, 2, D])
        self.w_router = self.inp("w_router", [DEPTH, D, NE])
        self.b_router = self.inp("b_router", [DEPTH, NE])
        ned = self.cfg.get("n_exp_decl", NE)
        self.w_up = self.inp("w_up", [DEPTH, ned, D, 2 * D])
        self.b_upT = self.inp("b_upT", [128, DEPTH, NE, 16])
        self.w_down = self.inp("w_down", [DEPTH, ned, D, D])
        self.b_down = self.inp("b_down", [DEPTH, NE, D])
        self.out = self.outp("out", [S, D])

        self.X = P.sbuf("X", [128, NT, D], F32)
        self.HT = P.sbuf("HT", [128, KC, S], BF16)
        self.ident = P.sbuf("ident32", [128, 128], F32)
        self.identb = P.sbuf("identb", [128, 128], BF16)
        self.cT = P.sbuf("cTs", [128, KC], F32)
        self.cB = P.sbuf("cB", [128, KC, 128], BF16)
        self.eps = P.sbuf("epsc", [128, 1], F32)
        self.ps = [P.psum(f"ps{i}", [128, 512], F32) for i in range(8)]
        self.G = P.sbuf("Grow", [128, D], F32)
        self.sc1p = P.sbuf("sc1p", [128, KC], F32)
        self.shp = P.sbuf("shp", [128, KC], F32)
        self.lng = P.sbuf("lng", [128, D], F32)
        self.lnb = P.sbuf("lnb", [128, D], F32)
        self.comb = P.sbuf("comb", [128, NT, NE], F32)
        self.bup = P.sbuf("bup", [128, NE, 16], F32)
        self.lnst = P.sbuf("lnst", [128, 2, 6], F32)
        self.lnmv = P.sbuf("lnmv", [128, 2], F32)
        self.lnr = P.sbuf("lnr", [128, 1], F32)
        self.arena_init(self.cfg.get("arena_bytes", 94208))

    def emit_consts(self):
        P = self.P
        P.dma(lambda e: e.dma_start(out=self.ident[:], in_=self.ident_in[:, :]),
              writes=["ident"], chan="c0")
        P.dma(lambda e: e.dma_start(out=self.cT[:], in_=self.cT_in[:, :]),
              writes=["cT"], chan="c0")
        P.dve(lambda e: e.tensor_copy(out=self.identb[:], in_=self.ident[:]),
              reads=["ident"], writes=["identb"])
        P.dve(lambda e: e.memset(self.eps[:], LN_EPS), writes=["eps"])
        P.act(lambda e: e.activation(out=self.cT[:], in_=self.cT[:], func=AF.Silu),
              reads=["cT"], writes=["cT"])
        P.dve(lambda e: e.tensor_copy(out=self.cB[:], in_=self.cT[:].unsqueeze(2).to_broadcast([128, KC, 128])),
              reads=["cT"], writes=["cB"])

    def load_x(self, src=None):
        P = self.P
        src = self.x_in if src is None else src
        v = src.rearrange("(tt p) d -> p tt d", p=128)
        for q in range(4):
            P.dma(lambda e, q=q: e.dma_start(out=self.X[:, 4 * q:4 * q + 4, :], in_=v[:, 4 * q:4 * q + 4, :]),
                  writes=[("X", t) for t in range(4 * q, 4 * q + 4)], chan="xio")

    def store_x(self, dst=None):
        P = self.P
        dst = self.out if dst is None else dst
        v = dst.rearrange("(tt p) d -> p tt d", p=128)
        for q in range(4):
            P.dma(lambda e, q=q: e.dma_start(out=v[:, 4 * q:4 * q + 4, :], in_=self.X[:, 4 * q:4 * q + 4, :]),
                  reads=[("X", t) for t in range(4 * q, 4 * q + 4)], writes=[("out", q)], chan="xout")
        P.op("sp", lambda e: e.nop(), reads=[("out", q) for q in range(4)])

    def emit_mod(self, l, j):
        P = self.P
        ps = self.ps
        modw = [self.carve([128, KC, 512], BF16) for _ in range(2)]
        adab = [self.carve([128, 512], F32) for _ in range(2)]
        mtmp = self.carve([128, 512], F32)
        mtmp2 = self.carve([128, 512], F32)
        P.dma(lambda e: e.dma_start(out=self.lnb[:], in_=self.ln_b[l, j].partition_broadcast(128)),
              writes=["lnb"], chan="c0")
        P.dma(lambda e: e.dma_start(out=self.lng[:], in_=self.ln_g[l, j].partition_broadcast(128)),
              writes=["lng"], chan="c0")
        wv = self.ada_w[l, j].rearrange("(kc p) e -> p kc e", p=128)
        idb = self.ident[:].unsqueeze(1).to_broadcast([128, 4, 128])
        for pc in range(6):
            s = pc % 2
            P.dma(lambda e, pc=pc, s=s: e.dma_start(out=adab[s][:], in_=self.ada_b[l, j, pc * 512:(pc + 1) * 512].partition_broadcast(128)),
                  writes=[("adab", s)], chan=f"adab{s}")
            P.dma(lambda e, pc=pc, s=s: e.dma_start(out=modw[s][:], in_=wv[:, :, pc * 512:(pc + 1) * 512]),
                  writes=[("modw", s)], chan=f"modw{s}", eng="pool")
            bank = 6 + (pc % 2)
            for kc in range(KC):
                P.pe(lambda e, kc=kc, s=s, bank=bank: e.matmul(ps[bank][:], lhsT=self.cB[:, kc, :], rhs=modw[s][:, kc, :],
                                                               start=(kc == 0), stop=(kc == KC - 1)),
                     reads=["cB", ("modw", s)], writes=[("ps", bank)])
            if pc >= 4:
                P.dve(lambda e, pc=pc, bank=bank, s=s: e.scalar_tensor_tensor(out=self.G[:, (pc - 4) * 512:(pc - 3) * 512], in0=ps[bank][:], scalar=1.0,
                                                                              in1=adab[s][:], op0=ALU.add, op1=ALU.add),
                      reads=[("ps", bank), ("adab", s)], writes=["G"])
            else:
                dst, name = (self.shp, "shp") if pc < 2 else (self.sc1p, "sc1p")
                c0 = (pc % 2) * 4
                addc = 0.0 if pc < 2 else 1.0
                P.dve(lambda e, bank=bank, s=s, addc=addc: e.scalar_tensor_tensor(out=mtmp[:], in0=ps[bank][:], scalar=addc,
                                                                                   in1=adab[s][:], op0=ALU.add, op1=ALU.add),
                      reads=[("ps", bank), ("adab", s)], writes=["mtmp"])
                P.dve(lambda e: e.tensor_tensor(out=mtmp2[:].rearrange("p (k q) -> p k q", q=128),
                                                in0=mtmp[:].rearrange("p (k q) -> p k q", q=128), in1=idb, op=ALU.mult),
                      reads=["mtmp", "ident"], writes=["mtmp2"])
                P.dve(lambda e, dst=dst, c0=c0: e.tensor_reduce(out=dst[:, c0:c0 + 4], in_=mtmp2[:].rearrange("p (k q) -> p k q", q=128),
                                                                axis=AX.X, op=ALU.add),
                      reads=["mtmp2"], writes=[name])

    def emit_hT(self, l, router):
        P = self.P
        ps = self.ps
        if router:
            wr = self.wr[:, :, 0:128]
            P.dve(lambda e: e.memset(wr, 0.0), writes=[("w1l", 1)])
            P.dma(lambda e: e.dma_start(out=wr[:, :, 0:NE], in_=self.w_router[l].rearrange("(kc p) e -> p kc e", p=128)),
                  writes=[("w1l", 1)], chan="c0")
            P.dve(lambda e: e.tensor_copy(out=self.wrb[:], in_=wr), reads=[("w1l", 1)], writes=["wrb"])
            P.dve(lambda e: e.tensor_tensor(out=self.wrl[:], in0=wr, in1=self.wrb[:], op=ALU.subtract), reads=[("w1l", 1), "wrb"], writes=["wrl"])
            P.dma(lambda e: e.dma_start(out=self.brow[:], in_=self.b_router[l].partition_broadcast(128)),
                  writes=["brow"], chan="c0")
        for tg in range(4):
            for kc in range(KC):
                bank = kc % 2
                for i in range(4):
                    tt = tg * 4 + i
                    P.pe(lambda e, tt=tt, kc=kc, i=i, bank=bank: e.transpose(out=ps[bank][:, i * 128:(i + 1) * 128],
                                                                           in_=self.X[:, tt, kc * 128:(kc + 1) * 128],
                                                                           identity=self.ident[:]),
                         reads=[("X", tt), "ident"], writes=[("ps", bank)])
                if not router:
                    P.act(lambda e, kc=kc, tg=tg, bank=bank: e.activation(out=self.HT[:, kc, tg * 512:(tg + 1) * 512], in_=ps[bank][:],
                                                                          func=AF.Identity, bias=self.shp[:, kc:kc + 1],
                                                                          scale=self.sc1p[:, kc:kc + 1]),
                          reads=[("ps", bank), "sc1p", "shp"], writes=[("HT", kc, tg)])
                else:
                    hs = kc % 2
                    h32 = self.eA[hs]
                    P.dve(lambda e, kc=kc, bank=bank, h32=h32: e.tensor_scalar(out=h32[:], in0=ps[bank][:], scalar1=self.sc1p[:, kc:kc + 1], scalar2=self.shp[:, kc:kc + 1],
                                                                               op0=ALU.mult, op1=ALU.add),
                          reads=[("ps", bank), "sc1p", "shp"], writes=[("eA", hs)])
                    P.act(lambda e, kc=kc, tg=tg, h32=h32: e.activation(out=self.HT[:, kc, tg * 512:(tg + 1) * 512], in_=h32[:], func=AF.Identity),
                          reads=[("eA", hs)], writes=[("HT", kc, tg)])
                    P.pool(lambda e, kc=kc, tg=tg, h32=h32: e.tensor_tensor(out=self.hlo[:, kc, :], in0=h32[:], in1=self.HT[:, kc, tg * 512:(tg + 1) * 512], op=ALU.subtract),
                           reads=[("eA", hs), ("HT", kc, tg)], writes=[("w1g", 1)])
            if router:
                for kc in range(KC):
                    trip = ((self.wrb, self.HT[:, kc, tg * 512:(tg + 1) * 512]), (self.wrb, self.hlo[:, kc, :]), (self.wrl, self.HT[:, kc, tg * 512:(tg + 1) * 512]))
                    for ti, (wmat, rhs_) in enumerate(trip):
                        P.pe(lambda e, kc=kc, ti=ti, wmat=wmat, rhs_=rhs_: e.matmul(ps[2][:], lhsT=wmat[:, kc, :], rhs=rhs_,
                                                                                   start=(kc == 0 and ti == 0), stop=(kc == KC - 1 and ti == 2)),
                             reads=[("HT", kc, tg), ("w1g", 1), "wrb", "wrl"], writes=[("ps", 2)])
            rsub = self.cfg.get("rsub", 9)
            if router and rsub >= 2:
                P.act(lambda e: e.activation(out=self.lgT[:], in_=ps[2][0:32, :], func=AF.Identity),
                      reads=[("ps", 2)], writes=["lgT"])
                for i in range(4):
                    tt = tg * 4 + i
                    P.pe(lambda e, i=i: e.transpose(out=ps[3][:, i * 32:(i + 1) * 32], in_=self.lgT[:, i * 128:(i + 1) * 128],
                                                    identity=self.ident[0:32, 0:32]),
                         reads=["lgT", "ident"], writes=[("ps", 3)])
                lg = self.lg
                P.dve(lambda e: e.tensor_tensor(out=lg[:], in0=ps[3][:, 0:128].rearrange("p (i e) -> p i e", e=32),
                                                in1=self.brow[:].unsqueeze(1).to_broadcast([128, 4, NE]), op=ALU.add),
                      reads=[("ps", 3), "brow"], writes=["lg"])
                for i in range(4 if rsub >= 3 else 0):
                    tt = tg * 4 + i
                    t8, ng, ex, sm = self.top8, self.negm, self.ex, self.ssum
                    P.dve(lambda e, i=i: e.max(out=t8[:], in_=lg[:, i, :]), reads=["lg"], writes=["t8"])
                    P.dve(lambda e: e.tensor_scalar(out=ng[:, 0:1], in0=t8[:, 0:1], scalar1=-1.0, scalar2=None, op0=ALU.mult),
                          reads=["t8"], writes=["ng"])
                    P.act(lambda e, i=i: e.activation(out=ex[:], in_=lg[:, i, :], func=AF.Exp, bias=ng[:, 0:1], scale=1.0),
                          reads=["lg", "ng"], writes=["ex"])
                    P.dve(lambda e, i=i: e.tensor_scalar(out=self.msk[:], in0=lg[:, i, :], scalar1=t8[:, 3:4], scalar2=None, op0=ALU.is_ge),
                          reads=["lg", "t8"], writes=["msk"])
                    P.dve(lambda e: e.tensor_tensor(out=ex[:], in0=ex[:], in1=self.msk[:], op=ALU.mult),
                          reads=["ex", "msk"], writes=["ex"])
                    P.dve(lambda e: e.tensor_reduce(out=sm[:, 0:1], in_=ex[:], axis=AX.X, op=ALU.add),
                          reads=["ex"], writes=["sm"])
                    P.dve(lambda e: e.reciprocal(out=sm[:, 0:1], in_=sm[:, 0:1]), reads=["sm"], writes=["sm"])
                    P.dve(lambda e, tt=tt: e.tensor_scalar(out=self.comb[:, tt, :], in0=ex[:], scalar1=sm[:, 0:1], scalar2=None, op0=ALU.mult),
                          reads=["ex", "sm"], writes=[("comb", tt)])

    def declare_moe(self):
        self.wrb = self.carve([128, KC, 128], BF16)
        self.wrl = self.carve([128, KC, 128], BF16)
        self.brow = self.carve([128, NE], F32)
        self.lgT = self.carve([32, 512], F32)
        self.lg = self.carve([128, 4, NE], F32)
        self.top8 = self.carve([128, 8], F32)
        self.negm = self.carve([128, 8], F32)
        self.ex = self.carve([128, NE], F32)
        self.msk = self.carve([128, NE], F32)
        self.ssum = self.carve([128, 8], F32)
        self.combT = self.carve([32, NT, 128], BF16)
        NW = 2
        self.NW = NW
        self.w1g = [self.carve([128, KC, 512], BF16) for i in range(NW)]
        self.w1l = [self.carve([128, KC, 512], BF16) for i in range(NW)]
        self.w2h = [self.carve([128, 4, D], BF16) for i in range(NW)]
        self.bdn = self.carve([32, D], F32)
        self.bdnG = self.carve([32, D], BF16)
        self.NA = 2
        self.actb = [self.carve([128, 4, 512], BF16) for i in range(self.NA)]
        self.NEp = 2
        self.eA = [self.carve([128, 512], F32) for i in range(self.NEp)]
        self.eB = [self.carve([128, 512], F32) for i in range(self.NEp)]
        self.eS = [self.carve([128, 512], F32) for i in range(self.NEp)]
        self.hlo = self.w1g[1]
        self.wr = self.w1l[1].bitcast(F32)
        self.h32 = self.eA[0]

    def emit_bup(self, l):
        P = self.P
        P.dma(lambda e: e.dma_start(out=self.bup[:], in_=self.b_upT[:, l, :, :]), writes=["bup"], chan="c0")
        P.dve(lambda e: e.tensor_scalar(out=self.bup[:, :, 8:16], in0=self.bup[:, :, 8:16], scalar1=1.0, scalar2=None, op0=ALU.add),
              reads=["bup"], writes=["bup"])

    def emit_ln(self, tt, src_keys=()):
        P = self.P
        X = self.X
        k = ("X", tt)
        for h in range(2):
            P.dve(lambda e, h=h: e.bn_stats(out=self.lnst[:, h, :], in_=X[:, tt, h * 512:(h + 1) * 512]),
                  reads=[k], writes=[("lnst", h)])
        P.dve(lambda e: e.bn_aggr(out=self.lnmv[:], in_=self.lnst[:].rearrange("p a b -> p (a b)")),
              reads=[("lnst", 0), ("lnst", 1)], writes=["lnmv"])
        P.act(lambda e: e.activation(out=self.lnr[:], in_=self.lnmv[:, 1:2], func=AF.Sqrt, bias=self.eps[:, 0:1], scale=1.0),
              reads=["lnmv", "eps"], writes=["lnr"])
        P.dve(lambda e: e.reciprocal(out=self.lnr[:], in_=self.lnr[:]), reads=["lnr"], writes=["lnr"])
        P.dve(lambda e: e.tensor_scalar(out=X[:, tt, :], in0=X[:, tt, :], scalar1=self.lnmv[:, 0:1], scalar2=self.lnr[:, 0:1],
                                        op0=ALU.subtract, op1=ALU.mult),
              reads=[k, "lnmv", "lnr"], writes=[k])
        P.dve(lambda e: e.tensor_tensor(out=X[:, tt, :], in0=X[:, tt, :], in1=self.lng[:], op=ALU.mult),
              reads=[k, "lng"], writes=[k])
        P.dve(lambda e: e.tensor_tensor(out=X[:, tt, :], in0=X[:, tt, :], in1=self.lnb[:], op=ALU.add),
              reads=[k, "lnb"], writes=[k])

    def emit_moe(self, l, n_exp=NE):
        P = self.P
        ps = self.ps
        X = self.X
        P.dma(lambda e: e.dma_start(out=self.bdn[:], in_=self.b_down[l]), writes=["bdn"], chan="c0")
        P.dve(lambda e: e.tensor_tensor(out=self.bdnG[:], in0=self.bdn[:], in1=self.G[0:32, :], op=ALU.mult),
              reads=["bdn", "G"], writes=["bdnG"])
        for tt in range(NT):
            bank = 2 + (tt % 2)
            P.pe(lambda e, tt=tt, bank=bank: e.transpose(out=ps[bank][0:32, 0:128], in_=self.comb[:, tt, :], identity=self.ident[:]),
                 reads=[("comb", tt), "ident"], writes=[("ps", bank)])
            P.act(lambda e, tt=tt, bank=bank: e.activation(out=self.combT[:, tt, :], in_=ps[bank][0:32, 0:128], func=AF.Identity),
                  reads=[("ps", bank)], writes=[("combT", tt)])
        for tt in range(NT):
            P.act(lambda e, tt=tt: e.activation(out=X[:, tt, :], in_=X[:, tt, :], func=AF.Copy, scale=float(DN_ALPHA)),
                  reads=[("X", tt)], writes=[("X", tt)])

        for tt in range(NT):
            for dh in range(2):
                ob = 4 + ((tt * 2 + dh) % 4)
                P.pe(lambda e, tt=tt, dh=dh, ob=ob: e.matmul(ps[ob][:], lhsT=self.combT[:, tt, :], rhs=self.bdnG[:, dh * 512:(dh + 1) * 512],
                                                             start=True, stop=True),
                     reads=[("combT", tt), "bdnG"], writes=[("ps", ob)])
                P.dve(lambda e, tt=tt, dh=dh, ob=ob: e.tensor_tensor(out=X[:, tt, dh * 512:(dh + 1) * 512], in0=ps[ob][:],
                                                                     in1=X[:, tt, dh * 512:(dh + 1) * 512], op=ALU.add),
                      reads=[("ps", ob), ("X", tt)], writes=[("X", tt)])

        units = [(e_, hf) for e_ in range(n_exp) for hf in range(2)]
        NW = self.NW

        def load_unit(u):
            e_, hf = units[u]
            s = u % NW
            wu = self.w_up[l, e_].rearrange("(kc p) f -> p kc f", p=128)
            wd = self.w_down[l, e_].rearrange("(j p) d -> p j d", p=128)
            P.dma(lambda e: e.dma_start(out=self.w1g[s][:], in_=wu[:, :, hf * 512:(hf + 1) * 512]),
                  writes=[("w1g", s)], chan=f"wg{s}", eng="pool")
            P.dma(lambda e: e.dma_start(out=self.w1l[s][:], in_=wu[:, :, D + hf * 512:D + (hf + 1) * 512]),
                  writes=[("w1l", s)], chan=f"wl{s}", eng="pool")
            P.dma(lambda e: e.dma_start(out=self.w2h[s][:], in_=wd[:, hf * 4:(hf + 1) * 4, :]),
                  writes=[("w2h", s)], chan=f"wd{s}", eng="pool")

        def fold_unit(u):
            s = u % NW
            for j in range(4):
                P.pool(lambda e, j=j: e.tensor_tensor(out=self.w2h[s][:, j, :], in0=self.w2h[s][:, j, :], in1=self.G[:], op=ALU.mult),
                       reads=[("w2h", s), "G"], writes=[("w2h", s)])

        epi_ctr = [0]
        zb_ctr = [0]

        def step1(u, tg, aslot):
            e_, hf = units[u]
            s = u % NW
            for j in range(4):
                zb = zb_ctr[0] % 2
                zb_ctr[0] += 1
                bg, bl = zb * 2, zb * 2 + 1
                for kc in range(KC):
                    P.pe(lambda e, kc=kc, j=j, bg=bg: e.matmul(ps[bg][:], lhsT=self.w1g[s][:, kc, j * 128:(j + 1) * 128],
                                                               rhs=self.HT[:, kc, tg * 512:(tg + 1) * 512],
                                                               start=(kc == 0), stop=(kc == KC - 1)),
                         reads=[("w1g", s), ("HT", kc, tg)], writes=[("ps", bg)])
                for kc in range(KC):
                    P.pe(lambda e, kc=kc, j=j, bl=bl: e.matmul(ps[bl][:], lhsT=self.w1l[s][:, kc, j * 128:(j + 1) * 128],
                                                               rhs=self.HT[:, kc, tg * 512:(tg + 1) * 512],
                                                               start=(kc == 0), stop=(kc == KC - 1)),
                         reads=[("w1l", s), ("HT", kc, tg)], writes=[("ps", bl)])
                es = epi_ctr[0] % self.NEp
                epi_ctr[0] += 1
                A, B, Sg = self.eA[es], self.eB[es], self.eS[es]
                fg = hf * 4 + j
                P.dve(lambda e, A=A, bg=bg, fg=fg: e.tensor_scalar(out=A[:], in0=ps[bg][:], scalar1=self.bup[:, e_, fg:fg + 1], scalar2=7.0,
                                                                   op0=ALU.add, op1=ALU.min),
                      reads=[("ps", bg), "bup"], writes=[("eA", es)])
                P.dve(lambda e, B=B, bl=bl, fg=fg: e.tensor_scalar(out=B[:], in0=ps[bl][:], scalar1=self.bup[:, e_, 8 + fg:9 + fg], scalar2=8.0,
                                                                   op0=ALU.add, op1=ALU.min),
                      reads=[("ps", bl), "bup"], writes=[("eB", es)])
                P.act(lambda e, A=A, Sg=Sg: e.activation(out=Sg[:], in_=A[:], func=AF.Sigmoid, scale=1.702),
                      reads=[("eA", es)], writes=[("eS", es)])
                P.pool(lambda e, A=A, Sg=Sg: e.tensor_tensor(out=Sg[:], in0=A[:], in1=Sg[:], op=ALU.mult),
                       reads=[("eA", es), ("eS", es)], writes=[("eS", es)])
                P.dve(lambda e, B=B, Sg=Sg, j=j: e.scalar_tensor_tensor(out=self.actb[aslot][:, j, :], in0=B[:], scalar=-6.0, in1=Sg[:],
                                                                       op0=ALU.max, op1=ALU.mult),
                      reads=[("eB", es), ("eS", es)], writes=[("actb", aslot, j)])

        ob_ctr = [0]

        def step2(u, tg, aslot):
            e_, hf = units[u]
            s = u % NW
            for i in range(4):
                tt = tg * 4 + i
                for dh in range(2):
                    ob = 4 + (ob_ctr[0] % 4)
                    ob_ctr[0] += 1
                    for j in range(4):
                        P.pe(lambda e, j=j, i=i, dh=dh, ob=ob: e.matmul(ps[ob][:], lhsT=self.actb[aslot][:, j, i * 128:(i + 1) * 128],
                                                                        rhs=self.w2h[s][:, j, dh * 512:(dh + 1) * 512],
                                                                        start=(j == 0), stop=(j == 3)),
                             reads=[("actb", aslot, j), ("w2h", s)], writes=[("ps", ob)])
                    P.dve(lambda e, tt=tt, dh=dh, ob=ob: e.scalar_tensor_tensor(out=X[:, tt, dh * 512:(dh + 1) * 512], in0=ps[ob][:],
                                                                                scalar=self.comb[:, tt, e_:e_ + 1],
                                                                                in1=X[:, tt, dh * 512:(dh + 1) * 512],
                                                                                op0=ALU.mult, op1=ALU.add),
                          reads=[("ps", ob), ("comb", tt), ("X", tt)], writes=[("X", tt)])
                if u == len(units) - 1:
                    self.emit_ln(tt)

        work = [(u, tg) for u in range(len(units)) for tg in range(4)]
        load_unit(0)
        if len(units) > 1:
            load_unit(1)
        prev = None
        for wi, (u, tg) in enumerate(work):
            aslot = wi % self.NA
            if tg == 0:
                fold_unit(u)
            step1(u, tg, aslot)
            if prev is not None:
                pu, ptg, pas = prev
                step2(pu, ptg, pas)
                if ptg == 3 and pu + NW < len(units):
                    load_unit(pu + NW)
            prev = (u, tg, aslot)
        pu, ptg, pas = prev
        step2(pu, ptg, pas)
def build(cfg):
    B = Builder(cfg)
    B.declare_common()
    mixer_on = cfg.get("mixer", True)
    if mixer_on:
        B.declare_mixer()
        if cfg.get("s5", True):
            B.declare_s5()
    B.emit_consts()
    if mixer_on:
        B.arena_reset()
        B.emit_mixer_consts()
    B.load_x()
    for l in cfg.get("layers", range(DEPTH)):
        if mixer_on:
            B.arena_reset()
            B.emit_mod(l, 0)
            B.arena_reset()
            B.emit_hT(l, router=False)
            B.emit_mixer(l)
        if cfg.get("moe", True):
            st = cfg.get("stage", 99)
            B.arena_reset()
            if st >= 1:
                B.emit_mod(l, 1)
            B.arena_reset()
            B.declare_moe()
            B.emit_bup(l)
            if st >= 2:
                B.emit_hT(l, router=cfg.get("router", True))
            if st >= 3:
                B.emit_moe(l, n_exp=cfg.get("n_exp", NE))
    B.store_x()
    B.P.emit()
    return B


def host_inputs(inputs, b):
    f = lambda a: np.ascontiguousarray(np.asarray(a))
    m = {}
    m["x"] = f(inputs["x"][b])
    m["cT"] = f(np.asarray(inputs["c"][b]).reshape(KC, 128).T)
    m["ident"] = np.eye(128, dtype=np.float32)
    for k in ("ada_w", "ada_b", "ln_g", "ln_b", "w_router", "b_router", "w_up", "w_down", "b_down"):
        m[k] = f(inputs[k])
    m["b_upT"] = f(np.asarray(inputs["b_up"]).reshape(DEPTH, NE, 16, 128).transpose(3, 0, 1, 2))
    w_in = np.asarray(inputs["w_in"])
    m["pos"] = f(np.asarray(inputs["positions"][b]).astype(np.int32))
    p = np.arange(128)
    d = p % 64
    inv = (10000.0 ** (-(2.0 * (d % 32)) / 64.0)).astype(np.float32)
    sgn = np.where(d < 32, -1.0, 1.0).astype(np.float32)
    m["ropec"] = f(np.stack([inv, sgn], axis=1))
    perm = np.arange(512).reshape(8, 64)
    perm = np.concatenate([perm[:, 32:], perm[:, :32]], axis=1).reshape(512)
    q = w_in[:, :, 0:512]; k = w_in[:, :, 512:1024]
    def chunked(w, width):
        L_, D_, n_ = w.shape
        return f(w.reshape(L_, KC, 128, n_ // width, width).transpose(0, 3, 2, 1, 4))
    qs = q[:, :, perm]; ks = k[:, :, perm]
    qk = np.stack([np.concatenate([q.reshape(DEPTH, D, 4, 128), qs.reshape(DEPTH, D, 4, 128)], axis=3),
                   np.concatenate([k.reshape(DEPTH, D, 4, 128), ks.reshape(DEPTH, D, 4, 128)], axis=3)], axis=2)
    m["w_qk"] = chunked(qk.reshape(DEPTH, D, 2048), 256)
    m["w_v"] = chunked(w_in[:, :, 1024:1536], 256)
    m["w_mlx"] = chunked(w_in[:, :, 1536:1792], 128)
    m["w_mlvo"] = chunked(w_in[:, :, 1792:2304], 256)
    m["w_mli"] = f(w_in[:, :, 2304:2308])
    m["w_mlf"] = f(w_in[:, :, 2308:2312])
    m["tri"] = f(np.triu(np.ones((128, 128), np.float32)))
    m["lamv"] = f(np.concatenate([np.asarray(inputs[k_]) for k_ in ("lam_q1", "lam_k1", "lam_q2", "lam_k2")], axis=1))
    m["da_g"] = f(inputs["da_norm_g"])
    cw = np.asarray(inputs["ml_conv_w"])
    m["convw"] = f(cw.reshape(DEPTH, 4, 2, 128).transpose(3, 0, 2, 1))
    m["convb"] = f(np.asarray(inputs["ml_conv_b"]).reshape(DEPTH, 2, 128).transpose(2, 0, 1))
    for nm, src in (("wq_bd", "ml_w_q"), ("wk_bd", "ml_w_k")):
        w = np.asarray(inputs[src])
        bd = np.zeros((DEPTH, 2, 128, 128), np.float32)
        for hc in range(2):
            for hl in range(2):
                bd[:, hc, hl * 64:(hl + 1) * 64, hl * 64:(hl + 1) * 64] = w[:, hc * 2 + hl]
        m[nm] = bd
    gbv = np.asarray(inputs["ml_gate_b"])
    m["gate_bi"] = f(gbv[:, 0:4].reshape(DEPTH, 4, 1))
    m["gate_bf"] = f(gbv[:, 4:8].reshape(DEPTH, 4, 1))
    sel = np.zeros((4, 4, 128), np.float32)
    for h in range(4):
        sel[h, h, :] = 1.0
    m["sel4"] = sel
    m["ml_g"] = f(inputs["ml_norm_g"])
    m["w_out"] = f(inputs["w_out"])
    a_re = np.asarray(inputs["s5_a_re"]); a_im = np.asarray(inputs["s5_a_im"]); ldt = np.asarray(inputs["s5_log_dt"])
    G_, P_, H_ = 16, 64, 16
    ldt_b = np.broadcast_to(ldt[:, :, None], (DEPTH, G_, P_))
    A = np.stack([a_re, a_im, ldt_b], axis=-1)
    m["s5A"] = f(A.reshape(DEPTH, 8, 2, P_, 3).transpose(2, 3, 0, 1, 4).reshape(128, DEPTH, 8, 3))
    A2 = np.broadcast_to(A[:, :, None, :, :], (DEPTH, G_, H_, P_, 3))
    m["s5A2"] = f(A2.reshape(DEPTH, 2, 8, H_, P_, 3).transpose(2, 3, 0, 1, 4, 5).reshape(128, DEPTH, 2, P_, 3))
    bb = np.stack([np.asarray(inputs["s5_b_re"]), np.asarray(inputs["s5_b_im"])], axis=1)
    m["s5bT"] = f(bb.reshape(DEPTH, 2, 2, 8, P_, H_).transpose(3, 5, 0, 1, 2, 4).reshape(128, DEPTH, 2, 2, P_))
    cc = np.stack([np.asarray(inputs["s5_c_re"]), np.asarray(inputs["s5_c_im"])], axis=1)
    m["s5cT"] = f(cc.reshape(DEPTH, 2, 8, 2, H_, P_).transpose(3, 5, 0, 1, 2, 4).reshape(128, DEPTH, 2, 8, H_))
    row = np.arange(128)
    bmask = np.zeros((128, 8), np.float32)
    for scl in range(4):
        for s2 in range(2):
            bmask[:, scl * 2 + s2] = ((row // 16) == 2 * scl + s2)
    m["s5bm"] = bmask
    cmask = np.zeros((128, 4, 8), np.float32)
    for scl in range(4):
        for gl in range(8):
            cmask[:, scl, gl] = (gl == 2 * scl + (row // 64))
    m["s5cm"] = cmask
    m["s5d"] = f(np.asarray(inputs["s5_d"]).reshape(DEPTH, 2, 128).transpose(2, 0, 1))
    wgl = np.asarray(inputs["s5_w_glu"])
    wbd = np.zeros((DEPTH, 2, 2, 128, 128), np.float32)
    for yc in range(2):
        for gl in range(8):
            g = yc * 8 + gl
            for hf in range(2):
                wbd[:, yc, hf, gl * 16:(gl + 1) * 16, gl * 16:(gl + 1) * 16] = wgl[:, g, :, hf * 16:(hf + 1) * 16]
    m["s5wg"] = wbd
    m["w_s5u"] = chunked(w_in[:, :, 2312:2568], 128)
    m["jidx"] = f(np.broadcast_to(np.arange(512, dtype=np.float32)[None, :], (128, 512)))
    return m


_CACHE = {}


def kernel(**inputs):
    cfg = {}
    if "B" not in _CACHE:
        _CACHE["B"] = build(cfg)
    B = _CACHE["B"]
    in_maps = []
    for b in range(8):
        m = host_inputs(inputs, b)
        in_maps.append({k: m[k] for k in B.din})
    res = run_bass_kernel_spmd(B.nc, in_maps, core_ids=list(range(8)))
    out = np.stack([res.results[b]["out"] for b in range(8)], axis=0)
    return out.astype(np.float32)


def _declare_mixer(self):
    self.pos_in = self.inp("pos", [S], I32)
    self.ropec_in = self.inp("ropec", [128, 2])
    self.w_qk = self.inp("w_qk", [DEPTH, 8, 128, KC, 256])
    self.w_v = self.inp("w_v", [DEPTH, 2, 128, KC, 256])
    self.w_mlx = self.inp("w_mlx", [DEPTH, 2, 128, KC, 128])
    self.w_mlvo = self.inp("w_mlvo", [DEPTH, 2, 128, KC, 256])
    self.w_mli = self.inp("w_mli", [DEPTH, D, 4])
    self.w_mlf = self.inp("w_mlf", [DEPTH, D, 4])
    self.tri_in = self.inp("tri", [128, 128])
    self.lamv_in = self.inp("lamv", [DEPTH, 256])
    self.da_g_in = self.inp("da_g", [DEPTH, 128])
    self.convw_in = self.inp("convw", [128, DEPTH, 2, 4])
    self.convb_in = self.inp("convb", [128, DEPTH, 2])
    self.wq_bd = self.inp("wq_bd", [DEPTH, 2, 128, 128])
    self.wk_bd = self.inp("wk_bd", [DEPTH, 2, 128, 128])
    self.gbi_in = self.inp("gate_bi", [DEPTH, 4, 1])
    self.gbf_in = self.inp("gate_bf", [DEPTH, 4, 1])
    self.sel4_in = self.inp("sel4", [4, 4, 128])
    self.ml_g_in = self.inp("ml_g", [DEPTH, 256])
    self.w_out = self.inp("w_out", [DEPTH, D, D])
    P = self.P
    self.trib = P.sbuf("trib", [128, 128], BF16)
    self.ropec = P.sbuf("ropec_s", [128, 2], F32)
    self.one1 = P.sbuf("one1", [128, 1], F32)


def _emit_mixer_consts(self):
    P = self.P
    tmp = self.carve([128, 128], F32)
    P.dma(lambda e: e.dma_start(out=tmp[:], in_=self.tri_in[:, :]), writes=["tritmp"], chan="c0")
    P.dve(lambda e: e.tensor_copy(out=self.trib[:], in_=tmp[:]), reads=["tritmp"], writes=["trib"])
    P.dma(lambda e: e.dma_start(out=self.ropec[:], in_=self.ropec_in[:, :]), writes=["ropec"], chan="c0")
    P.dve(lambda e: e.memset(self.one1[:], 1.0), writes=["one1"])


def _proj_fm(self, wsrc, ncol, consume, tag):
    P, ps = self.P, self.ps
    nchunk = ncol // 128
    for c in range(nchunk):
        s = c % 2
        wt = self.wst[s]
        P.dma(lambda e, c=c, wt=wt: e.dma_start(out=wt[:, :, 0:128], in_=wsrc[c]),
              writes=[("wst", s)], chan=f"wst{s}", eng="pool")
        for tg in range(4):
            bank = (c * 4 + tg) % 2
            for kc in range(KC):
                P.pe(lambda e, kc=kc, tg=tg, bank=bank, wt=wt: e.matmul(ps[bank][:], lhsT=wt[:, kc, 0:128], rhs=self.HT[:, kc, tg * 512:(tg + 1) * 512],
                                                                      start=(kc == 0), stop=(kc == KC - 1)),
                     reads=[("wst", s), ("HT", kc, tg)], writes=[("ps", bank)])
            consume(c, tg, bank)


def _proj_tm(self, wsrc, ncol, consume):
    P, ps = self.P, self.ps
    wt = self.wst[0]
    P.dma(lambda e: e.dma_start(out=wt[:, :, 0:ncol], in_=wsrc), writes=[("wst", 0)], chan="wst0", eng="pool")
    for tt in range(NT):
        bank = 2 + (tt % 2)
        for kc in range(KC):
            P.pe(lambda e, kc=kc, tt=tt, bank=bank: e.matmul(ps[bank][:, 0:ncol], lhsT=self.HT[:, kc, tt * 128:(tt + 1) * 128], rhs=wt[:, kc, 0:ncol],
                                                           start=(kc == 0), stop=(kc == KC - 1)),
                 reads=[("wst", 0)] + [("HT", kc, tt // 4)], writes=[("ps", bank)])
        consume(tt, bank)


def _blocks_flush(self, n_keep=0):
    pend = self.__dict__.setdefault("_bpend", [])
    while len(pend) > n_keep:
        fn = pend.pop(0)
        fn()


def _blocks(self, name, qt, kT, qT, vaug, vw, out_cb, scale=1.0, dmask=None):
    P, ps = self.P, self.ps
    pt = self.pt
    BP = 2
    SB = (0, 1, 4)
    OB = (5, 6, 7)
    cnt = self.__dict__.setdefault("_bcnt", [0, 0])
    pend = self.__dict__.setdefault("_bpend", [])
    ob = OB[cnt[1] % 3]
    cnt[1] += 1
    nk = qt + 1
    for g0 in range(0, nk, 4):
        n = min(4, nk - g0)
        sb = SB[cnt[0] % 3]
        sl = cnt[0] % 3
        cnt[0] += 1
        for i in range(n):
            kt = g0 + i
            P.pe(lambda e, i=i, kt=kt, sb=sb: e.matmul(ps[sb][:, i * 128:(i + 1) * 128], lhsT=kT(kt), rhs=qT(qt), start=True, stop=True),
                 reads=[(name, "kq")], writes=[("ps", sb)])
        if dmask is None:
            P.act(lambda e, n=n, sb=sb, sl=sl: e.activation(out=pt[sl][:, 0:n * 128], in_=ps[sb][:, 0:n * 128], func=AF.Exp, scale=scale),
                  reads=[("ps", sb)], writes=[("pt", sl)])
        else:
            dsl = cnt[0] % 2
            for i in range(n):
                fr, bi = dmask(g0 + i)
                P.act(lambda e, i=i, fr=fr, bi=bi, dsl=dsl: e.activation(out=self.dt[dsl][:, i * 128:(i + 1) * 128], in_=fr, func=AF.Exp, bias=bi, scale=1.0),
                      reads=[(name, "dm")], writes=[("dt", dsl)])
            P.dve(lambda e, n=n, sb=sb, sl=sl, dsl=dsl: e.tensor_tensor(out=pt[sl][:, 0:n * 128], in0=ps[sb][:, 0:n * 128], in1=self.dt[dsl][:, 0:n * 128], op=ALU.mult),
                  reads=[("ps", sb), ("dt", dsl)], writes=[("pt", sl)])
        if g0 + n - 1 == qt:
            i = n - 1
            P.pool(lambda e, i=i, sl=sl: e.tensor_tensor(out=pt[sl][:, i * 128:(i + 1) * 128], in0=pt[sl][:, i * 128:(i + 1) * 128], in1=self.trib[:], op=ALU.mult),
                   reads=[("pt", sl), "trib"], writes=[("pt", sl)])

        def pv(n=n, g0=g0, sl=sl, ob=ob, last=(g0 + n - 1 == qt)):
            for i in range(n):
                kt = g0 + i
                P.pe(lambda e, i=i, kt=kt: e.matmul(ps[ob][:, 0:vw], lhsT=pt[sl][:, i * 128:(i + 1) * 128], rhs=vaug(kt),
                                                    start=(kt == 0), stop=(kt == qt)),
                     reads=[("pt", sl), (name, "v")], writes=[("ps", ob)])
            if last:
                out_cb(qt, ob)
        pend.append(pv)
        _blocks_flush(self, BP)


def _emit_mlstm(self, l, R1):
    P, ps = self.P, self.ps
    self.wst = [self.carve([128, KC, 256], BF16), self.carve([128, KC, 128], BF16)]
    self.pt = [self.carve([128, 512], BF16) for _ in range(3)]
    self.dt = [self.carve([128, 512], F32) for _ in range(2)]
    xm = self.carve([128, 2, S + 4], BF16)
    xc = self.carve([128, 2, S], BF16)
    QTm = self.carve([128, S], BF16)
    KTm = self.carve([128, S], BF16)
    vml = self.carve([128, NT, 4, 66], BF16)
    sgo = self.carve([128, NT, 256], BF16)
    FT = self.carve([4, S], F32)
    Frow = self.carve([128, S], F32)
    acc = Frow
    fT = Frow[0:4, :]
    iT = xm[0:4, :, :].rearrange("p a b -> p (a b)").bitcast(F32)[:, 0:S]
    biasS = self.carve([128, NT, 4], F32)
    cw = self.carve([128, 2, 4], F32)
    cb = self.carve([128, 2], F32)
    wqb = self.carve([128, 2, 128], BF16)
    wkb = self.carve([128, 2, 128], BF16)
    wtmp = self.carve([128, 2, 128], F32)
    gb = self.carve([4, 2], F32)
    sel4 = self.carve([4, 4, 128], F32)
    mlg = self.carve([128, 256], F32)
    sm16 = self.carve([128, 2, NT], F32)
    Ost = xm[:, :, :].rearrange("p a b -> p (a b)").bitcast(F32)[:, 0:NT * 65].rearrange("p (q c) -> p q c", c=65)
    htmp = self.wst[0][:, :, :].rearrange("p a b -> p (a b)").bitcast(F32)[:, 0:NT * 64].rearrange("p (q c) -> p q c", c=64)

    P.dma(lambda e: e.dma_start(out=cw[:], in_=self.convw_in[:, l]), writes=["cw"], chan="c0")
    P.dma(lambda e: e.dma_start(out=cb[:], in_=self.convb_in[:, l]), writes=["cb"], chan="c0")
    P.dma(lambda e: e.dma_start(out=gb[:, 0:1], in_=self.gbi_in[l]), writes=["gb"], chan="c0")
    P.dma(lambda e: e.dma_start(out=gb[:, 1:2], in_=self.gbf_in[l]), writes=["gb"], chan="c0")
    P.dma(lambda e: e.dma_start(out=sel4[:], in_=self.sel4_in[:, :, :]), writes=["sel4"], chan="c0")
    P.dma(lambda e: e.dma_start(out=mlg[:], in_=self.ml_g_in[l].partition_broadcast(128)), writes=["mlg"], chan="c0")
    for (src, dst, nm) in ((self.wq_bd, wqb, "wqb"), (self.wk_bd, wkb, "wkb")):
        P.dma(lambda e, src=src: e.dma_start(out=wtmp[:], in_=src[l].rearrange("c p e -> p c e")), writes=["wtmp"], chan="c0")
        P.dve(lambda e, dst=dst: e.tensor_copy(out=dst[:], in_=wtmp[:]), reads=["wtmp"], writes=[nm])
    P.dve(lambda e: e.memset(xm[:, :, 0:4], 0.0), writes=["xm"])
    P.dve(lambda e: e.memset(vml[:], 1.0), writes=[("ml", "v")])

    def cons_x(c, tg, bank):
        P.act(lambda e: e.activation(out=xm[:, c, 4 + tg * 512:4 + (tg + 1) * 512], in_=ps[bank][:], func=AF.Identity),
              reads=[("ps", bank)], writes=["xm"])
    _proj_fm(self, self.w_mlx[l], 256, cons_x, "mlx")
    for hc in range(2):
        P.dve(lambda e, hc=hc: e.tensor_scalar(out=acc[:], in0=xm[:, hc, 1:1 + S], scalar1=cw[:, hc, 0:1], scalar2=None, op0=ALU.mult),
              reads=["xm", "cw"], writes=["acc"])
        for j in range(1, 4):
            P.dve(lambda e, hc=hc, j=j: e.scalar_tensor_tensor(out=acc[:], in0=xm[:, hc, 1 + j:1 + j + S], scalar=cw[:, hc, j:j + 1], in1=acc[:],
                                                               op0=ALU.mult, op1=ALU.add),
                  reads=["xm", "cw", "acc"], writes=["acc"])
        P.act(lambda e, hc=hc: e.activation(out=xc[:, hc, :], in_=acc[:], func=AF.Silu, bias=cb[:, hc:hc + 1], scale=1.0),
              reads=["acc", "cb"], writes=["xc"])

    def cons_v2(tt, bank):
        P.act(lambda e: e.activation(out=vml[:, tt, :, 0:64], in_=ps[bank][:, 0:256].rearrange("p (h d) -> p h d", d=64), func=AF.Identity),
              reads=[("ps", bank)], writes=[("ml", "v")])

    def cons_o2(tt, bank):
        P.act(lambda e: e.activation(out=sgo[:, tt, :], in_=ps[bank][:, 0:256], func=AF.Sigmoid),
              reads=[("ps", bank)], writes=["sgo"])
    _proj_tm(self, self.w_mlvo[l, 0], 256, cons_v2)
    _proj_tm(self, self.w_mlvo[l, 1], 256, cons_o2)
    for (wsrc, dstT, nm, alias) in ((self.w_mli, iT, "iT", "xm"), (self.w_mlf, fT, "fT", "acc")):
        wt = self.wst[1]
        P.dma(lambda e, wsrc=wsrc, wt=wt: e.dma_start(out=wt[:, :, 0:4], in_=wsrc[l].rearrange("(kc p) f -> p kc f", p=128)),
              writes=[("wst", 1)], chan="wst1", eng="pool")
        for tg in range(4):
            bank = tg % 2
            for kc in range(KC):
                P.pe(lambda e, kc=kc, tg=tg, bank=bank, wt=wt: e.matmul(ps[bank][0:4, :], lhsT=wt[:, kc, 0:4], rhs=self.HT[:, kc, tg * 512:(tg + 1) * 512],
                                                                      start=(kc == 0), stop=(kc == KC - 1)),
                     reads=[("wst", 1), ("HT", kc, tg)], writes=[("ps", bank)])
            P.act(lambda e, tg=tg, bank=bank, dstT=dstT: e.activation(out=dstT[:, tg * 512:(tg + 1) * 512], in_=ps[bank][0:4, :], func=AF.Identity),
                  reads=[("ps", bank)], writes=[nm, alias])
    P.dve(lambda e: e.tensor_scalar(out=iT, in0=iT, scalar1=gb[:, 0:1], scalar2=None, op0=ALU.add), reads=["iT", "gb"], writes=["iT"])
    P.dve(lambda e: e.tensor_scalar(out=fT, in0=fT, scalar1=gb[:, 1:2], scalar2=-1.0, op0=ALU.add, op1=ALU.mult), reads=["fT", "gb"], writes=["fT"])
    P.act(lambda e: e.activation(out=fT, in_=fT, func=AF.Exp), reads=["fT"], writes=["fT"])
    P.act(lambda e: e.activation(out=fT, in_=fT, func=AF.Ln, bias=self.one1[0:4, 0:1], scale=1.0), reads=["fT", "one1"], writes=["fT"])
    P.dve(lambda e: e.tensor_scalar(out=fT, in0=fT, scalar1=-1.0, scalar2=None, op0=ALU.mult), reads=["fT"], writes=["fT"])
    ones4 = self.dt[0][0:4, :]
    P.dve(lambda e: e.memset(ones4, 1.0), writes=[("dt", 0)])
    for c4 in range(4):
        init = 0.0 if c4 == 0 else FT[:, c4 * 512 - 1:c4 * 512]
        P.dve(lambda e, c4=c4, init=init: e.tensor_tensor_scan(out=FT[:, c4 * 512:(c4 + 1) * 512], data0=ones4, data1=fT[:, c4 * 512:(c4 + 1) * 512],
                                                               initial=init, op0=ALU.mult, op1=ALU.add),
              reads=["fT", ("dt", 0), "FT"], writes=["FT"])
    P.dve(lambda e: e.tensor_tensor(out=iT, in0=iT, in1=FT[:], op=ALU.subtract), reads=["iT", "FT"], writes=["iT"])
    for tt in range(NT):
        P.pe(lambda e, tt=tt: e.transpose(out=ps[2][:, tt * 4:(tt + 1) * 4], in_=iT[:, tt * 128:(tt + 1) * 128], identity=self.ident[0:4, 0:4]),
             reads=["iT", "ident"], writes=[("ps", 2)])
    P.dve(lambda e: e.tensor_copy(out=biasS[:].rearrange("p a b -> p (a b)"), in_=ps[2][:, 0:64]), reads=[("ps", 2)], writes=["biasS"])

    for g in range(4):
        hc, hl = g // 2, g % 2
        if hl == 0:
            for tg in range(4):
                for (wb, dstT, sc, nm) in ((wqb, QTm, 1.0, "wqb"), (wkb, KTm, 0.125, "wkb")):
                    bank = (tg % 2)
                    P.pe(lambda e, hc=hc, tg=tg, wb=wb, bank=bank: e.matmul(ps[bank][:], lhsT=wb[:, hc, :], rhs=xc[:, hc, tg * 512:(tg + 1) * 512], start=True, stop=True),
                         reads=["xc", nm], writes=[("ps", bank)])
                    P.act(lambda e, tg=tg, dstT=dstT, sc=sc, bank=bank: e.activation(out=dstT[:, tg * 512:(tg + 1) * 512], in_=ps[bank][:], func=AF.Copy, scale=sc),
                          reads=[("ps", bank)], writes=[("ml", "kq")])
        for tg in range(4):
            bank = 2 + tg % 2
            P.pe(lambda e, tg=tg, bank=bank, g=g: e.matmul(ps[bank][:], lhsT=sel4[:, g, :], rhs=FT[:, tg * 512:(tg + 1) * 512], start=True, stop=True),
                 reads=["FT", "sel4"], writes=[("ps", bank)])
            P.act(lambda e, tg=tg, bank=bank: e.activation(out=Frow[:, tg * 512:(tg + 1) * 512], in_=ps[bank][:], func=AF.Identity),
                  reads=[("ps", bank), "fT", "acc"], writes=[("ml", "dm"), "acc", "fT"])

        def out_cb(qt, ob, g=g):
            P.dve(lambda e: e.tensor_copy(out=Ost[:, qt, :], in_=ps[ob][:, 0:65]), reads=[("ps", ob), "xm", "iT"], writes=["Ost", "xm"])

        for qt in range(NT):
            _blocks(self, "ml", qt,
                    kT=lambda kt, hl=hl: KTm[hl * 64:(hl + 1) * 64, kt * 128:(kt + 1) * 128],
                    qT=lambda qt_, hl=hl: QTm[hl * 64:(hl + 1) * 64, qt_ * 128:(qt_ + 1) * 128],
                    vaug=lambda kt, g=g: vml[:, kt, g, 0:65], vw=65, out_cb=out_cb,
                    dmask=lambda kt, qt=qt, g=g: (Frow[:, qt * 128:(qt + 1) * 128], biasS[:, kt, g:g + 1]))
        _blocks_flush(self)
        num = Ost[:, :, 0:64]
        b16 = lambda ap: ap.unsqueeze(2).to_broadcast([128, NT, 64])
        EK = dict(reads=["Ost", "htmp", "sm16", "mlg", "sgo", "eps", ("wst", 0)], writes=["Ost", "htmp", "sm16", ("wst", 0)])
        P.act(lambda e: e.activation(out=sm16[:, 0, :], in_=Ost[:, :, 64], func=AF.Abs), **EK)
        P.dve(lambda e: e.tensor_scalar(out=sm16[:, 0, :], in0=sm16[:, 0, :], scalar1=1.0, scalar2=None, op0=ALU.max), **EK)
        P.dve(lambda e: e.reciprocal(out=sm16[:, 0, :], in_=sm16[:, 0, :]), **EK)
        P.dve(lambda e: e.tensor_tensor(out=htmp[:], in0=num, in1=b16(sm16[:, 0, :]), op=ALU.mult), **EK)
        P.dve(lambda e: e.tensor_tensor(out=num, in0=htmp[:], in1=htmp[:], op=ALU.mult), **EK)
        P.dve(lambda e: e.tensor_reduce(out=sm16[:, 1, :], in_=num, axis=AX.X, op=ALU.add), **EK)
        P.act(lambda e: e.activation(out=sm16[:, 1, :], in_=sm16[:, 1, :], func=AF.Sqrt, bias=self.eps[:, 0:1], scale=1.0 / 64.0), **EK)
        P.dve(lambda e: e.reciprocal(out=sm16[:, 1, :], in_=sm16[:, 1, :]), **EK)
        P.dve(lambda e: e.tensor_tensor(out=htmp[:], in0=htmp[:], in1=b16(sm16[:, 1, :]), op=ALU.mult), **EK)
        P.dve(lambda e, g=g: e.tensor_tensor(out=htmp[:], in0=htmp[:], in1=mlg[:, g * 64:(g + 1) * 64].unsqueeze(1).to_broadcast([128, NT, 64]), op=ALU.mult), **EK)
        P.dve(lambda e, g=g: e.tensor_tensor(out=sgo[:, :, g * 64:(g + 1) * 64], in0=htmp[:], in1=sgo[:, :, g * 64:(g + 1) * 64], op=ALU.mult),
              reads=["htmp", "sgo"], writes=["sgo"])
    for qt in range(NT):
        for hc in range(2):
            tb = 2 + ((qt * 2 + hc) % 2)
            psb = ps[tb][:].bitcast(BF16)
            P.pe(lambda e, qt=qt, hc=hc, psb=psb: e.transpose(out=psb[:, 0:128], in_=sgo[:, qt, hc * 128:(hc + 1) * 128], identity=self.identb[:]),
                 reads=["sgo", "identb"], writes=[("ps", tb)])
            P.act(lambda e, qt=qt, hc=hc, psb=psb: e.activation(out=R1[:, hc, qt * 128:(qt + 1) * 128], in_=psb[:, 0:128], func=AF.Identity),
                  reads=[("ps", tb)], writes=[("R1", hc)])


def _emit_da(self, l, R1):
    P, ps = self.P, self.ps
    lam_init = 0.8 - 0.6 * math.exp(-0.3 * l)
    wst2 = self.carve([128, 2 * KC * 256], BF16)
    self.wst = [wst2[:, i * KC * 256:(i + 1) * KC * 256].rearrange("p (k f) -> p k f", f=256) for i in range(2)]
    odah = wst2.bitcast(F32)[:, 0:NT * 128].rearrange("p (q c) -> p q c", c=128)
    odbh = wst2[:, 0:NT * 128].rearrange("p (q c) -> p q c", c=128)
    self.pt = [self.carve([128, 512], BF16) for _ in range(3)]
    QT = self.carve([128, 4, S], BF16)
    KT = self.carve([128, 4, S], BF16)
    Va = self.carve([128, NT, 4, 130], BF16)
    t1 = self.carve([128, 512], F32)
    sq5 = self.carve([128, 5 * 512], F32)
    cosT, sinT, posf, t2, twopi = [sq5[:, i * 512:(i + 1) * 512] for i in range(5)]
    posi = t2.bitcast(I32)
    sqh = sq5[:, 0:NT * 128].rearrange("p (q c) -> p q c", c=128)
    lamt = self.carve([128, 256], F32)
    lam = self.carve([128, 8], F32)
    dag = self.carve([128, 128], F32)
    sq = self.carve([128, 128], F32)
    sd = self.carve([128, 8], F32)
    sd16 = self.carve([128, NT], F32)
    YA = self.HT
    self.da_t1 = t1

    wst3 = Va[:, :, :, :].rearrange("p a b c -> p (a b c)")[:, 0:KC * 256].rearrange("p (k f) -> p k f", f=256)
    wslots = [self.wst[0], self.wst[1], wst3]
    P.dma(lambda e: e.dma_start(out=lamt[:], in_=self.lamv_in[l].partition_broadcast(128)), writes=["lamt"], chan="c0")
    P.dma(lambda e: e.dma_start(out=dag[:], in_=self.da_g_in[l].partition_broadcast(128)), writes=["dag"], chan="c0")
    for i in range(2):
        P.dve(lambda e, i=i: e.tensor_tensor(out=sq[:, 0:64], in0=lamt[:, i * 128:i * 128 + 64], in1=lamt[:, i * 128 + 64:i * 128 + 128], op=ALU.mult),
              reads=["lamt"], writes=["sq"])
        P.dve(lambda e, i=i: e.tensor_reduce(out=lam[:, i:i + 1], in_=sq[:, 0:64], axis=AX.X, op=ALU.add), reads=["sq"], writes=["lam"])
    P.act(lambda e: e.activation(out=lam[:, 0:2], in_=lam[:, 0:2], func=AF.Exp), reads=["lam"], writes=["lam"])
    P.dve(lambda e: e.tensor_tensor(out=lam[:, 2:3], in0=lam[:, 1:2], in1=lam[:, 0:1], op=ALU.subtract), reads=["lam"], writes=["lam"])
    P.dve(lambda e: e.tensor_scalar(out=lam[:, 2:3], in0=lam[:, 2:3], scalar1=-lam_init, scalar2=None, op0=ALU.add), reads=["lam"], writes=["lam"])
    P.dve(lambda e: e.tensor_scalar(out=dag[:], in0=dag[:], scalar1=float(1.0 - lam_init), scalar2=None, op0=ALU.mult), reads=["dag"], writes=["dag"])

    for tg in range(4):
        P.dma(lambda e, tg=tg: e.dma_start(out=posi, in_=self.pos_in[tg * 512:(tg + 1) * 512].partition_broadcast(128)),
              writes=["t2"], chan="c0")
        P.dve(lambda e: e.tensor_copy(out=posf[:], in_=posi), reads=["t2"], writes=["posf"])
        TWO_PI = 2.0 * math.pi
        for (dst, sh_, nm) in ((sinT, 0.0, "sinT"), (cosT, 0.5 * math.pi, "cosT")):
            P.dve(lambda e, sh_=sh_: e.tensor_scalar(out=t1[:], in0=posf[:], scalar1=self.ropec[:, 0:1], scalar2=sh_, op0=ALU.mult, op1=ALU.add),
                  reads=["posf", "ropec"], writes=["t1"])
            P.dve(lambda e: e.tensor_scalar(out=posi, in0=t1[:], scalar1=1.0 / TWO_PI, scalar2=None, op0=ALU.mult), reads=["t1"], writes=["t2"])
            P.dve(lambda e: e.tensor_copy(out=twopi[:], in_=posi), reads=["t2"], writes=["twopi"])
            P.dve(lambda e: e.scalar_tensor_tensor(out=t1[:], in0=twopi[:], scalar=-TWO_PI, in1=t1[:], op0=ALU.mult, op1=ALU.add),
                  reads=["twopi", "t1"], writes=["t1"])
            P.dve(lambda e: e.tensor_scalar(out=twopi[:], in0=t1[:], scalar1=math.pi, scalar2=-TWO_PI, op0=ALU.is_gt, op1=ALU.mult), reads=["t1"], writes=["twopi"])
            P.dve(lambda e: e.tensor_tensor(out=t1[:], in0=t1[:], in1=twopi[:], op=ALU.add), reads=["t1", "twopi"], writes=["t1"])
            P.dve(lambda e: e.tensor_scalar(out=twopi[:], in0=t1[:], scalar1=-math.pi, scalar2=TWO_PI, op0=ALU.is_lt, op1=ALU.mult), reads=["t1"], writes=["twopi"])
            P.dve(lambda e: e.tensor_tensor(out=t1[:], in0=t1[:], in1=twopi[:], op=ALU.add), reads=["t1", "twopi"], writes=["t1"])
            P.dve(lambda e: e.tensor_scalar(out=t1[:], in0=t1[:], scalar1=3.1415925, scalar2=-3.1415925, op0=ALU.min, op1=ALU.max), reads=["t1"], writes=["t1"])
            P.act(lambda e, dst=dst: e.activation(out=dst[:], in_=t1[:], func=AF.Sin), reads=["t1"], writes=[nm])
        P.dve(lambda e: e.tensor_scalar(out=sinT[:], in0=sinT[:], scalar1=self.ropec[:, 1:2], scalar2=None, op0=ALU.mult), reads=["sinT", "ropec"], writes=["sinT"])
        for c in range(8):
            step = tg * 8 + c
            s = step % 3
            wt = wslots[s]
            P.dma(lambda e, wt=wt, c=c: e.dma_start(out=wt[:, :, 0:256], in_=self.w_qk[l, c]), writes=[("wst", s)], chan=f"wst{s}", eng="pool")
            b0, b1 = (0, 1) if c % 2 == 0 else (2, 3)
            ta_, tb_, ka, kb = (t1, t2, "t1", "t2") if c % 2 == 0 else (posf, twopi, "posf", "twopi")
            for kc in range(KC):
                P.pe(lambda e, kc=kc, wt=wt, tg=tg, b0=b0: e.matmul(ps[b0][:], lhsT=wt[:, kc, 0:128], rhs=self.HT[:, kc, tg * 512:(tg + 1) * 512], start=(kc == 0), stop=(kc == KC - 1)),
                     reads=[("wst", s), ("HT", kc, tg)], writes=[("ps", b0)])
            for kc in range(KC):
                P.pe(lambda e, kc=kc, wt=wt, tg=tg, b1=b1: e.matmul(ps[b1][:], lhsT=wt[:, kc, 128:256], rhs=self.HT[:, kc, tg * 512:(tg + 1) * 512], start=(kc == 0), stop=(kc == KC - 1)),
                     reads=[("wst", s), ("HT", kc, tg)], writes=[("ps", b1)])
            dstT = QT if c < 4 else KT
            P.dve(lambda e, ta_=ta_, b0=b0: e.tensor_tensor(out=ta_[:], in0=ps[b0][:], in1=cosT[:], op=ALU.mult), reads=[("ps", b0), "cosT"], writes=[ka])
            P.dve(lambda e, tb_=tb_, b1=b1: e.tensor_tensor(out=tb_[:], in0=ps[b1][:], in1=sinT[:], op=ALU.mult), reads=[("ps", b1), "sinT"], writes=[kb])
            P.dve(lambda e, dstT=dstT, c=c, tg=tg, ta_=ta_, tb_=tb_: e.tensor_tensor(out=dstT[:, c % 4, tg * 512:(tg + 1) * 512], in0=ta_[:], in1=tb_[:], op=ALU.add),
                  reads=[ka, kb], writes=[("da", "kq")])

    P.dve(lambda e: e.memset(Va[:], 1.0), reads=[("wst", 2)], writes=[("da", "v"), ("wst", 2)])

    def cons_v(tt, bank):
        P.act(lambda e: e.activation(out=Va[:, tt, :, 0:128], in_=ps[bank][:, 0:256].rearrange("p (h d) -> p h d", d=128), func=AF.Identity),
              reads=[("ps", bank)], writes=[("da", "v")])

    def cons_v_b(tt, bank):
        P.act(lambda e: e.activation(out=Va[:, tt, 2:4, 0:128], in_=ps[bank][:, 0:256].rearrange("p (h d) -> p h d", d=128), func=AF.Identity),
              reads=[("ps", bank)], writes=[("da", "v")])

    def cons_v_a(tt, bank):
        P.act(lambda e: e.activation(out=Va[:, tt, 0:2, 0:128], in_=ps[bank][:, 0:256].rearrange("p (h d) -> p h d", d=128), func=AF.Identity),
              reads=[("ps", bank)], writes=[("da", "v")])
    _proj_tm(self, self.w_v[l, 0], 256, cons_v_a)
    _proj_tm(self, self.w_v[l, 1], 256, cons_v_b)
    self.P.barrier()

    def da_out(g, qt, ob):
        h, c = g // 2, g % 2
        if c == 0:
            P.dve(lambda e: e.reciprocal(out=sd[:, 0:1], in_=ps[ob][:, 128:129]), reads=[("ps", ob)], writes=["sd"])
            P.dve(lambda e: e.tensor_scalar(out=odah[:, qt, :], in0=ps[ob][:, 0:128], scalar1=sd[:, 0:1], scalar2=None, op0=ALU.mult),
                  reads=[("ps", ob), "sd", ("wst", 0), ("wst", 1)], writes=["odah", ("wst", 0), ("wst", 1)])
            return
        P.dve(lambda e: e.reciprocal(out=sd[:, 1:2], in_=ps[ob][:, 128:129]), reads=[("ps", ob)], writes=["sd1"])
        P.dve(lambda e: e.tensor_tensor(out=sd[:, 1:2], in0=sd[:, 1:2], in1=lam[:, 2:3], op=ALU.mult), reads=["sd1", "lam"], writes=["sd1"])
        P.dve(lambda e: e.scalar_tensor_tensor(out=odah[:, qt, :], in0=ps[ob][:, 0:128], scalar=sd[:, 1:2], in1=odah[:, qt, :], op0=ALU.mult, op1=ALU.add),
              reads=[("ps", ob), "sd1", "odah"], writes=["odah"])

    for h in range(4):
        for qt in range(NT):
            for c in range(2):
                _blocks(self, "da", qt,
                        kT=lambda kt, h=h, c=c: KT[c * 64:(c + 1) * 64, h, kt * 128:(kt + 1) * 128],
                        qT=lambda qt_, h=h, c=c: QT[c * 64:(c + 1) * 64, h, qt_ * 128:(qt_ + 1) * 128],
                        vaug=lambda kt, h=h: Va[:, kt, h, 0:129], vw=129,
                        out_cb=lambda qt_, ob, h=h, c=c: da_out(2 * h + c, qt_, ob), scale=0.125)
        _blocks_flush(self)
        bq = lambda ap: ap.unsqueeze(2).to_broadcast([128, NT, 128])
        DK = dict(reads=["odah", "sqh", "sd16", "dag", "eps", "cosT", "sinT", "posf", "t2", "twopi"], writes=["odah", "sqh", "sd16", "cosT", "sinT", "posf", "t2", "twopi"])
        P.dve(lambda e: e.tensor_tensor(out=sqh[:], in0=odah[:], in1=odah[:], op=ALU.mult), **DK)
        P.dve(lambda e: e.tensor_reduce(out=sd16[:], in_=sqh[:], axis=AX.X, op=ALU.add), **DK)
        P.act(lambda e: e.activation(out=sd16[:], in_=sd16[:], func=AF.Sqrt, bias=self.eps[:, 0:1], scale=1.0 / 128.0), **DK)
        P.dve(lambda e: e.reciprocal(out=sd16[:], in_=sd16[:]), **DK)
        P.dve(lambda e: e.tensor_tensor(out=sqh[:], in0=odah[:], in1=bq(sd16[:]), op=ALU.mult), **DK)
        P.dve(lambda e: e.tensor_tensor(out=odbh[:], in0=sqh[:], in1=dag[:].unsqueeze(1).to_broadcast([128, NT, 128]), op=ALU.mult), **DK)
        for q4 in range(4):
            tb = 2 + (q4 % 2)
            psb = ps[tb][:].bitcast(BF16)
            for i in range(4):
                qt = q4 * 4 + i
                P.pe(lambda e, qt=qt, i=i, psb=psb: e.transpose(out=psb[:, i * 128:(i + 1) * 128], in_=odbh[:, qt, :], identity=self.identb[:]),
                     reads=["odah", "identb"], writes=[("ps", tb)])
            P.act(lambda e, q4=q4, h=h, psb=psb: e.activation(out=YA[:, h, q4 * 512:(q4 + 1) * 512], in_=psb[:, 0:512], func=AF.Identity),
                  reads=[("ps", tb)], writes=[("YA", h)])
    return QT


def _emit_wout(self, l, R1, wmem):
    P, ps = self.P, self.ps
    X = self.X
    YA = self.HT
    t1 = self.da_t1
    wo = self.w_out[l].rearrange("(kc p) f -> p kc f", p=128)
    wob = [wmem[:, 0:2, :].rearrange("p a (k f) -> p (a k) f", f=512), wmem[:, 2:4, :].rearrange("p a (k f) -> p (a k) f", f=512)]
    for dh in range(2):
        P.dma(lambda e, dh=dh: e.dma_start(out=wob[dh], in_=wo[:, :, dh * 512:(dh + 1) * 512]), writes=[("wob", dh)], chan=f"wob{dh}", eng="pool")
    for tt in range(NT):
        for dh in range(2):
            bank = 4 + ((tt * 2 + dh) % 4)
            for kc in range(KC):
                src = YA[:, kc, tt * 128:(tt + 1) * 128] if kc < 4 else R1[:, kc - 4, tt * 128:(tt + 1) * 128]
                rk = ("YA", kc) if kc < 4 else ("R1", kc - 4)
                P.pe(lambda e, kc=kc, dh=dh, bank=bank, src=src: e.matmul(ps[bank][:], lhsT=src, rhs=wob[dh][:, kc, :], start=(kc == 0), stop=(kc == KC - 1)),
                     reads=[rk, ("wob", dh)], writes=[("ps", bank)])
            P.dve(lambda e, tt=tt, dh=dh, bank=bank: e.tensor_tensor(out=t1[:], in0=ps[bank][:], in1=self.G[:, dh * 512:(dh + 1) * 512], op=ALU.mult),
                  reads=[("ps", bank), "G"], writes=["t1"])
            P.dve(lambda e, tt=tt, dh=dh: e.scalar_tensor_tensor(out=X[:, tt, dh * 512:(dh + 1) * 512], in0=X[:, tt, dh * 512:(dh + 1) * 512], scalar=float(DN_ALPHA),
                                                                 in1=t1[:], op0=ALU.mult, op1=ALU.add),
                  reads=["t1", ("X", tt)], writes=[("X", tt)])
        self.emit_ln(tt)


def _emit_mixer(self, l):
    P = self.P
    self.arena_reset()
    R1 = self.carve([128, 4, S], BF16)
    P.dve(lambda e: e.memset(R1[:, 2:4, :], 0.0), writes=[("R1", 2), ("R1", 3)])
    mark = self.aoff
    _emit_mlstm(self, l, R1)
    if self.cfg.get("s5", True):
        P.barrier()
        self.aoff = mark
        self.emit_s5(l, R1)
    P.barrier()
    self.aoff = mark
    QT = _emit_da(self, l, R1)
    P.barrier()
    _emit_wout(self, l, R1, QT)


def _sin_reduce(self, P, ang, ki, kf, key, keys_extra=()):
    TWO_PI = 2.0 * math.pi
    rk = [key] + list(keys_extra)
    P.dve(lambda e: e.tensor_scalar(out=ki, in0=ang, scalar1=1.0 / TWO_PI, scalar2=None, op0=ALU.mult), reads=rk, writes=[key + "_ki"])
    P.dve(lambda e: e.tensor_copy(out=kf, in_=ki), reads=[key + "_ki"], writes=[key + "_kf"])
    P.dve(lambda e: e.scalar_tensor_tensor(out=ang, in0=kf, scalar=-TWO_PI, in1=ang, op0=ALU.mult, op1=ALU.add), reads=[key + "_kf", key], writes=[key])
    P.dve(lambda e: e.tensor_scalar(out=kf, in0=ang, scalar1=math.pi, scalar2=-TWO_PI, op0=ALU.is_gt, op1=ALU.mult), reads=[key], writes=[key + "_kf"])
    P.dve(lambda e: e.tensor_tensor(out=ang, in0=ang, in1=kf, op=ALU.add), reads=[key, key + "_kf"], writes=[key])
    P.dve(lambda e: e.tensor_scalar(out=kf, in0=ang, scalar1=-math.pi, scalar2=TWO_PI, op0=ALU.is_lt, op1=ALU.mult), reads=[key], writes=[key + "_kf"])
    P.dve(lambda e: e.tensor_tensor(out=ang, in0=ang, in1=kf, op=ALU.add), reads=[key, key + "_kf"], writes=[key])
    P.dve(lambda e: e.tensor_scalar(out=ang, in0=ang, scalar1=3.1415925, scalar2=-3.1415925, op0=ALU.min, op1=ALU.max), reads=[key], writes=[key])


def _sin_reduce_pair(self, P, A, B):
    TWO_PI = 2.0 * math.pi
    ch = (A, B)
    def both(mk):
        for (ang, ki, kf, key) in ch:
            mk(ang, ki, kf, key)
    both(lambda ang, ki, kf, key: P.dve(lambda e: e.tensor_scalar(out=ki, in0=ang, scalar1=1.0 / TWO_PI, scalar2=None, op0=ALU.mult), reads=[key], writes=[key + "_ki"]))
    both(lambda ang, ki, kf, key: P.dve(lambda e: e.tensor_copy(out=kf, in_=ki), reads=[key + "_ki"], writes=[key + "_kf"]))
    both(lambda ang, ki, kf, key: P.dve(lambda e: e.scalar_tensor_tensor(out=ang, in0=kf, scalar=-TWO_PI, in1=ang, op0=ALU.mult, op1=ALU.add), reads=[key + "_kf", key], writes=[key]))
    both(lambda ang, ki, kf, key: P.dve(lambda e: e.tensor_scalar(out=kf, in0=ang, scalar1=math.pi, scalar2=-TWO_PI, op0=ALU.is_gt, op1=ALU.mult), reads=[key], writes=[key + "_kf"]))
    both(lambda ang, ki, kf, key: P.dve(lambda e: e.tensor_tensor(out=ang, in0=ang, in1=kf, op=ALU.add), reads=[key, key + "_kf"], writes=[key]))
    both(lambda ang, ki, kf, key: P.dve(lambda e: e.tensor_scalar(out=kf, in0=ang, scalar1=-math.pi, scalar2=TWO_PI, op0=ALU.is_lt, op1=ALU.mult), reads=[key], writes=[key + "_kf"]))
    both(lambda ang, ki, kf, key: P.dve(lambda e: e.tensor_tensor(out=ang, in0=ang, in1=kf, op=ALU.add), reads=[key, key + "_kf"], writes=[key]))
    both(lambda ang, ki, kf, key: P.dve(lambda e: e.tensor_scalar(out=ang, in0=ang, scalar1=3.1415925, scalar2=-3.1415925, op0=ALU.min, op1=ALU.max), reads=[key], writes=[key]))


def _declare_s5(self):
    self.s5A_in = self.inp("s5A", [128, DEPTH, 8, 3])
    self.s5A2_in = self.inp("s5A2", [128, DEPTH, 2, 64, 3])
    self.s5bT_in = self.inp("s5bT", [128, DEPTH, 2, 2, 64])
    self.s5cT_in = self.inp("s5cT", [128, DEPTH, 2, 8, 16])
    self.s5bm_in = self.inp("s5bm", [128, 8])
    self.s5cm_in = self.inp("s5cm", [128, 4, 8])
    self.s5d_in = self.inp("s5d", [128, DEPTH, 2])
    self.s5wg_in = self.inp("s5wg", [DEPTH, 2, 2, 128, 128])
    self.w_s5u = self.inp("w_s5u", [DEPTH, 2, 128, KC, 128])
    self.jidx_in = self.inp("jidx", [128, 512])


def _emit_s5(self, l, R1):
    P, ps = self.P, self.ps
    C3 = lambda shape: self.carve(shape, F32)
    self.wst = [self.carve([128, KC, 128], BF16) for _ in range(2)]
    uT = self.carve([128, 2, S], BF16)
    BDr = self.carve([128, 8, 128], BF16)
    BDi = self.carve([128, 8, 128], BF16)
    CBr = self.carve([128, 8, 128], BF16)
    CBi = self.carve([128, 8, 128], BF16)
    wg = self.carve([128, 4, 128], BF16)
    wgt = C3([128, 4, 128])
    A = C3([128, 8, 3])
    A2 = C3([128, 2, 64, 3])
    bT = C3([128, 2, 2, 64])
    cT = C3([128, 2, 8, 16])
    bm = C3([128, 8])
    cm = C3([128, 4, 8])
    dsk = C3([128, 2])
    jidx = C3([128, 512])
    rr = C3([128, 8])
    th = C3([128, 8])
    zst = C3([128, 8, 2])
    T = [C3([128, 128]) for _ in range(8)]
    Ti = C3([128, 128]).bitcast(I32)
    ur, ui, zr, zi, ta, tb2 = [C3([128, 512]) for _ in range(6)]
    cosB = C3([128, 4, 512])
    sinB = C3([128, 4, 512])
    c512 = C3([128, 8])
    s512 = C3([128, 8])
    ns512 = C3([128, 8])
    zin = C3([128, 8, 2])
    ztmp = C3([128, 8])
    ki = C3([128, 512]).bitcast(I32)
    rt = C3([128, 512])
    xr = self.carve([128, 512], BF16)
    xi = self.carve([128, 512], BF16)
    gyb = self.carve([128, 512], BF16)

    ld = lambda dst, src, nm: P.dma(lambda e: e.dma_start(out=dst, in_=src), writes=[nm], chan="c0")
    ld(A[:], self.s5A_in[:, l], "s5A")
    ld(A2[:], self.s5A2_in[:, l], "s5A2")
    ld(bT[:], self.s5bT_in[:, l], "s5bT")
    ld(cT[:], self.s5cT_in[:, l], "s5cT")
    ld(bm[:], self.s5bm_in[:, :], "s5bm")
    ld(cm[:], self.s5cm_in[:, :, :], "s5cm")
    ld(dsk[:], self.s5d_in[:, l], "s5d")
    ld(jidx[:], self.jidx_in[:, :], "jidx")
    for yc in range(2):
        for hf in range(2):
            ld(wgt[:, yc * 2 + hf, :], self.s5wg_in[l, yc, hf], "wgt")
    P.dve(lambda e: e.tensor_copy(out=wg[:], in_=wgt[:]), reads=["wgt"], writes=["wg"])
    P.dve(lambda e: e.memset(zst[:], 0.0), writes=["zst"])

    P.act(lambda e: e.activation(out=rr[:], in_=A[:, :, 2], func=AF.Exp), reads=["s5A"], writes=["dt8"])
    P.dve(lambda e: e.tensor_tensor(out=th[:], in0=A[:, :, 1], in1=rr[:], op=ALU.mult), reads=["s5A", "dt8"], writes=["th"])
    P.dve(lambda e: e.tensor_tensor(out=rr[:], in0=A[:, :, 0], in1=rr[:], op=ALU.mult), reads=["s5A", "dt8", "th"], writes=["dt8"])
    P.act(lambda e: e.activation(out=rr[:], in_=rr[:], func=AF.Exp), reads=["dt8"], writes=["rr"])

    CK = "c512k"
    a8 = T[0][:, 0:8]; a8b = T[1][:, 0:8]; k8 = Ti[:, 0:8]; kf8 = T[2][:, 0:8]
    P.dve(lambda e: e.tensor_scalar(out=a8, in0=th[:], scalar1=512.0, scalar2=None, op0=ALU.mult), reads=["th"], writes=[CK])
    P.dve(lambda e: e.tensor_scalar(out=a8b, in0=a8, scalar1=0.5 * math.pi, scalar2=None, op0=ALU.add), reads=[CK], writes=[CK + "b"])
    _sin_reduce(self, P, a8, k8, kf8, CK)
    P.act(lambda e: e.activation(out=s512[:], in_=a8, func=AF.Sin), reads=[CK], writes=["s512"])
    _sin_reduce(self, P, a8b, k8, kf8, CK + "b", keys_extra=(CK, CK + "_ki", CK + "_kf"))
    P.act(lambda e: e.activation(out=c512[:], in_=a8b, func=AF.Sin), reads=[CK + "b"], writes=["c512"])
    P.dve(lambda e: e.tensor_scalar(out=ns512[:], in0=s512[:], scalar1=-1.0, scalar2=None, op0=ALU.mult), reads=["s512"], writes=["ns512"])

    are = A2[:, :, :, 0].rearrange("p a b -> p (a b)")
    aim = A2[:, :, :, 1].rearrange("p a b -> p (a b)")
    ldt = A2[:, :, :, 2].rearrange("p a b -> p (a b)")
    dt_, mag, ang, sn, cs, t5, t6, t7 = [t[:] for t in T]
    ZK = "zoh"
    def zd(fn):
        P.dve(fn, reads=[ZK, "s5A2", "s5bT", "s512", "c512", "ns512", CK, CK + "b"], writes=[ZK])
    def za(fn):
        P.act(fn, reads=[ZK, "s5A2", "s512", "c512", CK, CK + "b"], writes=[ZK])
    za(lambda e: e.activation(out=dt_, in_=ldt, func=AF.Exp))
    zd(lambda e: e.tensor_tensor(out=mag, in0=are, in1=dt_, op=ALU.mult))
    za(lambda e: e.activation(out=mag, in_=mag, func=AF.Exp))
    zd(lambda e: e.tensor_tensor(out=ang, in0=aim, in1=dt_, op=ALU.mult))
    zd(lambda e: e.tensor_scalar(out=t5, in0=ang, scalar1=0.5 * math.pi, scalar2=None, op0=ALU.add))
    _sin_reduce(self, P, ang, Ti, t6, ZK)
    za(lambda e: e.activation(out=sn, in_=ang, func=AF.Sin))
    _sin_reduce(self, P, t5, Ti, t6, ZK)
    za(lambda e: e.activation(out=cs, in_=t5, func=AF.Sin))
    zd(lambda e: e.tensor_tensor(out=cs, in0=cs, in1=mag, op=ALU.mult))
    zd(lambda e: e.tensor_scalar(out=cs, in0=cs, scalar1=-1.0, scalar2=None, op0=ALU.add))
    zd(lambda e: e.tensor_tensor(out=sn, in0=sn, in1=mag, op=ALU.mult))
    zd(lambda e: e.tensor_tensor(out=t5, in0=are, in1=are, op=ALU.mult))
    zd(lambda e: e.tensor_tensor(out=t6, in0=aim, in1=aim, op=ALU.mult))
    zd(lambda e: e.tensor_tensor(out=t7, in0=t5, in1=t6, op=ALU.add))
    zd(lambda e: e.reciprocal(out=t7, in_=t7))
    zd(lambda e: e.tensor_tensor(out=t5, in0=cs, in1=are, op=ALU.mult))
    zd(lambda e: e.tensor_tensor(out=dt_, in0=sn, in1=aim, op=ALU.mult))
    zd(lambda e: e.tensor_tensor(out=t5, in0=t5, in1=dt_, op=ALU.add))
    zd(lambda e: e.tensor_tensor(out=t5, in0=t5, in1=t7, op=ALU.mult))
    zd(lambda e: e.tensor_tensor(out=t6, in0=sn, in1=are, op=ALU.mult))
    zd(lambda e: e.tensor_tensor(out=dt_, in0=cs, in1=aim, op=ALU.mult))
    zd(lambda e: e.tensor_tensor(out=t6, in0=t6, in1=dt_, op=ALU.subtract))
    zd(lambda e: e.tensor_tensor(out=t6, in0=t6, in1=t7, op=ALU.mult))
    bre = bT[:, 0, :, :].rearrange("p a b -> p (a b)")
    bim = bT[:, 1, :, :].rearrange("p a b -> p (a b)")
    zd(lambda e: e.tensor_tensor(out=mag, in0=t5, in1=bre, op=ALU.mult))
    zd(lambda e: e.tensor_tensor(out=dt_, in0=t6, in1=bim, op=ALU.mult))
    zd(lambda e: e.tensor_tensor(out=mag, in0=mag, in1=dt_, op=ALU.subtract))
    zd(lambda e: e.tensor_tensor(out=ang, in0=t5, in1=bim, op=ALU.mult))
    zd(lambda e: e.tensor_tensor(out=dt_, in0=t6, in1=bre, op=ALU.mult))
    zd(lambda e: e.tensor_tensor(out=ang, in0=ang, in1=dt_, op=ALU.add))
    for (src, dst, nm) in ((mag, BDr, "BDr"), (ang, BDi, "BDi")):
        for sc in range(8):
            for s2 in range(2):
                P.dve(lambda e, src=src, dst=dst, sc=sc, s2=s2: e.tensor_scalar(out=dst[:, sc, s2 * 64:(s2 + 1) * 64], in0=src[:, (sc // 4) * 64:(sc // 4 + 1) * 64],
                                                                               scalar1=bm[:, (sc % 4) * 2 + s2:(sc % 4) * 2 + s2 + 1], scalar2=None, op0=ALU.mult),
                      reads=[ZK, "s5bm"], writes=[nm])
    for (ri, dst, nm, sgn) in ((0, CBr, "CBr", 1.0), (1, CBi, "CBi", -1.0)):
        for sc in range(8):
            P.dve(lambda e, ri=ri, dst=dst, sc=sc, sgn=sgn: e.scalar_tensor_tensor(
                out=dst[:, sc, :].rearrange("p (g h) -> p g h", h=16),
                in0=cT[:, ri, sc, :].unsqueeze(1).to_broadcast([128, 8, 16]), scalar=sgn,
                in1=cm[:, sc % 4, :].unsqueeze(2).to_broadcast([128, 8, 16]), op0=ALU.mult, op1=ALU.mult),
                  reads=["s5cT", "s5cm"], writes=[nm])

    def cons_u(c, tg, bank):
        P.act(lambda e: e.activation(out=uT[:, c, tg * 512:(tg + 1) * 512], in_=ps[bank][:], func=AF.Identity),
              reads=[("ps", bank)], writes=["uT"])
    _proj_fm(self, self.w_s5u[l], 256, cons_u, "s5u")

    GC = 2.0 * math.sqrt(2.0 / math.pi)
    SK = "s5w"
    ALLK = ["k_ur", "k_ui", "k_ta", "k_tb", "k_zr", "k_zi", "k_rt", "k_zin", "k_ztmp", "k_ztmp2"]
    for yc in range(2):
        kiB = zr[:].bitcast(I32)
        for scl in range(4):
            sc = yc * 4 + scl
            P.dve(lambda e, sc=sc: e.tensor_scalar(out=ta[:], in0=jidx[:], scalar1=th[:, sc:sc + 1], scalar2=0.0, op0=ALU.mult, op1=ALU.add),
                  reads=["jidx", "th", SK], writes=[SK, "tgA"] + ALLK)
            P.dve(lambda e, sc=sc: e.tensor_scalar(out=ur[:], in0=jidx[:], scalar1=th[:, sc:sc + 1], scalar2=0.5 * math.pi, op0=ALU.mult, op1=ALU.add),
                  reads=["jidx", "th", SK], writes=["tgB"])
            _sin_reduce_pair(self, P, (ta[:], ki, tb2[:], "tgA"), (ur[:], kiB, ui[:], "tgB"))
            P.act(lambda e, scl=scl: e.activation(out=sinB[:, scl, :], in_=ta[:], func=AF.Sin), reads=["tgA"], writes=["tgA", "s5tab"])
            P.act(lambda e, scl=scl: e.activation(out=cosB[:, scl, :], in_=ur[:], func=AF.Sin), reads=["tgB", "tgA", "tgA_ki", "tgA_kf", "tgB_ki", "tgB_kf"],
                  writes=["tgB", "s5tab", SK] + ALLK)
        for tg in range(4):
            yb = 6 + (tg % 2)
            for scl in range(4):
                sc = yc * 4 + scl
                cosT = cosB[:, scl, :]
                sinT = sinB[:, scl, :]
                P.pe(lambda e, sc=sc, yc=yc, tg=tg: e.matmul(ps[0][:], lhsT=BDr[:, sc, :], rhs=uT[:, yc, tg * 512:(tg + 1) * 512], start=True, stop=True),
                     reads=["BDr", "uT"], writes=[("ps", 0)])
                P.pe(lambda e, sc=sc, yc=yc, tg=tg: e.matmul(ps[1][:], lhsT=BDi[:, sc, :], rhs=uT[:, yc, tg * 512:(tg + 1) * 512], start=True, stop=True),
                     reads=["BDi", "uT"], writes=[("ps", 1)])
                def D(fn, r, w):
                    P.dve(fn, reads=list(r) + ["s5tab"], writes=list(w))
                D(lambda e, sc=sc: e.tensor_copy(out=rt[:], in_=rr[:, sc:sc + 1].to_broadcast([128, 512])), ["rr", "k_rt"], ["k_rt"])
                D(lambda e, cosT=cosT: e.tensor_tensor(out=ur[:], in0=ps[0][:], in1=cosT, op=ALU.mult), [("ps", 0), "k_ur"], ["k_ur"])
                D(lambda e, cosT=cosT: e.tensor_tensor(out=ui[:], in0=ps[1][:], in1=cosT, op=ALU.mult), [("ps", 1), "k_ui"], ["k_ui"])
                D(lambda e, sinT=sinT: e.tensor_tensor(out=ta[:], in0=ps[1][:], in1=sinT, op=ALU.mult), [("ps", 1), "k_ta"], ["k_ta"])
                D(lambda e, sinT=sinT: e.tensor_tensor(out=tb2[:], in0=ps[0][:], in1=sinT, op=ALU.mult), [("ps", 0), "k_tb"], ["k_tb"])
                if tg == 0:
                    D(lambda e, sc=sc: e.memset(zin[:, sc, :], 0.0), ["k_zin"], ["k_zin"])
                else:
                    D(lambda e, sc=sc: e.tensor_tensor(out=ztmp[:, 0:1], in0=zst[:, sc, 0:1], in1=c512[:, sc:sc + 1], op=ALU.mult), ["zst", "c512", "k_ztmp"], ["k_ztmp"])
                    D(lambda e, sc=sc: e.tensor_tensor(out=ztmp[:, 1:2], in0=zst[:, sc, 1:2], in1=c512[:, sc:sc + 1], op=ALU.mult), ["zst", "c512", "k_ztmp2"], ["k_ztmp2"])
                D(lambda e: e.tensor_tensor(out=ur[:], in0=ur[:], in1=ta[:], op=ALU.add), ["k_ur", "k_ta"], ["k_ur"])
                D(lambda e: e.tensor_tensor(out=ui[:], in0=ui[:], in1=tb2[:], op=ALU.subtract), ["k_ui", "k_tb"], ["k_ui"])
                if tg != 0:
                    D(lambda e, sc=sc: e.scalar_tensor_tensor(out=zin[:, sc, 0:1], in0=zst[:, sc, 1:2], scalar=ns512[:, sc:sc + 1], in1=ztmp[:, 0:1],
                                                              op0=ALU.mult, op1=ALU.add), ["zst", "ns512", "k_ztmp", "k_zin"], ["k_zin"])
                    D(lambda e, sc=sc: e.scalar_tensor_tensor(out=zin[:, sc, 1:2], in0=zst[:, sc, 0:1], scalar=s512[:, sc:sc + 1], in1=ztmp[:, 1:2],
                                                              op0=ALU.mult, op1=ALU.add), ["zst", "s512", "k_ztmp2", "k_zin"], ["k_zin"])
                D(lambda e, sc=sc: e.tensor_tensor_scan(out=zr[:], data0=rt[:], data1=ur[:], initial=zin[:, sc, 0:1], op0=ALU.mult, op1=ALU.add),
                  ["k_rt", "k_ur", "k_zin", "k_zr"], ["k_zr"])
                D(lambda e, sc=sc: e.tensor_tensor_scan(out=zi[:], data0=rt[:], data1=ui[:], initial=zin[:, sc, 1:2], op0=ALU.mult, op1=ALU.add),
                  ["k_rt", "k_ui", "k_zin", "k_zi"], ["k_zi"])
                D(lambda e, cosT=cosT: e.tensor_tensor(out=ta[:], in0=zr[:], in1=cosT, op=ALU.mult), ["k_zr", "k_ta"], ["k_ta"])
                D(lambda e, sinT=sinT: e.tensor_tensor(out=ur[:], in0=zr[:], in1=sinT, op=ALU.mult), ["k_zr", "k_ur"], ["k_ur"])
                D(lambda e, sinT=sinT: e.tensor_tensor(out=tb2[:], in0=zi[:], in1=sinT, op=ALU.mult), ["k_zi", "k_tb"], ["k_tb"])
                D(lambda e, cosT=cosT: e.tensor_tensor(out=ui[:], in0=zi[:], in1=cosT, op=ALU.mult), ["k_zi", "k_ui"], ["k_ui"])
                D(lambda e, sc=sc: e.tensor_copy(out=zst[:, sc, 0:1], in_=zr[:, 511:512]), ["k_zr", "zst"], ["zst"])
                D(lambda e, sc=sc: e.tensor_copy(out=zst[:, sc, 1:2], in_=zi[:, 511:512]), ["k_zi", "zst"], ["zst"])
                D(lambda e: e.tensor_tensor(out=xr[:], in0=ta[:], in1=tb2[:], op=ALU.subtract), ["k_ta", "k_tb", "s_xr"], ["s_xr"])
                D(lambda e: e.tensor_tensor(out=xi[:], in0=ur[:], in1=ui[:], op=ALU.add), ["k_ur", "k_ui", "s_xi"], ["s_xi"])
                P.pe(lambda e, sc=sc, scl=scl, yb=yb: e.matmul(ps[yb][:], lhsT=CBr[:, sc, :], rhs=xr[:], start=(scl == 0), stop=False),
                     reads=["CBr", "s_xr"], writes=[("ps", yb)])
                P.pe(lambda e, sc=sc, scl=scl, yb=yb: e.matmul(ps[yb][:], lhsT=CBi[:, sc, :], rhs=xi[:], start=False, stop=(scl == 3)),
                     reads=["CBi", "s_xi"], writes=[("ps", yb)])
            YK = dict(reads=[SK, "uT", "s5d", ("ps", yb)] + ALLK, writes=[SK] + ALLK)
            P.dve(lambda e, yc=yc, tg=tg, yb=yb: e.scalar_tensor_tensor(out=zr[:], in0=uT[:, yc, tg * 512:(tg + 1) * 512], scalar=dsk[:, yc:yc + 1], in1=ps[yb][:],
                                                                       op0=ALU.mult, op1=ALU.add), **YK)
            P.dve(lambda e: e.tensor_tensor(out=zi[:], in0=zr[:], in1=zr[:], op=ALU.mult), **YK)
            P.dve(lambda e: e.tensor_scalar(out=zi[:], in0=zi[:], scalar1=0.044715, scalar2=1.0, op0=ALU.mult, op1=ALU.add), **YK)
            P.dve(lambda e: e.tensor_tensor(out=zi[:], in0=zi[:], in1=zr[:], op=ALU.mult), **YK)
            P.act(lambda e: e.activation(out=zi[:], in_=zi[:], func=AF.Sigmoid, scale=GC), **YK)
            P.dve(lambda e: e.tensor_tensor(out=gyb[:], in0=zi[:], in1=zr[:], op=ALU.mult), reads=[SK, "s_gy"] + ALLK, writes=[SK, "s_gy"] + ALLK)
            P.pe(lambda e, yc=yc: e.matmul(ps[2][:], lhsT=wg[:, yc * 2, :], rhs=gyb[:], start=True, stop=True), reads=["wg", "s_gy"], writes=[("ps", 2)])
            P.pe(lambda e, yc=yc: e.matmul(ps[3][:], lhsT=wg[:, yc * 2 + 1, :], rhs=gyb[:], start=True, stop=True), reads=["wg", "s_gy"], writes=[("ps", 3)])
            P.act(lambda e: e.activation(out=ur[:], in_=ps[3][:], func=AF.Sigmoid), reads=[("ps", 3), SK] + ALLK, writes=[SK] + ALLK)
            P.dve(lambda e, yc=yc, tg=tg: e.tensor_tensor(out=R1[:, 2 + yc, tg * 512:(tg + 1) * 512], in0=ps[2][:], in1=ur[:], op=ALU.mult),
                  reads=[("ps", 2), SK] + ALLK, writes=[("R1", 2 + yc), SK] + ALLK)


Builder.declare_s5 = _declare_s5
Builder.emit_s5 = _emit_s5
Builder.declare_mixer = _declare_mixer
Builder.emit_mixer_consts = _emit_mixer_consts
Builder.emit_mixer = _emit_mixer
```
